# Optimizing a Trainium2 kernel written in Bass

```python
import math
import jax
import jax.numpy as jnp
from jax import lax
import numpy as np

D_MODEL = 1024
BATCH = 16
SEQ = 2048
DEPTH = 2

EPS = 1e-6
PLE_DIM = 256
N_EVEN = (DEPTH + 1) // 2
N_ODD = DEPTH // 2

MOBA_HEADS = 8
MOBA_HEAD_DIM = 64
MOBA_WIDTH = MOBA_HEADS * MOBA_HEAD_DIM
MOBA_BLOCK = 256
MOBA_TOPK = 3
MOBA_QBLOCK = 128
ALIBI_MAX_BIAS = 8.0

SSM_INNER = D_MODEL
SSM_HEAD_DIM = 64
SSM_HEADS = SSM_INNER // SSM_HEAD_DIM
SSM_STATE = 128
SSM_GROUPS = 2
SSM_CONV = 4
SSM_CHUNK = 128
SSM_CONV_DIM = SSM_INNER + 2 * SSM_GROUPS * SSM_STATE

EVEN_IN = 3 * MOBA_WIDTH + SSM_INNER + SSM_CONV_DIM + SSM_HEADS
EVEN_MIX = MOBA_WIDTH + SSM_INNER

GDN_K_HEADS = 8
GDN_V_HEADS = 16
GDN_KEY_DIM = 128
GDN_VALUE_DIM = 128
GDN_QK_WIDTH = GDN_K_HEADS * GDN_KEY_DIM
GDN_V_WIDTH = GDN_V_HEADS * GDN_VALUE_DIM
GDN_CONV = 4
GDN_CHUNK = 64
GDN_CONV_DIM = 2 * GDN_QK_WIDTH + GDN_V_WIDTH
ODD_IN = GDN_CONV_DIM + GDN_V_WIDTH + 2 * GDN_V_HEADS

D_FF = 2816
FFN_CONV = 3

kernel_name = 'hybrid_moba_mamba2_gdn_convffn_ple'


def rmsnorm(x, gain):
    xf = x.astype(jnp.float32)
    y = xf * lax.rsqrt(jnp.mean(xf * xf, axis=-1, keepdims=True) + EPS)
    return (y * gain.astype(jnp.float32)).astype(x.dtype)


def l2norm(x):
    return x * lax.rsqrt(jnp.sum(x * x, axis=-1, keepdims=True) + EPS)


def causal_dwconv(x, w):
    k, c = w.shape
    return lax.conv_general_dilated(
        x, w.astype(x.dtype)[:, None, :], window_strides=(1,), padding=[(k - 1, 0)],
        dimension_numbers=('NWC', 'WIO', 'NWC'), feature_group_count=c)


def moba_attention(q, k, v, slopes):
    s_len, h, dh = q.shape
    nb = -(-s_len // MOBA_BLOCK)
    pad = nb * MOBA_BLOCK - s_len
    kb = jnp.pad(k, ((0, pad), (0, 0), (0, 0))).reshape(nb, MOBA_BLOCK, h, dh).transpose(2, 0, 1, 3)
    vb = jnp.pad(v, ((0, pad), (0, 0), (0, 0))).reshape(nb, MOBA_BLOCK, h, dh).transpose(2, 0, 1, 3)
    scale = dh ** -0.5
    n_sel = min(MOBA_TOPK, nb - 1)
    blk_of = jnp.arange(s_len) // MOBA_BLOCK
    heads = jnp.arange(h)
    if n_sel > 0:
        counts = jnp.minimum(MOBA_BLOCK, s_len - jnp.arange(nb) * MOBA_BLOCK).astype(jnp.float32)
        kbar = kb.astype(jnp.float32).sum(axis=2) / counts[None, :, None]
        gate = jnp.einsum('shd,hnd->hsn', q.astype(jnp.float32), kbar)
        past = jnp.arange(nb)[None, :] < blk_of[:, None]
        gate = jnp.where(past[None], gate, -jnp.inf)
        _, sel = lax.top_k(gate, n_sel)
        sel_valid = sel < blk_of[None, :, None]

    def one_block(c):
        q0 = c * MOBA_QBLOCK
        qc = lax.dynamic_slice_in_dim(q, q0, MOBA_QBLOCK, axis=0)
        qpos = q0 + jnp.arange(MOBA_QBLOCK)
        own = q0 // MOBA_BLOCK
        k_own = lax.dynamic_index_in_dim(kb, own, axis=1, keepdims=False)
        v_own = lax.dynamic_index_in_dim(vb, own, axis=1, keepdims=False)
        dist_own = (qpos[:, None] - (own * MOBA_BLOCK + jnp.arange(MOBA_BLOCK))[None, :]).astype(jnp.float32)
        s_own = jnp.einsum('qhd,hkd->hqk', qc, k_own, preferred_element_type=jnp.float32) * scale \
            - slopes[:, None, None] * dist_own
        s_own = jnp.where(dist_own >= 0, s_own, -jnp.inf)
        if n_sel == 0:
            pr = jax.nn.softmax(s_own, axis=-1).astype(v.dtype)
            return jnp.einsum('hqk,hkd->qhd', pr, v_own)
        sel_c = lax.dynamic_slice_in_dim(sel, q0, MOBA_QBLOCK, axis=1)
        valid_c = lax.dynamic_slice_in_dim(sel_valid, q0, MOBA_QBLOCK, axis=1)
        k_sel = kb[heads[:, None, None], sel_c]
        v_sel = vb[heads[:, None, None], sel_c]
        kpos = sel_c[..., None] * MOBA_BLOCK + jnp.arange(MOBA_BLOCK)
        dist_sel = (qpos[None, :, None, None] - kpos).astype(jnp.float32)
        s_sel = jnp.einsum('qhd,hqjkd->hqjk', qc, k_sel, preferred_element_type=jnp.float32) * scale \
            - slopes[:, None, None, None] * dist_sel
        s_sel = jnp.where(valid_c[..., None], s_sel, -jnp.inf)
        s_all = jnp.concatenate([s_own, s_sel.reshape(h, MOBA_QBLOCK, n_sel * MOBA_BLOCK)], axis=-1)
        pr = jax.nn.softmax(s_all, axis=-1).astype(v.dtype)
        p_own = pr[..., :MOBA_BLOCK]
        p_sel = pr[..., MOBA_BLOCK:].reshape(h, MOBA_QBLOCK, n_sel, MOBA_BLOCK)
        return jnp.einsum('hqk,hkd->qhd', p_own, v_own) + jnp.einsum('hqjk,hqjkd->qhd', p_sel, v_sel)

    out = lax.map(one_block, jnp.arange(s_len // MOBA_QBLOCK))
    return out.reshape(s_len, h, dh)


def ssd_scan(x, dta, bm, cm):
    b, s_len, h, p = x.shape
    g, n = bm.shape[2], bm.shape[3]
    r = h // g
    L = SSM_CHUNK
    nc = s_len // L
    xc = x.reshape(b, nc, L, g, r, p)
    bc = bm.reshape(b, nc, L, g, n)
    cc = cm.reshape(b, nc, L, g, n)
    a = dta.reshape(b, nc, L, g, r).transpose(0, 3, 4, 1, 2)
    acum = jnp.cumsum(a, axis=-1)
    causal = jnp.tril(jnp.ones((L, L), dtype=bool))
    decay = jnp.exp(jnp.where(causal, acum[..., :, None] - acum[..., None, :], -jnp.inf))
    cb = jnp.einsum('bclgn,bcsgn->bgcls', cc, bc)
    y_diag = jnp.einsum('bgrcls,bcsgrp->bclgrp', cb[:, :, None] * decay, xc)
    decay_states = jnp.exp(acum[..., -1:] - acum)
    states = jnp.einsum('bcsgn,bgrcs,bcsgrp->bcgrpn', bc, decay_states, xc)
    chunk_decay = jnp.exp(acum[..., -1])

    def step(hs, inp):
        st, dec = inp
        return hs * dec[..., None, None] + st, hs

    h0 = jnp.zeros((b, g, r, p, n), jnp.float32)
    _, prev = lax.scan(step, h0, (jnp.moveaxis(states, 1, 0), jnp.moveaxis(chunk_decay, 3, 0)))
    y_off = jnp.einsum('bclgn,cbgrpn,bgrcl->bclgrp', cc, prev, jnp.exp(acum))
    return (y_diag + y_off).reshape(b, s_len, h, p)


def mamba2_heads(z, xbc, dt_raw, conv_w, conv_b, dt_bias, a_log, d_skip, norm_w):
    b, s_len, _ = z.shape
    xbc = jax.nn.silu(causal_dwconv(xbc, conv_w) + conv_b.astype(xbc.dtype))
    xs, bm, cm = jnp.split(xbc, [SSM_INNER, SSM_INNER + SSM_GROUPS * SSM_STATE], axis=-1)
    xs = xs.reshape(b, s_len, SSM_HEADS, SSM_HEAD_DIM).astype(jnp.float32)
    bm = bm.reshape(b, s_len, SSM_GROUPS, SSM_STATE).astype(jnp.float32)
    cm = cm.reshape(b, s_len, SSM_GROUPS, SSM_STATE).astype(jnp.float32)
    dt = jax.nn.softplus(dt_raw.astype(jnp.float32) + dt_bias.astype(jnp.float32))
    a = -jnp.exp(a_log.astype(jnp.float32))
    y = ssd_scan(xs * dt[..., None], dt * a, bm, cm) + d_skip.astype(jnp.float32)[:, None] * xs
    yg = (y.reshape(b, s_len, SSM_INNER) * jax.nn.silu(z.astype(jnp.float32)))
    yg = yg.reshape(b, s_len, SSM_GROUPS, SSM_INNER // SSM_GROUPS)
    yg = yg * lax.rsqrt(jnp.mean(yg * yg, axis=-1, keepdims=True) + EPS)
    return (yg.reshape(b, s_len, SSM_INNER) * norm_w.astype(jnp.float32)).astype(z.dtype)


def even_mixer(h, w_in, q_norm, k_norm, conv_w, conv_b, dt_bias, a_log, d_skip, ssm_norm, w_out, slopes):
    b, s_len, _ = h.shape
    proj = h @ w_in
    cuts = [MOBA_WIDTH, 2 * MOBA_WIDTH, 3 * MOBA_WIDTH, 3 * MOBA_WIDTH + SSM_INNER,
            3 * MOBA_WIDTH + SSM_INNER + SSM_CONV_DIM]
    q, k, v, z, xbc, dt_raw = jnp.split(proj, cuts, axis=-1)
    q = rmsnorm(q.reshape(b, s_len, MOBA_HEADS, MOBA_HEAD_DIM), q_norm)
    k = rmsnorm(k.reshape(b, s_len, MOBA_HEADS, MOBA_HEAD_DIM), k_norm)
    v = v.reshape(b, s_len, MOBA_HEADS, MOBA_HEAD_DIM)
    attn = lax.map(lambda t: moba_attention(t[0], t[1], t[2], slopes), (q, k, v))
    ssm = mamba2_heads(z, xbc, dt_raw, conv_w, conv_b, dt_bias, a_log, d_skip, ssm_norm)
    return jnp.concatenate([attn.reshape(b, s_len, MOBA_WIDTH), ssm], axis=-1) @ w_out


def gated_delta_rule(q, k, v, g, beta):
    b, s_len, h, dk = q.shape
    dv = v.shape[-1]
    L = GDN_CHUNK
    nc = s_len // L
    def chunks(t):
        return jnp.moveaxis(t.reshape(b, nc, L, h, *t.shape[3:]), 3, 1)
    qc = chunks(q * dk ** -0.5)
    kc = chunks(k)
    vc = chunks(v)
    bc = chunks(beta)
    gc = jnp.cumsum(chunks(g), axis=-1)
    tril = jnp.tril(jnp.ones((L, L), dtype=bool))
    strict = jnp.tril(jnp.ones((L, L), dtype=bool), -1)
    decay = jnp.exp(jnp.where(tril, gc[..., :, None] - gc[..., None, :], -jnp.inf))
    kbeta = kc * bc[..., None]
    a_mat = jnp.where(strict, jnp.einsum('bhcid,bhcjd->bhcij', kbeta, kc) * decay, 0.0)
    rhs = jnp.concatenate([vc * bc[..., None], kbeta * jnp.exp(gc)[..., None]], axis=-1)
    sol = lax.linalg.triangular_solve(a_mat, rhs, left_side=True, lower=True, unit_diagonal=True)
    u = sol[..., :dv]
    w = sol[..., dv:]
    qk = jnp.einsum('bhcid,bhcjd->bhcij', qc, kc) * decay
    q_dec = qc * jnp.exp(gc)[..., None]
    g_last = gc[..., -1]
    k_dec = kc * jnp.exp(g_last[..., None] - gc)[..., None]

    def step(st, inp):
        qk_i, qd_i, w_i, u_i, kd_i, gl_i = inp
        v_new = u_i - jnp.einsum('bhld,bhdv->bhlv', w_i, st)
        o = jnp.einsum('bhld,bhdv->bhlv', qd_i, st) + jnp.einsum('bhij,bhjv->bhiv', qk_i, v_new)
        st = st * jnp.exp(gl_i)[..., None, None] + jnp.einsum('bhld,bhlv->bhdv', kd_i, v_new)
        return st, o

    xs = tuple(jnp.moveaxis(t, 2, 0) for t in (qk, q_dec, w, u, k_dec, g_last))
    _, o = lax.scan(step, jnp.zeros((b, h, dk, dv), jnp.float32), xs)
    return o.transpose(1, 0, 3, 2, 4).reshape(b, s_len, h, dv)


def gdn_mixer(h, w_in, conv_w, dt_bias, a_log, norm_w, w_out):
    b, s_len, _ = h.shape
    proj = h @ w_in
    qkv, z, beta_raw, a_raw = jnp.split(
        proj, [GDN_CONV_DIM, GDN_CONV_DIM + GDN_V_WIDTH, GDN_CONV_DIM + GDN_V_WIDTH + GDN_V_HEADS], axis=-1)
    qkv = jax.nn.silu(causal_dwconv(qkv, conv_w)).astype(jnp.float32)
    q, k, v = jnp.split(qkv, [GDN_QK_WIDTH, 2 * GDN_QK_WIDTH], axis=-1)
    rep = GDN_V_HEADS // GDN_K_HEADS
    q = jnp.repeat(l2norm(q.reshape(b, s_len, GDN_K_HEADS, GDN_KEY_DIM)), rep, axis=2)
    k = jnp.repeat(l2norm(k.reshape(b, s_len, GDN_K_HEADS, GDN_KEY_DIM)), rep, axis=2)
    v = v.reshape(b, s_len, GDN_V_HEADS, GDN_VALUE_DIM)
    beta = jax.nn.sigmoid(beta_raw.astype(jnp.float32))
    g = -jnp.exp(a_log.astype(jnp.float32)) * jax.nn.softplus(a_raw.astype(jnp.float32) + dt_bias.astype(jnp.float32))
    o = gated_delta_rule(q, k, v, g, beta)
    o = o * lax.rsqrt(jnp.mean(o * o, axis=-1, keepdims=True) + EPS) * norm_w.astype(jnp.float32)
    o = o * jax.nn.silu(z.astype(jnp.float32).reshape(b, s_len, GDN_V_HEADS, GDN_VALUE_DIM))
    return o.reshape(b, s_len, GDN_V_WIDTH).astype(h.dtype) @ w_out


def conv_ffn(h, w_gate, w_up, conv_w, conv_b, w_down):
    gte = causal_dwconv(h @ w_gate, conv_w) + conv_b.astype(h.dtype)
    return (jax.nn.silu(gte) * (h @ w_up)) @ w_down


def per_layer_embedding(x, p_i, norm_w, w_proj, w_gate):
    return jax.nn.sigmoid(rmsnorm(x, norm_w) @ w_gate) * (p_i @ w_proj)


def setup_inputs(seed: int = 0) -> dict:
    key = jax.random.key(seed)
    ks = iter(jax.random.split(key, 40))
    f32 = jnp.float32

    def normal(shape, s=1.0):
        return s * jax.random.normal(next(ks), shape, f32)

    def dense(shape):
        return normal(shape, shape[-2] ** -0.5)

    def gain(shape):
        return 1.0 + normal(shape, 0.02)

    def dt_bias(shape):
        u = jax.random.uniform(next(ks), shape, f32)
        dt = jnp.exp(u * (math.log(0.1) - math.log(1e-3)) + math.log(1e-3))
        return dt + jnp.log(-jnp.expm1(-dt))

    def a_log(shape):
        return jnp.log(jax.random.uniform(next(ks), shape, f32, 1.0, 16.0))

    return {
        'x': normal((BATCH, SEQ, D_MODEL)),
        'p': normal((DEPTH, BATCH, SEQ, PLE_DIM)),
        'norm_mix': gain((DEPTH, D_MODEL)),
        'norm_ffn': gain((DEPTH, D_MODEL)),
        'norm_ple': gain((DEPTH, D_MODEL)),
        'w_in_even': dense((N_EVEN, D_MODEL, EVEN_IN)),
        'moba_q_norm': gain((N_EVEN, MOBA_HEAD_DIM)),
        'moba_k_norm': gain((N_EVEN, MOBA_HEAD_DIM)),
        'ssm_conv_w': normal((N_EVEN, SSM_CONV, SSM_CONV_DIM), SSM_CONV ** -0.5),
        'ssm_conv_b': normal((N_EVEN, SSM_CONV_DIM), 0.02),
        'ssm_dt_bias': dt_bias((N_EVEN, SSM_HEADS)),
        'ssm_a_log': a_log((N_EVEN, SSM_HEADS)),
        'ssm_d': gain((N_EVEN, SSM_HEADS)),
        'ssm_norm': gain((N_EVEN, SSM_INNER)),
        'w_out_even': dense((N_EVEN, EVEN_MIX, D_MODEL)),
        'w_in_odd': dense((N_ODD, D_MODEL, ODD_IN)),
        'gdn_conv_w': normal((N_ODD, GDN_CONV, GDN_CONV_DIM), GDN_CONV ** -0.5),
        'gdn_dt_bias': dt_bias((N_ODD, GDN_V_HEADS)),
        'gdn_a_log': a_log((N_ODD, GDN_V_HEADS)),
        'gdn_norm': gain((N_ODD, GDN_VALUE_DIM)),
        'w_out_odd': dense((N_ODD, GDN_V_WIDTH, D_MODEL)),
        'ffn_w_gate': dense((DEPTH, D_MODEL, D_FF)),
        'ffn_w_up': dense((DEPTH, D_MODEL, D_FF)),
        'ffn_conv_w': normal((DEPTH, FFN_CONV, D_FF), FFN_CONV ** -0.5),
        'ffn_conv_b': normal((DEPTH, D_FF), 0.02),
        'ffn_w_down': dense((DEPTH, D_FF, D_MODEL)),
        'ple_w_proj': dense((DEPTH, PLE_DIM, D_MODEL)),
        'ple_w_gate': dense((DEPTH, D_MODEL, D_MODEL)),
    }


def reference(x, p, norm_mix, norm_ffn, norm_ple, w_in_even, moba_q_norm, moba_k_norm,
              ssm_conv_w, ssm_conv_b, ssm_dt_bias, ssm_a_log, ssm_d, ssm_norm, w_out_even,
              w_in_odd, gdn_conv_w, gdn_dt_bias, gdn_a_log, gdn_norm, w_out_odd,
              ffn_w_gate, ffn_w_up, ffn_conv_w, ffn_conv_b, ffn_w_down, ple_w_proj, ple_w_gate):
    slopes = jnp.exp2(-ALIBI_MAX_BIAS * jnp.arange(1, MOBA_HEADS + 1, dtype=jnp.float32) / MOBA_HEADS)
    for i in range(DEPTH):
        j = i // 2
        h = rmsnorm(x, norm_mix[i])
        if i % 2 == 0:
            x = x + even_mixer(h, w_in_even[j], moba_q_norm[j], moba_k_norm[j], ssm_conv_w[j],
                               ssm_conv_b[j], ssm_dt_bias[j], ssm_a_log[j], ssm_d[j], ssm_norm[j],
                               w_out_even[j], slopes)
        else:
            x = x + gdn_mixer(h, w_in_odd[j], gdn_conv_w[j], gdn_dt_bias[j], gdn_a_log[j],
                              gdn_norm[j], w_out_odd[j])
        h = rmsnorm(x, norm_ffn[i])
        x = x + conv_ffn(h, ffn_w_gate[i], ffn_w_up[i], ffn_conv_w[i], ffn_conv_b[i], ffn_w_down[i])
        x = x + per_layer_embedding(x, p[i], norm_ple[i], ple_w_proj[i], ple_w_gate[i])
    return x
```

```python
from contextlib import ExitStack
import numpy as np
import os
GCUT = int(os.environ.get('GCUT', '99'))
import concourse.bass as bass
import concourse.mybir as mybir
from concourse.bass_utils import run_bass_kernel_spmd

F32 = mybir.dt.float32
BF16 = mybir.dt.bfloat16
AF = mybir.ActivationFunctionType
ALU = mybir.AluOpType
AX = mybir.AxisListType

ENGS = ("pe", "act", "dve", "pool", "sp")
EPOCH = 12000


class Buf:
    def __init__(self, k, name, t):
        self.k = k
        self.name = name
        self.t = t
        self.w = None
        self.r = []
        self.dsem = None
        self.dcnt = 0
        self.psum = False

    def __getitem__(self, idx):
        return self.t[idx]


class K:
    def __init__(self, nc, es):
        self.nc = nc
        self.es = es
        self.ops = {e: [] for e in ENGS}
        self.sems = {}
        self.ecnt = {e: 0 for e in ENGS}
        self.waited = {e: {} for e in ENGS}
        self.stack = [es]
        self.nbuf = 0
        self.ninstr = 0

    def sb(self, name, shape, dt=F32):
        self.uid = getattr(self, "uid", 0) + 1
        name = "%s_u%d" % (name, self.uid)
        t = self.es.enter_context(self.nc.sbuf_tensor(name, list(shape), dt))
        return Buf(self, name, t)

    def ps(self, name, shape, dt=F32):
        t = self.es.enter_context(self.nc.psum_tensor(name, list(shape), dt))
        b = Buf(self, name, t)
        b.psum = True
        return b

    def dram(self, name, shape, dt=F32, kind=None):
        if kind is None:
            t = self.nc.dram_tensor(name, list(shape), dt)
        else:
            t = self.nc.dram_tensor(name, list(shape), dt, kind=kind)
        return Buf(self, name, t.ap())

    def region(self, name):
        return Buf(self, name, None)

    def _dsem(self, b):
        if b.dsem is None:
            key = "d_" + b.name + "_%d" % self.nbuf
            self.nbuf += 1
            self.sems[key] = self.es.enter_context(self.nc.semaphore(key[:40]))
            b.dsem = key
        return b.dsem

    def _deps(self, eng, reads, writes):
        need = {}

        def add(ev, is_war=False):
            if ev is None:
                return
            key, val = ev
            if key.split("#")[0] == eng:
                if eng in ("pe", "sp") or is_war:
                    return
            if need.get(key, 0) < val:
                need[key] = val

        for b in reads:
            add(b.w)
            if b.psum:
                for ev in b.r:
                    if ev[0].split("#")[0] != eng:
                        add(ev)
        for b in writes:
            add(b.w, is_war=True)
            for ev in b.r:
                add(ev, is_war=True)
        out = []
        wd = self.waited[eng]
        for key, val in need.items():
            if wd.get(key, 0) >= val:
                continue
            wd[key] = val
            out.append((key, val))
        return out

    def _commit(self, ev, reads, writes):
        for b in reads:
            b.r.append(ev)
            if len(b.r) > 64:
                m = {}
                for kk, vv in b.r:
                    if m.get(kk, 0) < vv:
                        m[kk] = vv
                b.r = list(m.items())
        for b in writes:
            b.w = ev
            b.r = []

    def op(self, eng, fn, reads=(), writes=()):
        waits = self._deps(eng, reads, writes)
        self.ecnt[eng] += 1
        ep = (self.ecnt[eng] - 1) // EPOCH
        val = self.ecnt[eng] - ep * EPOCH
        ekey = "%s#%d" % (eng, ep)
        if ekey not in self.sems:
            self.sems[ekey] = self.stack[0].enter_context(self.nc.semaphore("es_%s_%d" % (eng, ep)))
        sems = self.sems
        wl = [(sems[k], v) for k, v in waits]
        mysem = sems[ekey]

        def emit(e, fn=fn, wl=wl, mysem=mysem):
            for s, v in wl:
                e.wait_ge(s, v)
            fn(e).then_inc(mysem, 1)

        self.ops[eng].append(emit)
        self._commit((ekey, val), reads, writes)
        self.ninstr += 1

    def dma(self, q, pairs, reads=(), writes=(), sembuf=None):
        assert sembuf is not None
        key = self._dsem(sembuf)
        waits = self._deps(q, reads, writes)
        sems = self.sems
        wl = [(sems[k], v) for k, v in waits]
        sembuf.dcnt += 16 * len(pairs)
        val = sembuf.dcnt
        dsem = sems[key]

        def emit(e, pairs=pairs, wl=wl, dsem=dsem):
            for s, v in wl:
                e.wait_ge(s, v)
            for o, i in pairs:
                e.dma_start(out=o, in_=i).then_inc(dsem, 16)

        self.ops[q].append(emit)
        self._commit((key, val), reads, writes)
        self.ninstr += len(pairs)

    def finish(self, final_bufs):
        nc = self.nc
        finals = []
        for b in final_bufs:
            if b.w is not None:
                finals.append(b.w)
        sems = self.sems

        def fin(e):
            for key, val in finals:
                e.wait_ge(sems[key], val)

        self.ops["sp"].append(fin)
        with nc.Block() as block:
            @block.tensor
            def _(e):
                for f in self.ops["pe"]:
                    f(e)

            @block.scalar
            def _(e):
                for f in self.ops["act"]:
                    f(e)

            @block.vector
            def _(e):
                for f in self.ops["dve"]:
                    f(e)

            @block.gpsimd
            def _(e):
                for f in self.ops["pool"]:
                    f(e)

            @block.sync
            def _(e):
                for f in self.ops["sp"]:
                    f(e)

    def barrier(self):
        targets = []
        for e in ("pe", "act", "dve", "pool"):
            if self.ecnt[e] > 0:
                ep = (self.ecnt[e] - 1) // EPOCH
                targets.append(("%s#%d" % (e, ep), self.ecnt[e] - ep * EPOCH))
        targets += [(key, cnt) for key, cnt in self.dsem_cnt.items() if cnt > 0]
        sems = self.sems
        for eng in ENGS:
            wd = self.waited[eng]
            wl = []
            for key, val in targets:
                if key.split("#")[0] == eng or wd.get(key, 0) >= val:
                    continue
                wd[key] = val
                wl.append((sems[key], val))

            def emit(e, wl=wl):
                for s, v in wl:
                    e.wait_ge(s, v)

            if wl:
                self.ops[eng].append(emit)


def _k_init_extra(self):
    self.dsem_cnt = {}
    self.free_dsems = []
    self.stack = [self.es]
    self._psf = []
    self._psf_i = 0


def _k_dsem(self, b):
    if b.dsem is None:
        if self.free_dsems:
            key = self.free_dsems.pop()
        else:
            key = "d%d" % self.nbuf
            self.nbuf += 1
            self.sems[key] = self.stack[0].enter_context(self.nc.semaphore(key))
            self.dsem_cnt[key] = 0
        b.dsem = key
        b.dcnt = self.dsem_cnt[key]
        self.cur_dsems.append(key)
    return b.dsem


K._dsem = _k_dsem


def _k_dma(self, q, pairs, reads=(), writes=(), sembuf=None, **kw):
    assert sembuf is not None
    key = self._dsem(sembuf)
    waits = self._deps(q, reads, writes)
    sems = self.sems
    wl = [(sems[k_], v) for k_, v in waits]
    sembuf.dcnt += 16 * len(pairs)
    val = sembuf.dcnt
    self.dsem_cnt[key] = val
    dsem = sems[key]

    def emit(e, pairs=pairs, wl=wl, dsem=dsem, kw=kw):
        for s, v in wl:
            e.wait_ge(s, v)
        for o, i in pairs:
            e.dma_start(out=o, in_=i, **kw).then_inc(dsem, 16)

    self.ops[q].append(emit)
    self._commit((key, val), reads, writes)
    self.ninstr += len(pairs)


K.dma = _k_dma


class Stage:
    def __init__(self, k):
        self.k = k

    def __enter__(self):
        k = self.k
        self.es = ExitStack()
        self.prev = k.es
        k.es = self.es
        self.prev_dsems = getattr(k, "cur_dsems", [])
        k.cur_dsems = []
        return self

    def __exit__(self, *a):
        k = self.k
        k.barrier()
        k.free_dsems.extend(k.cur_dsems)
        k.cur_dsems = self.prev_dsems
        k.es = self.prev
        self.es.close()
        return False


EPS = 1e-6
S = 2048
NSEQ = 2
NTOK = NSEQ * S
D = 1024
DFF = 2816
NEG = -30000.0

WSHAPES = {
    "norm_mix": [2, 1024], "norm_ffn": [2, 1024], "norm_ple": [2, 1024],
    "w_in_even": [1, 1024, 4112], "moba_q_norm": [1, 64], "moba_k_norm": [1, 64],
    "ssm_conv_w": [1, 4, 1536], "ssm_conv_b": [1, 1536], "ssm_dt_bias": [1, 16],
    "ssm_a_log": [1, 16], "ssm_d": [1, 16], "ssm_norm": [1, 1024],
    "w_out_even": [1, 1536, 1024], "w_in_odd": [1, 1024, 6176], "gdn_conv_w": [1, 4, 4096],
    "gdn_dt_bias": [1, 16], "gdn_a_log": [1, 16], "gdn_norm": [1, 128],
    "w_out_odd": [1, 2048, 1024], "ffn_w_gate": [2, 1024, 2816], "ffn_w_up": [2, 1024, 2816],
    "ffn_conv_w": [2, 3, 2816], "ffn_conv_b": [2, 2816], "ffn_w_down": [2, 2816, 1024],
    "ple_w_proj": [2, 256, 1024], "ple_w_gate": [2, 1024, 1024],
}


class Ctx:
    pass


def mk_consts(k, C):
    C.identf = k.sb("identf", [128, 128], F32)
    C.identb = k.sb("identb", [128, 128], BF16)
    C.ones32 = k.sb("ones32", [128, 128], F32)
    C.onesb = k.sb("onesb", [128, 128], BF16)
    C.U32 = k.sb("U32", [128, 128], F32)
    C.Ub = k.sb("Ub", [128, 128], BF16)
    C.SU32 = k.sb("SU32", [128, 128], F32)
    C.tribias = k.sb("tribias", [128, 128], BF16)
    C.blk1 = k.sb("blk1", [128, 128], BF16)
    tb32 = k.sb("tb32", [128, 128], F32)
    G = lambda fn, r, w: k.op("pool", fn, reads=r, writes=w)
    V = lambda fn, r, w: k.op("dve", fn, reads=r, writes=w)
    G(lambda e: e.memset(C.ones32[:, :], 1.0), [], [C.ones32])
    G(lambda e: e.affine_select(C.identf[:, :], C.ones32[:, :], pattern=[[-1, 128]], compare_op=ALU.is_equal,
                                fill=0.0, base=0, channel_multiplier=1), [C.ones32], [C.identf])
    G(lambda e: e.affine_select(C.U32[:, :], C.ones32[:, :], pattern=[[1, 128]], compare_op=ALU.is_ge,
                                fill=0.0, base=0, channel_multiplier=-1), [C.ones32], [C.U32])
    G(lambda e: e.affine_select(C.SU32[:, :], C.ones32[:, :], pattern=[[1, 128]], compare_op=ALU.is_gt,
                                fill=0.0, base=0, channel_multiplier=-1), [C.ones32], [C.SU32])
    V(lambda e: e.tensor_scalar(tb32[:, :], C.U32[:, :], -1.0, -NEG, op0=ALU.add, op1=ALU.mult), [C.U32], [tb32])
    V(lambda e: e.tensor_copy(C.tribias[:, :], tb32[:, :]), [tb32], [C.tribias])
    V(lambda e: e.tensor_copy(C.identb[:, :], C.identf[:, :]), [C.identf], [C.identb])
    V(lambda e: e.tensor_copy(C.onesb[:, :], C.ones32[:, :]), [C.ones32], [C.onesb])
    V(lambda e: e.tensor_copy(C.Ub[:, :], C.U32[:, :]), [C.U32], [C.Ub])
    G(lambda e: e.memset(C.blk1[:, :], 0.0), [], [C.blk1])
    G(lambda e: e.memset(C.blk1[0:64, 0:64], 1.0), [], [C.blk1])
    G(lambda e: e.memset(C.blk1[64:128, 64:128], 1.0), [], [C.blk1])
    C.pf = [k.ps("pf%d" % i, [128, 512], F32) for i in range(6)]
    C.pb = [k.ps("pb%d" % i, [128, 1024], BF16) for i in range(2)]
    C.pfi = 0
    C.pfn = 6
    C.pbi = 0
    C.evi = 0


def psf(C):
    C.pfi = (C.pfi + 1) % C.pfn
    return C.pf[C.pfi]


def psb(C):
    C.pbi = (C.pbi + 1) % len(C.pb)
    return C.pb[C.pbi]


class Ring:
    def __init__(self, bufs):
        self.bufs = bufs
        self.i = -1

    def next(self):
        self.i = (self.i + 1) % len(self.bufs)
        return self.bufs[self.i]


def evac_eng(C):
    C.evi += 1
    return "act" if (C.evi % 2) else "dve"


def copy_op(k, eng, out_ap, in_ap, reads, writes):
    if eng == "act":
        k.op("act", lambda e: e.copy(out_ap, in_ap), reads=reads, writes=writes)
    else:
        k.op(eng, lambda e: e.tensor_copy(out_ap, in_ap), reads=reads, writes=writes)


def load_w(k, Wd2, row0, nk, col0, ncols, wt):
    pairs = [(wt[:, kc, 0:ncols], Wd2[row0 + kc * 128: row0 + (kc + 1) * 128, col0:col0 + ncols]) for kc in range(nk)]
    k.dma("pool", pairs, writes=[wt], sembuf=wt)


def load_cols(k, C, dst_fn, rows_ap2, K, nch):
    rw = k.sb("rowsbuf", [K, nch * 128], F32)
    k.dma("sp", [(rw[:, :], rows_ap2)], writes=[rw], sembuf=rw)
    for c in range(nch):
        ps = psf(C)
        k.op("pe", lambda e, ps=ps, c=c: e.transpose(ps[:, 0:K], rw[0:K, c * 128:(c + 1) * 128], C.identf[0:K, 0:K]),
             reads=[rw, C.identf], writes=[ps])
        k.op("dve", lambda e, ps=ps, c=c: e.tensor_copy(dst_fn(c), ps[:, 0:K]), reads=[ps], writes=[])


def norm_to_hT(k, C, src_d, tok0, ntok, gain_ap, hT, hcol0=0):
    gbc = k.sb("gbc", [128, D], F32)
    k.dma("sp", [(gbc[:, :], gain_ap.partition_broadcast(128))], writes=[gbc], sembuf=gbc)
    xr = Ring([k.sb("nx%d" % i, [128, D], F32) for i in range(4)])
    junk = k.sb("njunk", [128, D], BF16)
    hbr = Ring([k.sb("nhb%d" % i, [128, D], BF16) for i in range(3)])
    ssr = Ring([k.sb("nss%d" % i, [128, 2], F32) for i in range(4)])
    for tt in range(ntok // 128):
        xt = xr.next(); hb = hbr.next(); ss = ssr.next()
        r0 = tok0 + tt * 128
        k.dma("sp", [(xt[:, :], src_d[r0:r0 + 128, :])], writes=[xt], sembuf=xt)
        k.op("act", lambda e, ss=ss: e.memzero(ss[:, :]), writes=[ss])
        k.op("act", lambda e, xt=xt, ss=ss: e.activation(junk[:, :], xt[:, :], AF.Square, accum_out=ss[:, 0:1]),
             reads=[xt, ss], writes=[junk, ss])
        k.op("act", lambda e, ss=ss: e.activation(ss[:, 1:2], ss[:, 0:1], AF.Sqrt, bias=EPS, scale=1.0 / D),
             reads=[ss], writes=[ss])
        k.op("dve", lambda e, ss=ss: e.reciprocal(ss[:, 1:2], ss[:, 1:2]),
             reads=[ss], writes=[ss])
        k.op("dve", lambda e, xt=xt, ss=ss, hb=hb: e.scalar_tensor_tensor(hb[:, :], xt[:, :], ss[:, 1:2], gbc[:, :],
                                                                       op0=ALU.mult, op1=ALU.mult),
             reads=[xt, ss, gbc], writes=[hb])
        pt = psb(C)
        for kc in range(8):
            k.op("pe", lambda e, kc=kc, hb=hb, pt=pt: e.transpose(pt[:, kc * 128:(kc + 1) * 128], hb[:, kc * 128:(kc + 1) * 128], C.identb[:, :]),
                 reads=[hb, C.identb], writes=[pt])
        c0 = hcol0 + tt * 128
        hb_ = hT[c0 // 512]
        copy_op(k, evac_eng(C), hb_[:, :, c0 % 512:c0 % 512 + 128], pt[:, :].rearrange("p (a b) -> p a b", a=8), [pt], [hb_])


def linear_tm(k, C, lhsT_fn, nk, Wd2, row0, col0, ncols, ntt, evac, wring, cbw=512):
    for cb in range(0, ncols, cbw):
        n = min(cbw, ncols - cb)
        wt = wring.next()
        load_w(k, Wd2, row0, nk, col0 + cb, n, wt)
        for tt in range(ntt):
            ps = psf(C)
            for kc in range(nk):
                lb, lap = lhsT_fn(kc, tt)
                k.op("pe", lambda e, ps=ps, lap=lap, wt=wt, kc=kc, n=n: e.matmul(ps[:, 0:n], lap, wt[:, kc, 0:n], start=(kc == 0), stop=(kc == nk - 1)),
                     reads=[lb, wt], writes=[ps])
            evac(cb, n, tt, ps)


def linear_fm(k, C, rhs_fn, nk, Wd2, row0, col0, ncols, ntb, evac, wring):
    for cb in range(0, ncols, 512):
        n = min(512, ncols - cb)
        wt = wring.next()
        load_w(k, Wd2, row0, nk, col0 + cb, n, wt)
        for cc in range(0, n, 128):
            m = min(128, n - cc)
            for tb in range(ntb):
                ps = psf(C)
                for kc in range(nk):
                    rb, rap = rhs_fn(kc, tb)
                    k.op("pe", lambda e, ps=ps, rap=rap, wt=wt, kc=kc, cc=cc, m=m: e.matmul(ps[0:m, :], wt[:, kc, cc:cc + m], rap, start=(kc == 0), stop=(kc == nk - 1)),
                         reads=[rb, wt], writes=[ps])
                evac(cb + cc, m, tb, ps)


def stage_resid_linear(k, C, aT_d, nk, Wd2, x_src, x_dst):
    with Stage(k):
        wres = k.sb("wres", [128, nk, D], BF16)
        for half in range(2):
            k.dma("pool", [(wres[:, kc, half * 512:(half + 1) * 512], Wd2[kc * 128:(kc + 1) * 128, half * 512:(half + 1) * 512])
                           for kc in range(nk)], writes=[wres], sembuf=wres)
        abr = Ring([k.sb("ab%d" % i, [128, nk, 512], BF16) for i in range(2)])
        xr = Ring([k.sb("ox%d" % i, [128, D], F32) for i in range(3)])
        for tb in range(NTOK // 512):
            ab = abr.next()
            k.dma("sp", [(ab[:, :, :], aT_d[:, tb * 512:(tb + 1) * 512].rearrange("(kc p) t -> p kc t", p=128))],
                  writes=[ab], sembuf=ab)
            for t4 in range(4):
                tt = tb * 4 + t4
                xt = xr.next()
                k.dma("sp", [(xt[:, :], x_src[tt * 128:(tt + 1) * 128, :])], writes=[xt], sembuf=xt)
                for cb in range(2):
                    ps = psf(C)
                    for kc in range(nk):
                        k.op("pe", lambda e, ps=ps, ab=ab, kc=kc, t4=t4, cb=cb: e.matmul(
                            ps[:, :], ab[:, kc, t4 * 128:(t4 + 1) * 128], wres[:, kc, cb * 512:(cb + 1) * 512],
                            start=(kc == 0), stop=(kc == nk - 1)), reads=[ab, wres], writes=[ps])
                    k.op("dve", lambda e, ps=ps, xt=xt, cb=cb: e.tensor_tensor(
                        xt[:, cb * 512:(cb + 1) * 512], xt[:, cb * 512:(cb + 1) * 512], ps[:, :], op=ALU.add),
                        reads=[xt, ps], writes=[xt])
                k.dma("act", [(x_dst[tt * 128:(tt + 1) * 128, :], xt[:, :])], reads=[xt], sembuf=xt)


def stage_ffn_a(k, C, x_src, gain_ap, Wg2, Wu2, convw2, convb1, actT_d):
    with Stage(k):
        hT = [k.sb("hT%d" % i, [128, 8, 512], BF16) for i in range(NTOK // 512)]
        norm_to_hT(k, C, x_src, 0, NTOK, gain_ap, hT)
        cw = k.sb("cw", [128, 22, 3], F32)
        cbias = k.sb("cbias", [128, 22], F32)
        load_cols(k, C, lambda c: cw[:, c, :], convw2, 3, 22)
        load_cols(k, C, lambda c: cbias[:, c:c + 1], convb1.rearrange("(o c) -> o c", o=1), 1, 22)
        k.barrier()
        wgr = Ring([k.sb("wg%d" % i, [128, 8, 512], BF16) for i in range(2)])
        wur = Ring([k.sb("wu%d" % i, [128, 8, 512], BF16) for i in range(2)])
        grr = Ring([k.sb("graw%d" % i, [128, NSEQ, 2 + S], BF16) for i in range(2)])
        for g in grr.bufs:
            k.op("pool", lambda e, g=g: e.memset(g[:, :, 0:2], 0.0), writes=[g])
        dgr = Ring([k.sb("dg%d" % i, [128, 3, 128], BF16) for i in range(2)])
        sgr = Ring([k.sb("sg%d" % i, [128, 512], F32) for i in range(2)])
        str_ = Ring([k.sb("fst%d" % i, [128, 512], BF16) for i in range(3)])
        for cb512 in range(0, DFF, 512):
            n = min(512, DFF - cb512)
            wg = wgr.next(); wu = wur.next()
            load_w(k, Wg2, 0, 8, cb512, n, wg)
            load_w(k, Wu2, 0, 8, cb512, n, wu)
            for cc in range(0, n, 128):
                c = (cb512 + cc) // 128
                g = grr.next(); dg = dgr.next()
                for tap in range(3):
                    k.op("pool", lambda e, dg=dg, tap=tap, c=c: e.tensor_scalar(dg[:, tap, :], C.identb[:, :], cw[:, c, tap:tap + 1], None, op0=ALU.mult),
                         reads=[C.identb, cw], writes=[dg])
                for tb in range(8):
                    ps = psf(C)
                    for kc in range(8):
                        k.op("pe", lambda e, ps=ps, wg=wg, kc=kc, cc=cc, tb=tb: e.matmul(
                            ps[:, :], wg[:, kc, cc:cc + 128], hT[tb][:, kc, :], start=(kc == 0), stop=(kc == 7)),
                            reads=[wg, hT[tb]], writes=[ps])
                    sq, t4 = tb // 4, tb % 4
                    copy_op(k, evac_eng(C), g[:, sq, 2 + t4 * 512: 2 + (t4 + 1) * 512], ps[:, :], [ps], [g])
                for tb in range(8):
                    sq, t4 = tb // 4, tb % 4
                    pu = psf(C)
                    for kc in range(8):
                        k.op("pe", lambda e, pu=pu, wu=wu, kc=kc, cc=cc, tb=tb: e.matmul(
                            pu[:, :], wu[:, kc, cc:cc + 128], hT[tb][:, kc, :], start=(kc == 0), stop=(kc == 7)),
                            reads=[wu, hT[tb]], writes=[pu])
                    pc = psf(C)
                    for tap in range(3):
                        k.op("pe", lambda e, pc=pc, dg=dg, tap=tap, g=g, sq=sq, t4=t4: e.matmul(
                            pc[:, :], dg[:, tap, :], g[:, sq, t4 * 512 + tap: t4 * 512 + tap + 512], start=(tap == 0), stop=(tap == 2)),
                            reads=[dg, g], writes=[pc])
                    sg = sgr.next(); st = str_.next()
                    k.op("act", lambda e, sg=sg, pc=pc, c=c: e.activation(sg[:, :], pc[:, :], AF.Silu, bias=cbias[:, c:c + 1], scale=1.0),
                         reads=[pc, cbias], writes=[sg])
                    k.op("dve", lambda e, st=st, sg=sg, pu=pu: e.tensor_tensor(st[:, :], sg[:, :], pu[:, :], op=ALU.mult),
                         reads=[sg, pu], writes=[st])
                    k.dma("sp", [(actT_d[c * 128:(c + 1) * 128, tb * 512:(tb + 1) * 512], st[:, :])], reads=[st], sembuf=st)


def stage_ple(k, C, x_src, gain_ap, Wgate2, Wproj2, p_d, x_dst):
    with Stage(k):
        hT = [k.sb("hT%d" % i, [128, 8, 512], BF16) for i in range(NTOK // 512)]
        norm_to_hT(k, C, x_src, 0, NTOK, gain_ap, hT)
        pT = k.sb("pT", [128, 2, NTOK], BF16)
        pr = Ring([k.sb("pl%d" % i, [128, 256], F32) for i in range(2)])
        pbr = Ring([k.sb("plb%d" % i, [128, 256], BF16) for i in range(2)])
        for tt in range(NTOK // 128):
            pt_ = pr.next(); pb_ = pbr.next()
            k.dma("sp", [(pt_[:, :], p_d[tt * 128:(tt + 1) * 128, :])], writes=[pt_], sembuf=pt_)
            k.op("pool", lambda e, pt_=pt_, pb_=pb_: e.tensor_copy(pb_[:, :], pt_[:, :]), reads=[pt_], writes=[pb_])
            pp = psb(C)
            for j in range(2):
                k.op("pe", lambda e, pp=pp, pb_=pb_, j=j: e.transpose(pp[:, j * 128:(j + 1) * 128], pb_[:, j * 128:(j + 1) * 128], C.identb[:, :]),
                     reads=[pb_, C.identb], writes=[pp])
            copy_op(k, evac_eng(C), pT[:, :, tt * 128:(tt + 1) * 128], pp[:, 0:256].rearrange("p (a b) -> p a b", a=2), [pp], [pT])
        wgr = Ring([k.sb("wpg%d" % i, [128, 8, 512], BF16) for i in range(2)])
        wpr = Ring([k.sb("wpp%d" % i, [128, 2, 512], BF16) for i in range(2)])
        xr = Ring([k.sb("px%d" % i, [128, 512], F32) for i in range(3)])
        sgr = Ring([k.sb("psg%d" % i, [128, 512], F32) for i in range(2)])
        for cb in range(2):
            wg = wgr.next(); wp = wpr.next()
            load_w(k, Wgate2, 0, 8, cb * 512, 512, wg)
            load_w(k, Wproj2, 0, 2, cb * 512, 512, wp)
            for tt in range(NTOK // 128):
                xt = xr.next(); sg = sgr.next()
                k.dma("sp", [(xt[:, :], x_src[tt * 128:(tt + 1) * 128, cb * 512:(cb + 1) * 512])], writes=[xt], sembuf=xt)
                p1 = psf(C)
                for kc in range(8):
                    k.op("pe", lambda e, p1=p1, wg=wg, kc=kc, tt=tt: e.matmul(
                        p1[:, :], hT[tt // 4][:, kc, (tt % 4) * 128:(tt % 4 + 1) * 128], wg[:, kc, :], start=(kc == 0), stop=(kc == 7)),
                        reads=[hT[tt // 4], wg], writes=[p1])
                p2 = psf(C)
                for kc in range(2):
                    k.op("pe", lambda e, p2=p2, wp=wp, kc=kc, tt=tt: e.matmul(
                        p2[:, :], pT[:, kc, tt * 128:(tt + 1) * 128], wp[:, kc, :], start=(kc == 0), stop=(kc == 1)),
                        reads=[pT, wp], writes=[p2])
                k.op("act", lambda e, sg=sg, p1=p1: e.activation(sg[:, :], p1[:, :], AF.Sigmoid), reads=[p1], writes=[sg])
                k.op("dve", lambda e, sg=sg, p2=p2: e.tensor_tensor(sg[:, :], sg[:, :], p2[:, :], op=ALU.mult), reads=[sg, p2], writes=[sg])
                k.op("pool", lambda e, sg=sg, xt=xt: e.tensor_tensor(xt[:, :], xt[:, :], sg[:, :], op=ALU.add), reads=[sg, xt], writes=[xt])
                k.dma("sp", [(x_dst[tt * 128:(tt + 1) * 128, cb * 512:(cb + 1) * 512], xt[:, :])], reads=[xt], sembuf=xt)


def stage_inproj0(k, C, x_src, gain_ap, Wd2, scr):
    with Stage(k):
        hT = [k.sb("hT%d" % i, [128, 8, 512], BF16) for i in range(NTOK // 512)]
        norm_to_hT(k, C, x_src, 0, NTOK, gain_ap, hT)
        wring = Ring([k.sb("w%d" % i, [128, 8, 512], BF16) for i in range(3)])
        stb = Ring([k.sb("stb%d" % i, [128, 512], BF16) for i in range(4)])
        stf = Ring([k.sb("stf%d" % i, [128, 512], F32) for i in range(3)])
        rhs_fn = lambda kc, tb: (hT[tb], hT[tb][:, kc, :])
        lhs_fn = lambda kc, tt: (hT[tt // 4], hT[tt // 4][:, kc, (tt % 4) * 128:(tt % 4 + 1) * 128])

        def ev_fm(dst, r0):
            def ev(c, m, tb, ps):
                st = stb.next()
                copy_op(k, evac_eng(C), st[0:m, :], ps[0:m, :], [ps], [st])
                k.dma("sp", [(dst[r0 + c:r0 + c + m, tb * 512:(tb + 1) * 512], st[0:m, :])], reads=[st], sembuf=st)
            return ev

        def ev_tm(dst, c0, ring):
            def ev(cb, n, tt, ps):
                st = ring.next()
                copy_op(k, evac_eng(C), st[:, 0:n], ps[:, 0:n], [ps], [st])
                k.dma("sp", [(dst[tt * 128:(tt + 1) * 128, c0 + cb:c0 + cb + n], st[:, 0:n])], reads=[st], sembuf=st)
            return ev

        linear_fm(k, C, rhs_fn, 8, Wd2, 0, 0, 1024, NTOK // 512, ev_fm(scr.qkT, 0), wring)
        linear_tm(k, C, lhs_fn, 8, Wd2, 0, 1024, 512, NTOK // 128, ev_tm(scr.v, 0, stb), wring)
        linear_tm(k, C, lhs_fn, 8, Wd2, 0, 1536, 1024, NTOK // 128, ev_tm(scr.z, 0, stf), wring)
        linear_fm(k, C, rhs_fn, 8, Wd2, 0, 2560, 1536, NTOK // 512, ev_fm(scr.xbcT, 0), wring)
        linear_tm(k, C, lhs_fn, 8, Wd2, 0, 4096, 16, NTOK // 128, ev_tm(scr.dt, 0, stf), wring)


def load_col(k, dst, ap1, n, reps, scale=None):
    for r in range(reps):
        k.dma("sp", [(dst[r * n:(r + 1) * n, 0:1], ap1.rearrange("(p o) -> p o", o=1))], writes=[dst], sembuf=dst)
    if scale is not None:
        k.op("dve", lambda e: e.tensor_scalar(dst[:, 0:1], dst[:, 0:1], scale, None, op0=ALU.mult), reads=[dst], writes=[dst])


def stage_attn(k, C, scr, gq_ap, gk_ap, etab_d, auxc_d):
    with Stage(k):
        C.pfn = 4
        o_ps, d_ps = C.pf[4], C.pf[5]
        E = k.sb("E", [68, 8 * 16 * 128], BF16)
        for i in range(8):
            k.dma("pool", [(E[:, i * 2048:(i + 1) * 2048], etab_d[:, i * 2048:(i + 1) * 2048])], writes=[E], sembuf=E)
        gq = k.sb("gq", [128, 1], F32)
        gk = k.sb("gk", [128, 1], F32)
        load_col(k, gq, gq_ap, 64, 2, scale=0.125)
        load_col(k, gk, gk_ap, 64, 2)
        auxT = k.sb("auxT", [68, S], BF16)
        k.op("pool", lambda e: e.memset(auxT[0:64, :], 0.0), writes=[auxT])
        k.dma("pool", [(auxT[64:68, :], auxc_d[:, :])], writes=[auxT], sembuf=auxT)
        qn = [k.sb("qn%d" % c, [128, S], BF16) for c in range(4)]
        kn = [k.sb("kn%d" % c, [128, S], BF16) for c in range(4)]
        rawr = Ring([k.sb("araw%d" % i, [128, S], BF16) for i in range(2)])
        sqr = Ring([k.sb("asq%d" % i, [128, S], BF16) for i in range(2)])
        rsr = Ring([k.sb("ars%d" % i, [128, 512], F32) for i in range(2)])
        kb32 = k.sb("kb32", [128, 4, 8], F32)
        kbhi = k.sb("kbhi", [128, 4, 8], BF16)
        kblo = k.sb("kblo", [128, 4, 8], BF16)
        kbl32 = k.sb("kbl32", [128, 4, 8], F32)
        gs = k.sb("gs", [128, 8, 8], F32)
        cmp_ = k.sb("cmp", [128, 8, 8, 8], F32)
        cnt = k.sb("cnt", [128, 8, 8], F32)
        mbr = Ring([k.sb("mb%d" % i, [128, 64], BF16) for i in range(2)])
        v_sb = k.sb("v_sb", [128, 16, 512], BF16)
        ptr = Ring([k.sb("pt%d" % i, [128, 512], BF16) for i in range(3)])
        rec = k.sb("rec", [64, 512], F32)
        osb = Ring([k.sb("osb%d" % i, [64, 512], BF16) for i in range(2)])
        for s in range(NSEQ):
            t0 = s * S
            for c8 in range(8):
                isq = c8 < 4
                dst = qn[c8] if isq else kn[c8 - 4]
                gcol = gq if isq else gk
                raw = rawr.next(); sq = sqr.next()
                k.dma("sp", [(raw[:, :], scr.qkT[c8 * 128:(c8 + 1) * 128, t0:t0 + S])], writes=[raw], sembuf=raw)
                k.op("act", lambda e, sq=sq, raw=raw: e.activation(sq[:, :], raw[:, :], AF.Square), reads=[raw], writes=[sq])
                for tb in range(4):
                    ps = psf(C); rs = rsr.next()
                    k.op("pe", lambda e, ps=ps, sq=sq, tb=tb: e.matmul(ps[:, :], C.blk1[:, :], sq[:, tb * 512:(tb + 1) * 512], start=True, stop=True),
                         reads=[C.blk1, sq], writes=[ps])
                    k.op("act", lambda e, ps=ps, rs=rs: e.activation(rs[:, :], ps[:, :], AF.Sqrt, bias=EPS, scale=1.0 / 64), reads=[ps], writes=[rs])
                    k.op("dve", lambda e, rs=rs: e.reciprocal(rs[:, :], rs[:, :]), reads=[rs], writes=[rs])
                    k.op("dve", lambda e, dst=dst, raw=raw, gcol=gcol, rs=rs, tb=tb: e.scalar_tensor_tensor(
                        dst[:, tb * 512:(tb + 1) * 512], raw[:, tb * 512:(tb + 1) * 512], gcol[:, 0:1], rs[:, :], op0=ALU.mult, op1=ALU.mult),
                        reads=[raw, gcol, rs], writes=[dst])
            for c in range(4):
                k.op("dve", lambda e, c=c: e.tensor_reduce(kb32[:, c, :], kn[c][:, :].rearrange("p (n j) -> p n j", j=256), axis=AX.X, op=ALU.add),
                     reads=[kn[c]], writes=[kb32])
            k.op("dve", lambda e: e.tensor_scalar(kb32[:, :, :], kb32[:, :, :], 1.0 / 256, None, op0=ALU.mult), reads=[kb32], writes=[kb32])
            k.op("dve", lambda e: e.tensor_copy(kbhi[:, :, :], kb32[:, :, :]), reads=[kb32], writes=[kbhi])
            k.op("dve", lambda e: e.tensor_tensor(kbl32[:, :, :], kb32[:, :, :], kbhi[:, :, :], op=ALU.subtract), reads=[kb32, kbhi], writes=[kbl32])
            k.op("dve", lambda e: e.tensor_copy(kblo[:, :, :], kbl32[:, :, :]), reads=[kbl32], writes=[kblo])
            for qt in range(8, 16):
                nb = qt // 2
                for par in range(2):
                    pg = psf(C)
                    pb_ = par * 64
                    for i4 in range(4):
                        c = i4
                        k.op("pe", lambda e, pg=pg, i4=i4, c=c, pb_=pb_, qt=qt: e.matmul(
                            pg[:, i4 * 8:(i4 + 1) * 8], qn[c][pb_:pb_ + 64, qt * 128:(qt + 1) * 128], kbhi[pb_:pb_ + 64, c, :], start=True, stop=False),
                            reads=[qn[c], kbhi], writes=[pg])
                        k.op("pe", lambda e, pg=pg, i4=i4, c=c, pb_=pb_, qt=qt: e.matmul(
                            pg[:, i4 * 8:(i4 + 1) * 8], qn[c][pb_:pb_ + 64, qt * 128:(qt + 1) * 128], kblo[pb_:pb_ + 64, c, :], start=False, stop=True),
                            reads=[qn[c], kblo], writes=[pg])
                    k.op("dve", lambda e, pg=pg, par=par: e.tensor_copy(gs[:, par * 4:(par + 1) * 4, :], pg[:, 0:32].rearrange("p (h n) -> p h n", h=4)), reads=[pg], writes=[gs])
                k.op("dve", lambda e, nb=nb: e.tensor_tensor(
                    cmp_[:, :, 0:nb, 0:nb], gs[:, :, 0:nb].unsqueeze(2).broadcast_to([128, 8, nb, nb]),
                    gs[:, :, 0:nb].unsqueeze(3).broadcast_to([128, 8, nb, nb]), op=ALU.is_gt), reads=[gs], writes=[cmp_])
                k.op("dve", lambda e, nb=nb: e.tensor_reduce(cnt[:, :, 0:nb], cmp_[:, :, 0:nb, 0:nb], axis=AX.X, op=ALU.add), reads=[cmp_], writes=[cnt])
                mb = mbr.next()
                k.op("pool", lambda e, mb=mb: e.memset(mb[:, :], 0.0), writes=[mb])
                k.op("dve", lambda e, mb=mb, nb=nb: e.tensor_scalar(
                    mb[:, :].rearrange("p (h n) -> p h n", h=8)[:, :, 0:nb], cnt[:, :, 0:nb], 3.0, NEG, op0=ALU.is_ge, op1=ALU.mult),
                    reads=[cnt, mb], writes=[mb])
                pp = psb(C)
                k.op("pe", lambda e, pp=pp, mb=mb: e.transpose(pp[0:64, 0:128], mb[:, :], C.identb[:, :]), reads=[mb, C.identb], writes=[pp])
                copy_op(k, evac_eng(C), auxT[0:64, qt * 128:(qt + 1) * 128], pp[0:64, 0:128], [pp], [auxT])
            if getattr(C, "dbg_aux", None) is not None and s == 0:
                k.dma("sp", [(C.dbg_aux[:, :], auxT[:, :])], reads=[auxT], sembuf=auxT)
            k.dma("sp", [(v_sb[:, :, :], scr.v[t0:t0 + S, :].rearrange("(kt p) c -> p kt c", p=128))], writes=[v_sb], sembuf=v_sb)
            def emit_pv(pt, kt, h, j0, n, nkt):
                k.op("pe", lambda e: e.matmul(o_ps[0:64, j0:512], v_sb[:, kt, h * 64:(h + 1) * 64], pt[:, 0:n], start=(kt == 0), stop=(kt == nkt - 1)),
                     reads=[v_sb, pt], writes=[o_ps])
                k.op("pe", lambda e: e.matmul(d_ps[0:64, j0:512], C.onesb[:, 0:64], pt[:, 0:n], start=(kt == 0), stop=(kt == nkt - 1)),
                     reads=[C.onesb, pt], writes=[d_ps])

            pend = None
            for h in range(8):
                c, pb_ = h // 2, (h % 2) * 64
                for qc in range(4):
                    nkt = 4 * qc + 4
                    for kt in range(nkt):
                        j0 = max(0, kt - 4 * qc) * 128
                        n = 512 - j0
                        q0 = qc * 512 + j0
                        ps = psf(C)
                        diag = kt >= 4 * qc
                        k.op("pe", lambda e, ps=ps, c=c, pb_=pb_, kt=kt, q0=q0, n=n: e.matmul(
                            ps[:, 0:n], kn[c][pb_:pb_ + 64, kt * 128:(kt + 1) * 128], qn[c][pb_:pb_ + 64, q0:q0 + n], start=True, stop=False),
                            reads=[kn[c], qn[c]], writes=[ps])
                        if diag:
                            k.op("pe", lambda e, ps=ps: e.matmul(ps[:, 0:128], C.identb[:, :], C.tribias[:, :], start=False, stop=False),
                                 reads=[C.identb, C.tribias], writes=[ps])
                        eo = (h * 16 + kt) * 128
                        k.op("pe", lambda e, ps=ps, eo=eo, q0=q0, n=n: e.matmul(
                            ps[:, 0:n], E[0:68, eo:eo + 128], auxT[0:68, q0:q0 + n], start=False, stop=True),
                            reads=[E, auxT], writes=[ps])
                        pt = ptr.next()
                        k.op("act", lambda e, pt=pt, ps=ps, n=n: e.activation(pt[:, 0:n], ps[:, 0:n], AF.Exp), reads=[ps], writes=[pt])
                        if pend is not None:
                            emit_pv(*pend)
                        pend = (pt, kt, h, j0, n, nkt)
                    emit_pv(*pend)
                    pend = None
                    ob = osb.next()
                    k.op("dve", lambda e: e.reciprocal(rec[:, :], d_ps[0:64, :]), reads=[d_ps], writes=[rec])
                    k.op("dve", lambda e, ob=ob: e.tensor_tensor(ob[:, :], o_ps[0:64, :], rec[:, :], op=ALU.mult), reads=[o_ps, rec], writes=[ob])
                    k.dma("sp", [(scr.mixT[h * 64:(h + 1) * 64, t0 + qc * 512:t0 + (qc + 1) * 512], ob[:, :])], reads=[ob], sembuf=ob)
        C.pfn = 6


def bc_row(k, name, ap1, n):
    t = k.sb(name, [128, n], F32)
    k.dma("sp", [(t[:, :], ap1.partition_broadcast(128))], writes=[t], sembuf=t)
    return t


def stage_ssd(k, C, scr, Wt):
    V = lambda fn, r, w: k.op("dve", fn, reads=r, writes=w)
    A = lambda fn, r, w: k.op("act", fn, reads=r, writes=w)
    G = lambda fn, r, w: k.op("pool", fn, reads=r, writes=w)
    P = lambda fn, r, w: k.op("pe", fn, reads=r, writes=w)
    with Stage(k):
        C.pfn = 2
        yd = [C.pf[2], C.pf[3]]
        yo = [C.pf[4], C.pf[5]]
        cw = k.sb("scw", [128, 12, 4], F32)
        cbias = k.sb("scb", [128, 12], F32)
        convw2 = Wt["ssm_conv_w"][0]
        load_cols(k, C, lambda c: cw[:, c, :], convw2, 4, 12)
        load_cols(k, C, lambda c: cbias[:, c:c + 1], Wt["ssm_conv_b"][0].rearrange("(o c) -> o c", o=1), 1, 12)
        k.barrier()
        dtb_bc = bc_row(k, "dtb_bc", Wt["ssm_dt_bias"][0], 16)
        a_bc = bc_row(k, "a_bc", Wt["ssm_a_log"][0], 16)
        A(lambda e: e.activation(a_bc[:, :], a_bc[:, :], AF.Exp), [a_bc], [a_bc])
        V(lambda e: e.tensor_scalar(a_bc[:, :], a_bc[:, :], -1.0, None, op0=ALU.mult), [a_bc], [a_bc])
        d_bc = bc_row(k, "d_bc", Wt["ssm_d"][0], 16)
        nw_bc = bc_row(k, "nw_bc", Wt["ssm_norm"][0], 1024)
        xc = [k.sb("xc%d" % i, [128, S], BF16) for i in range(12)]
        rawr = Ring([k.sb("sraw%d" % i, [128, 3 + S], BF16) for i in range(2)])
        for r_ in rawr.bufs:
            G(lambda e, r_=r_: e.memset(r_[:, 0:3], 0.0), [], [r_])
        dgr = Ring([k.sb("sdg%d" % i, [128, 4, 128], BF16) for i in range(2)])
        xs_tm = k.sb("xs_tm", [128, 16, 1024], BF16)
        B_tm = k.sb("B_tm", [128, 16, 256], BF16)
        dt_sp = k.sb("dt_sp", [128, 16, 16], F32)
        dta = k.sb("dta", [128, 16, 16], F32)
        gU = k.sb("gU", [128, 16, 128], F32)
        acs = k.sb("acs", [128, 32], F32)
        eac = k.sb("eac", [128, 16], F32)
        cdec = k.sb("cdec", [128, 16], F32)
        wst = k.sb("wst", [128, 16], F32)
        t1r = Ring([k.sb("st1%d" % i, [128, 512], F32) for i in range(2)])
        t2r = Ring([k.sb("st2%d" % i, [128, 512], F32) for i in range(2)])
        mtr = Ring([k.sb("smt%d" % i, [128, 512], BF16) for i in range(2)])
        cbm = [k.sb("cbm%d" % g, [128, 128], F32) for g in range(2)]
        xdtr = Ring([k.sb("xdt%d" % i, [128, 1024], BF16) for i in range(2)])
        xdtwr = Ring([k.sb("xdtw%d" % i, [128, 1024], BF16) for i in range(2)])
        yr = Ring([k.sb("sy%d" % i, [128, 1024], F32) for i in range(2)])
        tmp2 = k.sb("stmp2", [128, 1024], F32)
        zr = Ring([k.sb("sz%d" % i, [128, 1024], F32) for i in range(2)])
        junk = k.sb("sjunk", [128, 512], BF16)
        ss2 = k.sb("sss2", [128, 4], F32)
        obr = Ring([k.sb("sob%d" % i, [128, 1024], BF16) for i in range(2)])
        obTr = Ring([k.sb("sobT%d" % i, [128, 8, 128], BF16) for i in range(2)])
        prev32 = [k.sb("prev32_%d" % g, [128, 512], F32) for g in range(2)]
        prevb = [k.sb("prevb_%d" % g, [128, 512], BF16) for g in range(2)]
        for s in range(NSEQ):
            t0 = s * S
            for c in range(12):
                raw = rawr.next(); dg = dgr.next()
                k.dma("sp", [(raw[:, 3:3 + S], scr.xbcT[c * 128:(c + 1) * 128, t0:t0 + S])], writes=[raw], sembuf=raw)
                for tap in range(4):
                    G(lambda e, dg=dg, tap=tap, c=c: e.tensor_scalar(dg[:, tap, :], C.identb[:, :], cw[:, c, tap:tap + 1], None, op0=ALU.mult),
                      [C.identb, cw], [dg])
                for tb in range(4):
                    ps = psf(C)
                    for tap in range(4):
                        P(lambda e, ps=ps, dg=dg, tap=tap, raw=raw, tb=tb: e.matmul(
                            ps[:, :], dg[:, tap, :], raw[:, tb * 512 + tap: tb * 512 + tap + 512], start=(tap == 0), stop=(tap == 3)), [dg, raw], [ps])
                    A(lambda e, ps=ps, c=c, tb=tb: e.activation(xc[c][:, tb * 512:(tb + 1) * 512], ps[:, :], AF.Silu, bias=cbias[:, c:c + 1], scale=1.0),
                      [ps, cbias], [xc[c]])
            for kt in range(16):
                pp = psb(C)
                for c in range(8):
                    P(lambda e, pp=pp, c=c, kt=kt: e.transpose(pp[:, c * 128:(c + 1) * 128], xc[c][:, kt * 128:(kt + 1) * 128], C.identb[:, :]),
                      [xc[c], C.identb], [pp])
                copy_op(k, evac_eng(C), xs_tm[:, kt, :], pp[:, :], [pp], [xs_tm])
                pp = psb(C)
                for g in range(2):
                    P(lambda e, pp=pp, g=g, kt=kt: e.transpose(pp[:, g * 128:(g + 1) * 128], xc[8 + g][:, kt * 128:(kt + 1) * 128], C.identb[:, :]),
                      [xc[8 + g], C.identb], [pp])
                copy_op(k, evac_eng(C), B_tm[:, kt, :], pp[:, 0:256], [pp], [B_tm])
            k.dma("sp", [(dt_sp[:, :, :], scr.dt[t0:t0 + S, :].rearrange("(kt p) h -> p kt h", p=128))], writes=[dt_sp], sembuf=dt_sp)
            V(lambda e: e.tensor_tensor(dt_sp[:, :, :], dt_sp[:, :, :], dtb_bc[:, :].unsqueeze(1).broadcast_to([128, 16, 16]), op=ALU.add),
              [dt_sp, dtb_bc], [dt_sp])
            A(lambda e: e.activation(dt_sp[:, :, :], dt_sp[:, :, :], AF.Exp), [dt_sp], [dt_sp])
            A(lambda e: e.activation(dt_sp[:, :, :], dt_sp[:, :, :], AF.Ln, bias=1.0, scale=1.0), [dt_sp], [dt_sp])
            V(lambda e: e.tensor_tensor(dta[:, :, :], dt_sp[:, :, :], a_bc[:, :].unsqueeze(1).broadcast_to([128, 16, 16]), op=ALU.mult),
              [dt_sp, a_bc], [dta])
            for g in range(2):
                G(lambda e, g=g: e.memset(prev32[g][:, :], 0.0), [], [prev32[g]])
                G(lambda e, g=g: e.memset(prevb[g][:, :], 0.0), [], [prevb[g]])
            for c in range(16):
                xdt = xdtr.next(); xdtw = xdtwr.next(); y = yr.next(); zt = zr.next(); ob = obr.next(); obT = obTr.next()
                k.dma("sp", [(zt[:, :], scr.z[t0 + c * 128:t0 + (c + 1) * 128, :])], writes=[zt], sembuf=zt)
                ps = psf(C)
                P(lambda e, ps=ps, c=c: e.matmul(ps[:, 0:16], C.U32[:, :], dta[:, c, :], start=True, stop=True), [C.U32, dta], [ps])
                P(lambda e, ps=ps, c=c: e.matmul(ps[:, 16:32], C.ones32[:, :], dta[:, c, :], start=True, stop=True), [C.ones32, dta], [ps])
                V(lambda e, ps=ps: e.tensor_copy(acs[:, :], ps[:, 0:32]), [ps], [acs])
                A(lambda e: e.activation(eac[:, :], acs[:, 0:16], AF.Exp), [acs], [eac])
                A(lambda e: e.activation(cdec[:, :], acs[:, 16:32], AF.Exp), [acs], [cdec])
                V(lambda e: e.tensor_tensor(wst[:, :], acs[:, 16:32], acs[:, 0:16], op=ALU.subtract), [acs], [wst])
                A(lambda e: e.activation(wst[:, :], wst[:, :], AF.Exp), [wst], [wst])
                V(lambda e, c=c: e.tensor_tensor(gU[:, :, :], C.U32[:, :].unsqueeze(1).broadcast_to([128, 16, 128]),
                                                 dta[:, c, :].unsqueeze(2).broadcast_to([128, 16, 128]), op=ALU.mult), [C.U32, dta], [gU])
                V(lambda e, xdt=xdt, c=c: e.tensor_tensor(xdt[:, :].rearrange("p (h d) -> p h d", h=16), xs_tm[:, c, :].rearrange("p (h d) -> p h d", h=16),
                                                          dt_sp[:, c, :].unsqueeze(2).broadcast_to([128, 16, 64]), op=ALU.mult), [xs_tm, dt_sp], [xdt])
                G(lambda e, xdt=xdt, xdtw=xdtw: e.tensor_tensor(xdtw[:, :].rearrange("p (h d) -> p h d", h=16), xdt[:, :].rearrange("p (h d) -> p h d", h=16),
                                                                wst[:, :].unsqueeze(2).broadcast_to([128, 16, 64]), op=ALU.mult), [xdt, wst], [xdtw])
                for g in range(2):
                    ps = psf(C)
                    P(lambda e, ps=ps, g=g, c=c: e.matmul(ps[:, 0:128], xc[8 + g][:, c * 128:(c + 1) * 128], xc[10 + g][:, c * 128:(c + 1) * 128], start=True, stop=True),
                      [xc[8 + g], xc[10 + g]], [ps])
                    V(lambda e, ps=ps, g=g: e.tensor_tensor(cbm[g][:, :], ps[:, 0:128], C.U32[:, :], op=ALU.mult), [ps, C.U32], [cbm[g]])
                    P(lambda e, g=g, c=c: e.matmul(yo[g][:, :], xc[10 + g][:, c * 128:(c + 1) * 128], prevb[g][:, :], start=True, stop=True),
                      [xc[10 + g], prevb[g]], [yo[g]])
                for hg in range(4):
                    g = hg // 2
                    R = psf(C); t1 = t1r.next(); t2 = t2r.next(); mt = mtr.next()
                    P(lambda e, R=R, hg=hg: e.matmul(R[:, :], C.ones32[:, :], gU[:, hg * 4:(hg + 1) * 4, :].rearrange("p a b -> p (a b)"), start=True, stop=True),
                      [C.ones32, gU], [R])
                    V(lambda e, R=R, t1=t1, hg=hg: e.tensor_tensor(t1[:, :].rearrange("p (a b) -> p a b", a=4), R[:, :].rearrange("p (a b) -> p a b", a=4),
                                                                  acs[:, hg * 4:(hg + 1) * 4].unsqueeze(2).broadcast_to([128, 4, 128]), op=ALU.subtract), [R, acs], [t1])
                    V(lambda e, t1=t1: e.tensor_scalar(t1[:, :], t1[:, :], 0.0, None, op0=ALU.min), [t1], [t1])
                    A(lambda e, t1=t1, t2=t2: e.activation(t2[:, :], t1[:, :], AF.Exp), [t1], [t2])
                    V(lambda e, t2=t2, mt=mt, g=g: e.tensor_tensor(mt[:, :].rearrange("p (a b) -> p a b", a=4), t2[:, :].rearrange("p (a b) -> p a b", a=4),
                                                                  cbm[g][:, :].unsqueeze(1).broadcast_to([128, 4, 128]), op=ALU.mult), [t2, cbm[g]], [mt])
                    for hh in range(4):
                        h = hg * 4 + hh
                        P(lambda e, mt=mt, hh=hh, h=h, g=g, xdt=xdt: e.matmul(yd[g][:, (h % 8) * 64:(h % 8 + 1) * 64], mt[:, hh * 128:(hh + 1) * 128],
                                                                             xdt[:, h * 64:(h + 1) * 64], start=True, stop=True), [mt, xdt], [yd[g]])
                for g in range(2):
                    ysl = y[:, g * 512:(g + 1) * 512]
                    V(lambda e, ysl=ysl, g=g: e.tensor_tensor(ysl.rearrange("p (h d) -> p h d", h=8), yo[g][:, :].rearrange("p (h d) -> p h d", h=8),
                                                             eac[:, g * 8:(g + 1) * 8].unsqueeze(2).broadcast_to([128, 8, 64]), op=ALU.mult), [yo[g], eac], [y])
                    V(lambda e, ysl=ysl, g=g: e.tensor_tensor(ysl, ysl, yd[g][:, :], op=ALU.add), [y, yd[g]], [y])
                G(lambda e, c=c: e.tensor_tensor(tmp2[:, :].rearrange("p (h d) -> p h d", h=16), xs_tm[:, c, :].rearrange("p (h d) -> p h d", h=16),
                                                 d_bc[:, :].unsqueeze(2).broadcast_to([128, 16, 64]), op=ALU.mult), [xs_tm, d_bc], [tmp2])
                G(lambda e, y=y: e.tensor_tensor(y[:, :], y[:, :], tmp2[:, :], op=ALU.add), [y, tmp2], [y])
                for g in range(2):
                    st = psf(C)
                    P(lambda e, st=st, g=g, c=c, xdtw=xdtw: e.matmul(st[:, :], B_tm[:, c, g * 128:(g + 1) * 128], xdtw[:, g * 512:(g + 1) * 512], start=True, stop=True),
                      [B_tm, xdtw], [st])
                    V(lambda e, g=g: e.tensor_tensor(prev32[g][:, :].rearrange("p (h d) -> p h d", h=8), prev32[g][:, :].rearrange("p (h d) -> p h d", h=8),
                                                     cdec[:, g * 8:(g + 1) * 8].unsqueeze(2).broadcast_to([128, 8, 64]), op=ALU.mult), [prev32[g], cdec], [prev32[g]])
                    V(lambda e, g=g, st=st: e.tensor_tensor(prev32[g][:, :], prev32[g][:, :], st[:, :], op=ALU.add), [prev32[g], st], [prev32[g]])
                    G(lambda e, g=g: e.tensor_copy(prevb[g][:, :], prev32[g][:, :]), [prev32[g]], [prevb[g]])
                A(lambda e, zt=zt: e.activation(zt[:, :], zt[:, :], AF.Silu), [zt], [zt])
                V(lambda e, y=y, zt=zt: e.tensor_tensor(y[:, :], y[:, :], zt[:, :], op=ALU.mult), [y, zt], [y])
                G(lambda e: e.memset(ss2[:, :], 0.0), [], [ss2])
                for g in range(2):
                    A(lambda e, y=y, g=g: e.activation(junk[:, :], y[:, g * 512:(g + 1) * 512], AF.Square, accum_out=ss2[:, g:g + 1]), [y, ss2], [junk, ss2])
                A(lambda e: e.activation(ss2[:, 2:4], ss2[:, 0:2], AF.Sqrt, bias=EPS, scale=1.0 / 512), [ss2], [ss2])
                V(lambda e: e.reciprocal(ss2[:, 2:4], ss2[:, 2:4]), [ss2], [ss2])
                for g in range(2):
                    V(lambda e, y=y, ob=ob, g=g: e.scalar_tensor_tensor(ob[:, g * 512:(g + 1) * 512], y[:, g * 512:(g + 1) * 512], ss2[:, 2 + g:3 + g],
                                                                       nw_bc[:, g * 512:(g + 1) * 512], op0=ALU.mult, op1=ALU.mult), [y, ss2, nw_bc], [ob])
                pp = psb(C)
                for j in range(8):
                    P(lambda e, pp=pp, ob=ob, j=j: e.transpose(pp[:, j * 128:(j + 1) * 128], ob[:, j * 128:(j + 1) * 128], C.identb[:, :]), [ob, C.identb], [pp])
                copy_op(k, evac_eng(C), obT[:, :, :], pp[:, :].rearrange("p (a b) -> p a b", a=8), [pp], [obT])
                k.dma("sp", [(scr.mixT[512:1536, t0 + c * 128:t0 + (c + 1) * 128].rearrange("(j p) t -> p j t", p=128), obT[:, :, :])], reads=[obT], sembuf=obT)
        C.pfn = 6


def host_consts():
    slopes = np.array([2.0 ** (-(h + 1)) for h in range(8)], dtype=np.float64)
    etab = np.zeros((68, 8, 16, 128), dtype=np.float32)
    for h in range(8):
        for kt in range(16):
            etab[((h % 2) * 4 + h // 2) * 8 + kt // 2, h, kt, :] = 1.0
            etab[64, h, kt, :] = -128.0 * slopes[h]
            etab[65, h, kt, :] = -slopes[h]
            etab[66, h, kt, :] = slopes[h] * np.arange(128)
            etab[67, h, kt, :] = 128.0 * slopes[h] * kt
    auxc = np.zeros((4, S), dtype=np.float32)
    pos = np.arange(S)
    auxc[0] = pos // 128
    auxc[1] = pos % 128
    auxc[2] = 1.0
    auxc[3] = 1.0
    return {"etab": etab.reshape(68, 8 * 16 * 128), "auxc": auxc}


def build(upto="full", dumps=()):
    nc = bass.Bass("TRN2", target_bir_lowering=False)
    root = ExitStack()
    k = K(nc, root)
    _k_init_extra(k)
    k.cur_dsems = []
    C = Ctx()
    x_d = k.dram("x", [NTOK, D], F32, kind="ExternalInput").t
    p_d = [k.dram("p%d" % i, [NTOK, 256], F32, kind="ExternalInput").t for i in range(2)]
    Wt = {n: k.dram(n, shp, F32, kind="ExternalInput").t for n, shp in WSHAPES.items()}
    etab_d = k.dram("etab", [68, 8 * 16 * 128], F32, kind="ExternalInput").t
    auxc_d = k.dram("auxc", [4, S], F32, kind="ExternalInput").t
    y_d = k.dram("y", [NTOK, D], F32, kind="ExternalOutput").t
    scr = Ctx()
    scr.qkT = k.dram("s_qkT", [1024, NTOK], BF16).t
    scr.v = k.dram("s_v", [NTOK, 512], BF16).t
    scr.z = k.dram("s_z", [NTOK, 1024], F32).t
    scr.xbcT = k.dram("s_xbcT", [1536, NTOK], BF16).t
    scr.dt = k.dram("s_dt", [NTOK, 16], F32).t
    scr.mixT = k.dram("s_mixT", [2048, NTOK], BF16).t
    scr.actT = k.dram("s_actT", [DFF, NTOK], BF16).t
    scr.qkvT = k.dram("s_qkvT", [4096, NTOK], BF16).t
    scr.zT = k.dram("s_zT", [2048, NTOK], F32).t
    scr.ba = k.dram("s_ba", [NTOK, 32], F32).t
    xa = k.dram("s_xa", [NTOK, D], F32).t
    xb = k.dram("s_xb", [NTOK, D], F32).t
    mk_consts(k, C)
    k.barrier()
    if "auxT" in dumps:
        C.dbg_aux = k.dram("dbg_auxT", [68, S], BF16, kind="ExternalOutput").t
        dumps = [d_ for d_ in dumps if d_ != "auxT"]
    order = ["in0", "attn", "ssd", "x1", "x2", "x3", "in1", "gdn", "x4", "x5", "full"]
    lim = order.index(upto)
    last = x_d

    def done(name):
        return order.index(name) > lim

    stage_inproj0(k, C, x_d, Wt["norm_mix"][0], Wt["w_in_even"][0], scr)
    if not done("attn"):
        stage_attn(k, C, scr, Wt["moba_q_norm"][0], Wt["moba_k_norm"][0], etab_d, auxc_d)
    if not done("ssd"):
        stage_ssd(k, C, scr, Wt)
    if not done("x1"):
        stage_resid_linear(k, C, scr.mixT[0:1536, :], 12, Wt["w_out_even"][0], x_d, xa)
        last = xa
    if not done("x2"):
        stage_ffn_a(k, C, xa, Wt["norm_ffn"][0], Wt["ffn_w_gate"][0], Wt["ffn_w_up"][0], Wt["ffn_conv_w"][0], Wt["ffn_conv_b"][0], scr.actT)
        stage_resid_linear(k, C, scr.actT, 22, Wt["ffn_w_down"][0], xa, xb)
        last = xb
    if not done("x3"):
        stage_ple(k, C, xb, Wt["norm_ple"][0], Wt["ple_w_gate"][0], Wt["ple_w_proj"][0], p_d[0], xa)
        last = xa
    if not done("in1"):
        stage_inproj1(k, C, xa, Wt["norm_mix"][1], Wt["w_in_odd"][0], scr)
    if not done("gdn"):
        stage_gdn(k, C, scr, Wt)
    if not done("x4"):
        stage_resid_linear(k, C, scr.mixT, 16, Wt["w_out_odd"][0], xa, xb)
        last = xb
    if not done("x5"):
        stage_ffn_a(k, C, xb, Wt["norm_ffn"][1], Wt["ffn_w_gate"][1], Wt["ffn_w_up"][1], Wt["ffn_conv_w"][1], Wt["ffn_conv_b"][1], scr.actT)
        stage_resid_linear(k, C, scr.actT, 22, Wt["ffn_w_down"][1], xb, xa)
        last = xa
    if not done("full"):
        stage_ple(k, C, xa, Wt["norm_ple"][1], Wt["ple_w_gate"][1], Wt["ple_w_proj"][1], p_d[1], xb)
        last = xb
    with Stage(k):
        cp = k.sb("cpbuf", [128, 4, D], F32)
        for i in range(NTOK // 512):
            k.dma("sp", [(cp[:, :, :], last[i * 512:(i + 1) * 512, :].rearrange("(a p) d -> p a d", p=128))], writes=[cp], sembuf=cp)
            k.dma("sp", [(y_d[i * 512:(i + 1) * 512, :].rearrange("(a p) d -> p a d", p=128), cp[:, :, :])], reads=[cp], sembuf=cp)
        for name in dumps:
            src = getattr(scr, name)
            shp = list(src.shape)
            dd = k.dram("dbg_" + name, shp, src.dtype, kind="ExternalOutput").t
            rows = shp[0]
            cb2 = k.sb("cpb_" + name, [128, shp[1]], src.dtype)
            for i in range(rows // 128):
                k.dma("sp", [(cb2[:, :], src[i * 128:(i + 1) * 128, :])], writes=[cb2], sembuf=cb2)
                k.dma("sp", [(dd[i * 128:(i + 1) * 128, :], cb2[:, :])], reads=[cb2], sembuf=cb2)
    k.finish([])
    root.close()
    return nc


_NC_CACHE = {}


def kernel(**inputs):
    n = 8
    hc = host_consts()
    x = np.ascontiguousarray(inputs["x"], dtype=np.float32).reshape(n, NTOK, D)
    p = np.ascontiguousarray(inputs["p"], dtype=np.float32)
    in_maps = []
    for c in range(n):
        m = {"x": x[c], "p0": np.ascontiguousarray(p[0, 2 * c:2 * c + 2].reshape(NTOK, 256)),
             "p1": np.ascontiguousarray(p[1, 2 * c:2 * c + 2].reshape(NTOK, 256)),
             "etab": hc["etab"], "auxc": hc["auxc"]}
        for nme in WSHAPES:
            m[nme] = np.ascontiguousarray(inputs[nme], dtype=np.float32)
        in_maps.append(m)
    if "full" not in _NC_CACHE:
        _NC_CACHE["full"] = build("full")
    res = run_bass_kernel_spmd(_NC_CACHE["full"], in_maps, core_ids=list(range(n)))
    out = np.stack([r["y"] for r in res.results], axis=0)
    return out.reshape(16, S, D).astype(np.float32)


def stage_inproj1(k, C, x_src, gain_ap, Wd2, scr):
    with Stage(k):
        hT = [k.sb("hT%d" % i, [128, 8, 512], BF16) for i in range(NTOK // 512)]
        norm_to_hT(k, C, x_src, 0, NTOK, gain_ap, hT)
        wring = Ring([k.sb("w%d" % i, [128, 8, 512], BF16) for i in range(3)])
        stb = Ring([k.sb("stb%d" % i, [128, 512], BF16) for i in range(4)])
        stf = Ring([k.sb("stf%d" % i, [128, 512], F32) for i in range(3)])
        rhs_fn = lambda kc, tb: (hT[tb], hT[tb][:, kc, :])
        lhs_fn = lambda kc, tt: (hT[tt // 4], hT[tt // 4][:, kc, (tt % 4) * 128:(tt % 4 + 1) * 128])

        def ev_fm(dst, ring):
            def ev(c, m, tb, ps):
                st = ring.next()
                copy_op(k, evac_eng(C), st[0:m, :], ps[0:m, :], [ps], [st])
                k.dma("sp", [(dst[c:c + m, tb * 512:(tb + 1) * 512], st[0:m, :])], reads=[st], sembuf=st)
            return ev

        def ev_tm(cb, n, tt, ps):
            st = stf.next()
            copy_op(k, evac_eng(C), st[:, 0:n], ps[:, 0:n], [ps], [st])
            k.dma("sp", [(scr.ba[tt * 128:(tt + 1) * 128, cb:cb + n], st[:, 0:n])], reads=[st], sembuf=st)

        linear_fm(k, C, rhs_fn, 8, Wd2, 0, 0, 4096, NTOK // 512, ev_fm(scr.qkvT, stb), wring)
        linear_fm(k, C, rhs_fn, 8, Wd2, 0, 4096, 2048, NTOK // 512, ev_fm(scr.zT, stf), wring)
        linear_tm(k, C, lhs_fn, 8, Wd2, 0, 6144, 32, NTOK // 128, ev_tm, wring)


def stage_gdn(k, C, scr, Wt, nseq=NSEQ, nkh=8, nck=None, phases=(1, 2)):
    V = lambda fn, r, w: k.op("dve", fn, reads=r, writes=w)
    A = lambda fn, r, w: k.op("act", fn, reads=r, writes=w)
    G = lambda fn, r, w: k.op("pool", fn, reads=r, writes=w)
    P = lambda fn, r, w: k.op("pe", fn, reads=r, writes=w)
    NCK = S // 128
    with Stage(k):
        cw = k.sb("gcw", [128, 32, 4], F32)
        convw2 = Wt["gdn_conv_w"][0]
        load_cols(k, C, lambda c: cw[:, c, :], convw2, 4, 32)
        k.barrier()
        dtb_bc = bc_row(k, "gdtb", Wt["gdn_dt_bias"][0], 16)
        a_bc = bc_row(k, "ga", Wt["gdn_a_log"][0], 16)
        A(lambda e: e.activation(a_bc[:, :], a_bc[:, :], AF.Exp), [a_bc], [a_bc])
        V(lambda e: e.tensor_scalar(a_bc[:, :], a_bc[:, :], -1.0, None, op0=ALU.mult), [a_bc], [a_bc])
        nwc = k.sb("gnw", [128, 1], F32)
        load_col(k, nwc, Wt["gdn_norm"][0], 128, 1)
        ba = k.sb("gba", [128, NCK, 32], F32)
        bet = k.sb("gbet", [128, NCK, 16], F32)
        nbet = k.sb("gnbet", [128, NCK, 16], F32)
        gg = k.sb("ggg", [128, NCK, 16], F32)
        rawr = Ring([k.sb("graw%d" % i, [128, 3 + S], BF16) for i in range(2)])
        for r_ in rawr.bufs:
            G(lambda e, r_=r_: e.memset(r_[:, 0:3], 0.0), [], [r_])
        dgr = Ring([k.sb("gdg%d" % i, [128, 4, 128], BF16) for i in range(2)])
        cvr = Ring([k.sb("gcv%d" % i, [128, S], BF16) for i in range(2)])
        sqr = Ring([k.sb("gsq%d" % i, [128, 512], BF16) for i in range(2)])
        rsr = Ring([k.sb("grs%d" % i, [128, 512], F32) for i in range(2)])
        QT = k.sb("gQT", [128, S], BF16)
        KT = k.sb("gKT", [128, S], BF16)
        K_tm = k.sb("gK_tm", [128, NCK, 128], BF16)
        V_tm = k.sb("gV_tm", [128, NCK, 256], BF16)
        u0b = k.sb("gu0b", [128, NCK, 2, 128], BF16)
        w0T = k.sb("gw0T", [128, NCK, 2, 128], BF16)
        qkT = k.sb("gqkT", [128, NCK, 2, 128], BF16)
        QdT = k.sb("gQdT", [128, NCK, 2, 128], BF16)
        kdec = k.sb("gkdec", [128, NCK, 2, 128], BF16)
        egl = k.sb("gegl", [128, NCK, 2], F32)
        acsA = k.sb("gacsA", [128, NCK, 4], F32)
        ecolA = k.sb("gecolA", [128, NCK, 4], F32)
        gUall = k.sb("ggUall", [128, 8, 2, 128], F32)
        t1r = Ring([k.sb("gt1%d" % i, [128, 512], F32) for i in range(2)])
        decr = Ring([k.sb("gdec%d" % i, [128, 512], F32) for i in range(2)])
        eRr = Ring([k.sb("geR%d" % i, [128, 512], F32) for i in range(2)])
        tmpr = Ring([k.sb("gtmp%d" % i, [128, 512], F32) for i in range(2)])
        Ybuf = [[k.sb("gY%d_%d" % (g_, i), [128, 4, 128], BF16) for i in range(2)] for g_ in range(4)]
        Wbuf = [[k.sb("gW%d_%d" % (g_, i), [128, 4, 128], BF16) for i in range(2)] for g_ in range(4)]
        Tbuf = [[k.sb("gT%d_%d" % (g_, i), [128, 4, 128], BF16) for i in range(2)] for g_ in range(4)]
        kegA = [k.sb("gkeg%d" % g_, [128, 2, 2, 128], BF16) for g_ in range(4)]
        S32 = [k.sb("gS32_%d" % i, [128, 128], F32) for i in range(2)]
        Sb = [k.sb("gSb_%d" % i, [128, 128], BF16) for i in range(2)]
        vnr = Ring([k.sb("gvn%d" % i, [128, 128], BF16) for i in range(3)])
        szT = [k.sb("gsz%d" % i, [128, S], F32) for i in range(2)]
        outT = [k.sb("gout%d" % i, [128, S], BF16) for i in range(2)]
        osq = Ring([k.sb("gosq%d" % i, [128, 128], BF16) for i in range(6)])
        ocpr = Ring([k.sb("gocp%d" % i, [128, 128], F32) for i in range(6)])
        ors = Ring([k.sb("gors%d" % i, [128, 128], F32) for i in range(4)])
        otm = Ring([k.sb("gotm%d" % i, [128, 128], F32) for i in range(4)])

        def conv_chunk(ch, t0, dst):
            raw = rawr.next(); dg = dgr.next()
            k.dma("sp", [(raw[:, 3:3 + S], scr.qkvT[ch * 128:(ch + 1) * 128, t0:t0 + S])], writes=[raw], sembuf=raw)
            for tap in range(4):
                G(lambda e, dg=dg, tap=tap: e.tensor_scalar(dg[:, tap, :], C.identb[:, :], cw[:, ch, tap:tap + 1], None, op0=ALU.mult), [C.identb, cw], [dg])
            for tb in range(4):
                ps = psf(C)
                for tap in range(4):
                    P(lambda e, ps=ps, dg=dg, tap=tap, raw=raw, tb=tb: e.matmul(
                        ps[:, :], dg[:, tap, :], raw[:, tb * 512 + tap: tb * 512 + tap + 512], start=(tap == 0), stop=(tap == 3)), [dg, raw], [ps])
                A(lambda e, ps=ps, tb=tb: e.activation(dst[:, tb * 512:(tb + 1) * 512], ps[:, :], AF.Silu), [ps], [dst])

        def l2norm(src, dst, scale):
            for tb in range(4):
                sq = sqr.next(); rs = rsr.next(); ps = psf(C)
                A(lambda e, sq=sq, tb=tb: e.activation(sq[:, :], src[:, tb * 512:(tb + 1) * 512], AF.Square), [src], [sq])
                P(lambda e, ps=ps, sq=sq: e.matmul(ps[:, :], C.onesb[:, :], sq[:, :], start=True, stop=True), [C.onesb, sq], [ps])
                A(lambda e, ps=ps, rs=rs: e.activation(rs[:, :], ps[:, :], AF.Sqrt, bias=EPS, scale=1.0), [ps], [rs])
                V(lambda e, rs=rs: e.reciprocal(rs[:, :], rs[:, :]), [rs], [rs])
                V(lambda e, rs=rs, tb=tb: e.scalar_tensor_tensor(dst[:, tb * 512:(tb + 1) * 512], src[:, tb * 512:(tb + 1) * 512], scale, rs[:, :],
                                                                 op0=ALU.mult, op1=ALU.mult), [src, rs], [dst])

        C.pfn = 5
        for s in range(nseq):
            t0 = s * S
            k.dma("sp", [(ba[:, :, :], scr.ba[t0:t0 + S, :].rearrange("(c p) h -> p c h", p=128))], writes=[ba], sembuf=ba)
            A(lambda e: e.activation(bet[:, :, :], ba[:, :, 0:16], AF.Sigmoid), [ba], [bet])
            V(lambda e: e.tensor_scalar(nbet[:, :, :], bet[:, :, :], -1.0, None, op0=ALU.mult), [bet], [nbet])
            V(lambda e: e.tensor_tensor(gg[:, :, :], ba[:, :, 16:32], dtb_bc[:, :].unsqueeze(1).broadcast_to([128, NCK, 16]), op=ALU.add), [ba, dtb_bc], [gg])
            A(lambda e: e.activation(gg[:, :, :], gg[:, :, :], AF.Exp), [gg], [gg])
            A(lambda e: e.activation(gg[:, :, :], gg[:, :, :], AF.Ln, bias=1.0, scale=1.0), [gg], [gg])
            V(lambda e: e.tensor_tensor(gg[:, :, :], gg[:, :, :], a_bc[:, :].unsqueeze(1).broadcast_to([128, NCK, 16]), op=ALU.mult), [gg, a_bc], [gg])
            for kh in range(nkh):
                cq = cvr.next(); conv_chunk(kh, t0, cq); l2norm(cq, QT, 128.0 ** -0.5)
                ck = cvr.next(); conv_chunk(8 + kh, t0, ck); l2norm(ck, KT, 1.0)
                for c in range(0, NCK, 8):
                    pp = psb(C)
                    for j in range(8):
                        P(lambda e, pp=pp, j=j, c=c: e.transpose(pp[:, j * 128:(j + 1) * 128], KT[:, (c + j) * 128:(c + j + 1) * 128], C.identb[:, :]), [KT, C.identb], [pp])
                    copy_op(k, evac_eng(C), K_tm[:, c:c + 8, :], pp[:, :].rearrange("p (a b) -> p a b", a=8), [pp], [K_tm])
                for hv in range(2):
                    cv = cvr.next(); conv_chunk(16 + 2 * kh + hv, t0, cv)
                    for c in range(0, NCK, 8):
                        pp = psb(C)
                        for j in range(8):
                            P(lambda e, pp=pp, j=j, c=c, cv=cv: e.transpose(pp[:, j * 128:(j + 1) * 128], cv[:, (c + j) * 128:(c + j + 1) * 128], C.identb[:, :]), [cv, C.identb], [pp])
                        copy_op(k, evac_eng(C), V_tm[:, c:c + 8, hv * 128:(hv + 1) * 128], pp[:, :].rearrange("p (a b) -> p a b", a=8), [pp], [V_tm])
                    h = 2 * kh + hv
                    k.dma("sp", [(szT[hv][:, :], scr.zT[h * 128:(h + 1) * 128, t0:t0 + S])], writes=[szT[hv]], sembuf=szT[hv])
                    A(lambda e, hv=hv: e.activation(szT[hv][:, :], szT[hv][:, :], AF.Silu), [szT[hv]], [szT[hv]])
                NC1 = (nck or NCK) if 1 in phases else 0
                if NC1:
                    pa = psf(C)
                    for c in range(NC1):
                        P(lambda e, pa=pa, c=c, kh=kh: e.matmul(pa[:, c * 4:c * 4 + 2], C.U32[:, :], gg[:, c, 2 * kh:2 * kh + 2], start=True, stop=True), [C.U32, gg], [pa])
                        P(lambda e, pa=pa, c=c, kh=kh: e.matmul(pa[:, c * 4 + 2:c * 4 + 4], C.ones32[:, :], gg[:, c, 2 * kh:2 * kh + 2], start=True, stop=True), [C.ones32, gg], [pa])
                    V(lambda e, pa=pa: e.tensor_copy(acsA[:, 0:NC1, :], pa[:, 0:NC1 * 4].rearrange("p (c f) -> p c f", f=4)), [pa], [acsA])
                    A(lambda e: e.activation(ecolA[:, 0:NC1, 0:2], acsA[:, 0:NC1, 0:2], AF.Exp), [acsA], [ecolA])
                    V(lambda e: e.tensor_tensor(ecolA[:, 0:NC1, 2:4], acsA[:, 0:NC1, 2:4], acsA[:, 0:NC1, 0:2], op=ALU.subtract), [acsA], [ecolA])
                    A(lambda e: e.activation(ecolA[:, 0:NC1, 2:4], ecolA[:, 0:NC1, 2:4], AF.Exp), [ecolA], [ecolA])
                    A(lambda e: e.activation(egl[:, 0:NC1, :], acsA[:, 0:NC1, 2:4], AF.Exp), [acsA], [egl])
                for half0 in range(0, NC1, 8):
                    ncs = min(8, NC1 - half0)
                    ngr = ncs // 2
                    G(lambda e, half0=half0, ncs=ncs, kh=kh: e.tensor_tensor(
                        gUall[:, 0:ncs, :, :], C.U32[:, :].unsqueeze(1).unsqueeze(1).broadcast_to([128, ncs, 2, 128]),
                        gg[:, half0:half0 + ncs, 2 * kh:2 * kh + 2].unsqueeze(3).broadcast_to([128, ncs, 2, 128]), op=ALU.mult), [C.U32, gg], [gUall])
                    Yc = [None] * ngr; Wc = [None] * ngr; Tc = [None] * ngr
                    for gi in range(ngr):
                        c0 = half0 + 2 * gi
                        bsl = bet[:, c0:c0 + 2, 2 * kh:2 * kh + 2].unsqueeze(3).broadcast_to([128, 2, 2, 128])
                        v4 = lambda t: t[:, :].rearrange("p (a b c) -> p a b c", a=2, b=2)
                        R = psf(C); t1 = t1r.next(); dec = decr.next(); eR = eRr.next(); tmp = tmpr.next()
                        P(lambda e, R=R, gi=gi: e.matmul(R[:, :], C.ones32[:, :], gUall[:, 2 * gi:2 * gi + 2, :, :].rearrange("p a b c -> p (a b c)"), start=True, stop=True),
                          [C.ones32, gUall], [R])
                        V(lambda e, R=R, t1=t1, c0=c0: e.tensor_tensor(v4(t1), v4(R), acsA[:, c0:c0 + 2, 0:2].unsqueeze(3).broadcast_to([128, 2, 2, 128]), op=ALU.subtract), [R, acsA], [t1])
                        V(lambda e, t1=t1: e.tensor_scalar(t1[:, :], t1[:, :], 0.0, None, op0=ALU.min), [t1], [t1])
                        A(lambda e, R=R, eR=eR: e.activation(eR[:, :], R[:, :], AF.Exp), [R], [eR])
                        A(lambda e, t1=t1, dec=dec: e.activation(dec[:, :], t1[:, :], AF.Exp), [t1], [dec])
                        G(lambda e, dec=dec: e.tensor_tensor(dec[:, :].rearrange("p (a c) -> p a c", a=4), dec[:, :].rearrange("p (a c) -> p a c", a=4),
                                                             C.U32[:, :].unsqueeze(1).broadcast_to([128, 4, 128]), op=ALU.mult), [dec, C.U32], [dec])
                        pk = C.pf[5]
                        for cl in range(2):
                            cs = slice((c0 + cl) * 128, (c0 + cl + 1) * 128)
                            P(lambda e, cl=cl, cs=cs: e.matmul(pk[:, cl * 256:cl * 256 + 128], KT[:, cs], KT[:, cs], start=True, stop=True), [KT], [pk])
                            P(lambda e, cl=cl, cs=cs: e.matmul(pk[:, cl * 256 + 128:cl * 256 + 256], KT[:, cs], QT[:, cs], start=True, stop=True), [KT, QT], [pk])
                        pk3 = pk[:, :].rearrange("p (a f) -> p a f", a=2)
                        V(lambda e, tmp=tmp, dec=dec, pk3=pk3: e.tensor_tensor(v4(tmp), pk3[:, :, 0:128].unsqueeze(2).broadcast_to([128, 2, 2, 128]), v4(dec), op=ALU.mult), [pk, dec], [tmp])
                        V(lambda e, tmp=tmp, bsl=bsl: e.tensor_tensor(v4(tmp), v4(tmp), bsl, op=ALU.mult), [tmp, bet], [tmp])
                        X = Ybuf[gi][0]
                        G(lambda e, X=X, tmp=tmp: e.tensor_tensor(X[:, :, :], tmp[:, :].rearrange("p (a c) -> p a c", a=4), C.SU32[:, :].unsqueeze(1).broadcast_to([128, 4, 128]), op=ALU.mult),
                          [tmp, C.SU32], [X])
                        V(lambda e, dec=dec, pk3=pk3, c0=c0: e.tensor_tensor(qkT[:, c0:c0 + 2, :, :], pk3[:, :, 128:256].unsqueeze(2).broadcast_to([128, 2, 2, 128]), v4(dec), op=ALU.mult), [pk, dec], [qkT])
                        G(lambda e, eR=eR, c0=c0: e.tensor_tensor(QdT[:, c0:c0 + 2, :, :], QT[:, c0 * 128:(c0 + 2) * 128].rearrange("p (a c) -> p a c", a=2).unsqueeze(2).broadcast_to([128, 2, 2, 128]),
                                                                 v4(eR), op=ALU.mult), [QT, eR], [QdT])
                        kb4 = K_tm[:, c0:c0 + 2, :].unsqueeze(2).broadcast_to([128, 2, 2, 128])
                        G(lambda e, gi=gi, c0=c0, kb4=kb4: e.tensor_tensor(kegA[gi][:, :, :, :], kb4, ecolA[:, c0:c0 + 2, 0:2].unsqueeze(3).broadcast_to([128, 2, 2, 128]), op=ALU.mult), [K_tm, ecolA], [kegA[gi]])
                        G(lambda e, c0=c0, kb4=kb4: e.tensor_tensor(kdec[:, c0:c0 + 2, :, :], kb4, ecolA[:, c0:c0 + 2, 2:4].unsqueeze(3).broadcast_to([128, 2, 2, 128]), op=ALU.mult), [K_tm, ecolA], [kdec])
                        pp = psb(C)
                        for p4 in range(4):
                            P(lambda e, pp=pp, X=X, p4=p4: e.transpose(pp[:, p4 * 128:(p4 + 1) * 128], X[:, p4, :], C.identb[:, :]), [X, C.identb], [pp])
                        W = Wbuf[gi][0]
                        copy_op(k, evac_eng(C), W[:, :, :], pp[:, 0:512].rearrange("p (a c) -> p a c", a=4), [pp], [W])
                        Tt = Tbuf[gi][0]
                        V(lambda e, Tt=Tt, X=X: e.tensor_tensor(Tt[:, :, :], C.identb[:, :].unsqueeze(1).broadcast_to([128, 4, 128]), X[:, :, :], op=ALU.subtract), [C.identb, X], [Tt])
                        Yc[gi], Wc[gi], Tc[gi] = X, W, Tt
                    for lvl in range(1, 7):
                        nb_ = lvl % 2
                        for gi in range(ngr):
                            Y, W = Yc[gi], Wc[gi]
                            pw = psf(C)
                            for p4 in range(4):
                                P(lambda e, pw=pw, Y=Y, W=W, p4=p4: e.matmul(pw[:, p4 * 128:(p4 + 1) * 128], Y[:, p4, :], W[:, p4, :], start=True, stop=True), [Y, W], [pw])
                            if lvl < 6:
                                py = psf(C)
                                for p4 in range(4):
                                    P(lambda e, py=py, Y=Y, W=W, p4=p4: e.matmul(py[:, p4 * 128:(p4 + 1) * 128], W[:, p4, :], Y[:, p4, :], start=True, stop=True), [Y, W], [py])
                            W2 = Wbuf[gi][nb_]
                            copy_op(k, "act", W2[:, :, :], pw[:, :].rearrange("p (a c) -> p a c", a=4), [pw], [W2])
                            if lvl < 6:
                                Y2 = Ybuf[gi][nb_]
                                copy_op(k, "dve", Y2[:, :, :], py[:, :].rearrange("p (a c) -> p a c", a=4), [py], [Y2])
                                Yc[gi] = Y2
                            Wc[gi] = W2
                        for gi in range(ngr):
                            W, Tt = Wc[gi], Tc[gi]
                            pt_ = psf(C)
                            for p4 in range(4):
                                P(lambda e, pt_=pt_, W=W, Tt=Tt, p4=p4: e.matmul(pt_[:, p4 * 128:(p4 + 1) * 128], W[:, p4, :], Tt[:, p4, :], start=True, stop=False), [W, Tt], [pt_])
                                P(lambda e, pt_=pt_, Tt=Tt, p4=p4: e.matmul(pt_[:, p4 * 128:(p4 + 1) * 128], C.identb[:, :], Tt[:, p4, :], start=False, stop=True), [C.identb, Tt], [pt_])
                            Tt2 = Tbuf[gi][nb_]
                            copy_op(k, evac_eng(C), Tt2[:, :, :], pt_[:, :].rearrange("p (a c) -> p a c", a=4), [pt_], [Tt2])
                            Tc[gi] = Tt2
                    for gi in range(ngr):
                        c0 = half0 + 2 * gi
                        Tt = Tc[gi]
                        pu = psf(C); pw_ = psf(C)
                        for cl in range(2):
                            for hv in range(2):
                                p4 = cl * 2 + hv
                                P(lambda e, pu=pu, Tt=Tt, p4=p4, cl=cl, hv=hv, c0=c0: e.matmul(pu[:, p4 * 128:(p4 + 1) * 128], Tt[:, p4, :], V_tm[:, c0 + cl, hv * 128:(hv + 1) * 128], start=True, stop=True), [Tt, V_tm], [pu])
                                P(lambda e, pw_=pw_, Tt=Tt, p4=p4, cl=cl, hv=hv, gi=gi: e.matmul(pw_[:, p4 * 128:(p4 + 1) * 128], kegA[gi][:, cl, hv, :], Tt[:, p4, :], start=True, stop=True), [Tt, kegA[gi]], [pw_])
                        V(lambda e, pu=pu, c0=c0, kh=kh: e.tensor_tensor(u0b[:, c0:c0 + 2, :, :], pu[:, :].rearrange("p (a b c) -> p a b c", a=2, b=2),
                                                                      bet[:, c0:c0 + 2, 2 * kh:2 * kh + 2].unsqueeze(3).broadcast_to([128, 2, 2, 128]), op=ALU.mult), [pu, bet], [u0b])
                        copy_op(k, "act", w0T[:, c0:c0 + 2, :, :], pw_[:, :].rearrange("p (a b c) -> p a b c", a=2, b=2), [pw_], [w0T])
                for hv in range(2):
                    G(lambda e, hv=hv: e.memset(S32[hv][:, :], 0.0), [], [S32[hv]])
                    G(lambda e, hv=hv: e.memset(Sb[hv][:, :], 0.0), [], [Sb[hv]])
                pend_out = []

                def emit_post(c, hv, sq, ocp):
                    cs = slice(c * 128, (c + 1) * 128)
                    rs = ors.next(); tm_ = otm.next(); pn = psf(C)
                    P(lambda e, pn=pn, sq=sq: e.matmul(pn[:, 0:128], C.onesb[:, :], sq[:, :], start=True, stop=True), [C.onesb, sq], [pn])
                    A(lambda e, pn=pn, rs=rs: e.activation(rs[:, :], pn[:, 0:128], AF.Sqrt, bias=EPS, scale=1.0 / 128), [pn], [rs])
                    V(lambda e, rs=rs: e.reciprocal(rs[:, :], rs[:, :]), [rs], [rs])
                    V(lambda e, tm_=tm_, ocp=ocp, rs=rs: e.scalar_tensor_tensor(tm_[:, :], ocp[:, :], nwc[:, 0:1], rs[:, :], op0=ALU.mult, op1=ALU.mult), [ocp, nwc, rs], [tm_])
                    G(lambda e, tm_=tm_, hv=hv, cs=cs: e.tensor_tensor(outT[hv][:, cs], tm_[:, :], szT[hv][:, cs], op=ALU.mult), [tm_, szT[hv]], [outT[hv]])

                for c in range((nck or NCK) if 2 in phases else 0):
                    cur_out = []
                    for hv in range(2):
                        h = 2 * kh + hv
                        p1 = psf(C); vn = vnr.next()
                        P(lambda e, p1=p1, c=c, hv=hv: e.matmul(p1[:, 0:128], w0T[:, c, hv, :], Sb[hv][:, :], start=True, stop=True), [w0T, Sb[hv]], [p1])
                        V(lambda e, p1=p1, vn=vn, c=c, hv=hv, h=h: e.scalar_tensor_tensor(vn[:, :], p1[:, 0:128], nbet[:, c, h:h + 1], u0b[:, c, hv, :], op0=ALU.mult, op1=ALU.add),
                          [p1, nbet, u0b], [vn])
                        po = psf(C)
                        P(lambda e, po=po, c=c, hv=hv: e.matmul(po[:, 0:128], Sb[hv][:, :], QdT[:, c, hv, :], start=True, stop=False), [Sb[hv], QdT], [po])
                        P(lambda e, po=po, c=c, hv=hv, vn=vn: e.matmul(po[:, 0:128], vn[:, :], qkT[:, c, hv, :], start=False, stop=True), [vn, qkT], [po])
                        p2 = psf(C)
                        P(lambda e, p2=p2, c=c, hv=hv, vn=vn: e.matmul(p2[:, 0:128], kdec[:, c, hv, :], vn[:, :], start=True, stop=True), [kdec, vn], [p2])
                        V(lambda e, p2=p2, c=c, hv=hv: e.scalar_tensor_tensor(S32[hv][:, :], S32[hv][:, :], egl[:, c, hv:hv + 1], p2[:, 0:128], op0=ALU.mult, op1=ALU.add),
                          [S32[hv], egl, p2], [S32[hv]])
                        G(lambda e, hv=hv: e.tensor_copy(Sb[hv][:, :], S32[hv][:, :]), [S32[hv]], [Sb[hv]])
                        sq = osq.next(); ocp = ocpr.next()
                        A(lambda e, sq=sq, po=po: e.activation(sq[:, :], po[:, 0:128], AF.Square), [po], [sq])
                        A(lambda e, ocp=ocp, po=po: e.copy(ocp[:, :], po[:, 0:128]), [po], [ocp])
                        cur_out.append((c, hv, sq, ocp))
                    for args in pend_out:
                        emit_post(*args)
                    pend_out = cur_out
                for args in pend_out:
                    emit_post(*args)
                for hv in range(2):
                    h = 2 * kh + hv
                    k.dma("sp", [(scr.mixT[h * 128:(h + 1) * 128, t0:t0 + S], outT[hv][:, :])], reads=[outT[hv]], sembuf=outT[hv])
        C.pfn = 6
```

```python
from contextlib import ExitStack
import numpy as np
import os
GCUT = int(os.environ.get('GCUT', '99'))
import concourse.bass as bass
import concourse.mybir as mybir
from concourse.bass_utils import run_bass_kernel_spmd

F32 = mybir.dt.float32
BF16 = mybir.dt.bfloat16
AF = mybir.ActivationFunctionType
ALU = mybir.AluOpType
AX = mybir.AxisListType

ENGS = ("pe", "act", "dve", "pool", "sp")
EPOCH = 12000


class Buf:
    def __init__(self, k, name, t):
        self.k = k
        self.name = name
        self.t = t
        self.w = None
        self.r = []
        self.dsem = None
        self.dcnt = 0
        self.psum = False

    def __getitem__(self, idx):
        return self.t[idx]


class K:
    def __init__(self, nc, es):
        self.nc = nc
        self.es = es
        self.ops = {e: [] for e in ENGS}
        self.sems = {}
        self.ecnt = {e: 0 for e in ENGS}
        self.waited = {e: {} for e in ENGS}
        self.stack = [es]
        self.nbuf = 0
        self.ninstr = 0

    def sb(self, name, shape, dt=F32):
        self.uid = getattr(self, "uid", 0) + 1
        name = "%s_u%d" % (name, self.uid)
        t = self.es.enter_context(self.nc.sbuf_tensor(name, list(shape), dt))
        return Buf(self, name, t)

    def ps(self, name, shape, dt=F32):
        t = self.es.enter_context(self.nc.psum_tensor(name, list(shape), dt))
        b = Buf(self, name, t)
        b.psum = True
        return b

    def dram(self, name, shape, dt=F32, kind=None):
        if kind is None:
            t = self.nc.dram_tensor(name, list(shape), dt)
        else:
            t = self.nc.dram_tensor(name, list(shape), dt, kind=kind)
        return Buf(self, name, t.ap())

    def region(self, name):
        return Buf(self, name, None)

    def _dsem(self, b):
        if b.dsem is None:
            key = "d_" + b.name + "_%d" % self.nbuf
            self.nbuf += 1
            self.sems[key] = self.es.enter_context(self.nc.semaphore(key[:40]))
            b.dsem = key
        return b.dsem

    def _deps(self, eng, reads, writes):
        need = {}

        def add(ev, is_war=False):
            if ev is None:
                return
            key, val = ev
            if key.split("#")[0] == eng:
                if eng in ("pe", "sp") or is_war:
                    return
            if need.get(key, 0) < val:
                need[key] = val

        for b in reads:
            add(b.w)
            if b.psum:
                for ev in b.r:
                    if ev[0].split("#")[0] != eng:
                        add(ev)
        for b in writes:
            add(b.w, is_war=True)
            for ev in b.r:
                add(ev, is_war=True)
        out = []
        wd = self.waited[eng]
        for key, val in need.items():
            if wd.get(key, 0) >= val:
                continue
            wd[key] = val
            out.append((key, val))
        return out

    def _commit(self, ev, reads, writes):
        for b in reads:
            b.r.append(ev)
            if len(b.r) > 64:
                m = {}
                for kk, vv in b.r:
                    if m.get(kk, 0) < vv:
                        m[kk] = vv
                b.r = list(m.items())
        for b in writes:
            b.w = ev
            b.r = []

    def op(self, eng, fn, reads=(), writes=()):
        waits = self._deps(eng, reads, writes)
        self.ecnt[eng] += 1
        ep = (self.ecnt[eng] - 1) // EPOCH
        val = self.ecnt[eng] - ep * EPOCH
        ekey = "%s#%d" % (eng, ep)
        if ekey not in self.sems:
            self.sems[ekey] = self.stack[0].enter_context(self.nc.semaphore("es_%s_%d" % (eng, ep)))
        sems = self.sems
        wl = [(sems[k], v) for k, v in waits]
        mysem = sems[ekey]

        def emit(e, fn=fn, wl=wl, mysem=mysem):
            for s, v in wl:
                e.wait_ge(s, v)
            fn(e).then_inc(mysem, 1)

        self.ops[eng].append(emit)
        self._commit((ekey, val), reads, writes)
        self.ninstr += 1

    def dma(self, q, pairs, reads=(), writes=(), sembuf=None):
        assert sembuf is not None
        key = self._dsem(sembuf)
        waits = self._deps(q, reads, writes)
        sems = self.sems
        wl = [(sems[k], v) for k, v in waits]
        sembuf.dcnt += 16 * len(pairs)
        val = sembuf.dcnt
        dsem = sems[key]

        def emit(e, pairs=pairs, wl=wl, dsem=dsem):
            for s, v in wl:
                e.wait_ge(s, v)
            for o, i in pairs:
                e.dma_start(out=o, in_=i).then_inc(dsem, 16)

        self.ops[q].append(emit)
        self._commit((key, val), reads, writes)
        self.ninstr += len(pairs)

    def finish(self, final_bufs):
        nc = self.nc
        finals = []
        for b in final_bufs:
            if b.w is not None:
                finals.append(b.w)
        sems = self.sems

        def fin(e):
            for key, val in finals:
                e.wait_ge(sems[key], val)

        self.ops["sp"].append(fin)
        with nc.Block() as block:
            @block.tensor
            def _(e):
                for f in self.ops["pe"]:
                    f(e)

            @block.scalar
            def _(e):
                for f in self.ops["act"]:
                    f(e)

            @block.vector
            def _(e):
                for f in self.ops["dve"]:
                    f(e)

            @block.gpsimd
            def _(e):
                for f in self.ops["pool"]:
                    f(e)

            @block.sync
            def _(e):
                for f in self.ops["sp"]:
                    f(e)

    def barrier(self):
        targets = []
        for e in ("pe", "act", "dve", "pool"):
            if self.ecnt[e] > 0:
                ep = (self.ecnt[e] - 1) // EPOCH
                targets.append(("%s#%d" % (e, ep), self.ecnt[e] - ep * EPOCH))
        targets += [(key, cnt) for key, cnt in self.dsem_cnt.items() if cnt > 0]
        sems = self.sems
        for eng in ENGS:
            wd = self.waited[eng]
            wl = []
            for key, val in targets:
                if key.split("#")[0] == eng or wd.get(key, 0) >= val:
                    continue
                wd[key] = val
                wl.append((sems[key], val))

            def emit(e, wl=wl):
                for s, v in wl:
                    e.wait_ge(s, v)

            if wl:
                self.ops[eng].append(emit)


def _k_init_extra(self):
    self.dsem_cnt = {}
    self.free_dsems = []
    self.stack = [self.es]
    self._psf = []
    self._psf_i = 0


def _k_dsem(self, b):
    if b.dsem is None:
        if self.free_dsems:
            key = self.free_dsems.pop()
        else:
            key = "d%d" % self.nbuf
            self.nbuf += 1
            self.sems[key] = self.stack[0].enter_context(self.nc.semaphore(key))
            self.dsem_cnt[key] = 0
        b.dsem = key
        b.dcnt = self.dsem_cnt[key]
        self.cur_dsems.append(key)
    return b.dsem


K._dsem = _k_dsem


def _k_dma(self, q, pairs, reads=(), writes=(), sembuf=None, **kw):
    assert sembuf is not None
    key = self._dsem(sembuf)
    waits = self._deps(q, reads, writes)
    sems = self.sems
    wl = [(sems[k_], v) for k_, v in waits]
    sembuf.dcnt += 16 * len(pairs)
    val = sembuf.dcnt
    self.dsem_cnt[key] = val
    dsem = sems[key]

    def emit(e, pairs=pairs, wl=wl, dsem=dsem, kw=kw):
        for s, v in wl:
            e.wait_ge(s, v)
        for o, i in pairs:
            e.dma_start(out=o, in_=i, **kw).then_inc(dsem, 16)

    self.ops[q].append(emit)
    self._commit((key, val), reads, writes)
    self.ninstr += len(pairs)


K.dma = _k_dma


class Stage:
    def __init__(self, k):
        self.k = k

    def __enter__(self):
        k = self.k
        self.es = ExitStack()
        self.prev = k.es
        k.es = self.es
        self.prev_dsems = getattr(k, "cur_dsems", [])
        k.cur_dsems = []
        return self

    def __exit__(self, *a):
        k = self.k
        k.barrier()
        k.free_dsems.extend(k.cur_dsems)
        k.cur_dsems = self.prev_dsems
        k.es = self.prev
        self.es.close()
        return False


EPS = 1e-6
S = 2048
NSEQ = 2
NTOK = NSEQ * S
D = 1024
DFF = 2816
NEG = -30000.0

WSHAPES = {
    "norm_mix": [2, 1024], "norm_ffn": [2, 1024], "norm_ple": [2, 1024],
    "w_in_even": [1, 1024, 4112], "moba_q_norm": [1, 64], "moba_k_norm": [1, 64],
    "ssm_conv_w": [1, 4, 1536], "ssm_conv_b": [1, 1536], "ssm_dt_bias": [1, 16],
    "ssm_a_log": [1, 16], "ssm_d": [1, 16], "ssm_norm": [1, 1024],
    "w_out_even": [1, 1536, 1024], "w_in_odd": [1, 1024, 6176], "gdn_conv_w": [1, 4, 4096],
    "gdn_dt_bias": [1, 16], "gdn_a_log": [1, 16], "gdn_norm": [1, 128],
    "w_out_odd": [1, 2048, 1024], "ffn_w_gate": [2, 1024, 2816], "ffn_w_up": [2, 1024, 2816],
    "ffn_conv_w": [2, 3, 2816], "ffn_conv_b": [2, 2816], "ffn_w_down": [2, 2816, 1024],
    "ple_w_proj": [2, 256, 1024], "ple_w_gate": [2, 1024, 1024],
}


class Ctx:
    pass


def mk_consts(k, C):
    C.identf = k.sb("identf", [128, 128], F32)
    C.identb = k.sb("identb", [128, 128], BF16)
    C.ones32 = k.sb("ones32", [128, 128], F32)
    C.onesb = k.sb("onesb", [128, 128], BF16)
    C.U32 = k.sb("U32", [128, 128], F32)
    C.Ub = k.sb("Ub", [128, 128], BF16)
    C.SU32 = k.sb("SU32", [128, 128], F32)
    C.tribias = k.sb("tribias", [128, 128], BF16)
    C.blk1 = k.sb("blk1", [128, 128], BF16)
    tb32 = k.sb("tb32", [128, 128], F32)
    G = lambda fn, r, w: k.op("pool", fn, reads=r, writes=w)
    V = lambda fn, r, w: k.op("dve", fn, reads=r, writes=w)
    G(lambda e: e.memset(C.ones32[:, :], 1.0), [], [C.ones32])
    G(lambda e: e.affine_select(C.identf[:, :], C.ones32[:, :], pattern=[[-1, 128]], compare_op=ALU.is_equal,
                                fill=0.0, base=0, channel_multiplier=1), [C.ones32], [C.identf])
    G(lambda e: e.affine_select(C.U32[:, :], C.ones32[:, :], pattern=[[1, 128]], compare_op=ALU.is_ge,
                                fill=0.0, base=0, channel_multiplier=-1), [C.ones32], [C.U32])
    G(lambda e: e.affine_select(C.SU32[:, :], C.ones32[:, :], pattern=[[1, 128]], compare_op=ALU.is_gt,
                                fill=0.0, base=0, channel_multiplier=-1), [C.ones32], [C.SU32])
    V(lambda e: e.tensor_scalar(tb32[:, :], C.U32[:, :], -1.0, -NEG, op0=ALU.add, op1=ALU.mult), [C.U32], [tb32])
    V(lambda e: e.tensor_copy(C.tribias[:, :], tb32[:, :]), [tb32], [C.tribias])
    V(lambda e: e.tensor_copy(C.identb[:, :], C.identf[:, :]), [C.identf], [C.identb])
    V(lambda e: e.tensor_copy(C.onesb[:, :], C.ones32[:, :]), [C.ones32], [C.onesb])
    V(lambda e: e.tensor_copy(C.Ub[:, :], C.U32[:, :]), [C.U32], [C.Ub])
    G(lambda e: e.memset(C.blk1[:, :], 0.0), [], [C.blk1])
    G(lambda e: e.memset(C.blk1[0:64, 0:64], 1.0), [], [C.blk1])
    G(lambda e: e.memset(C.blk1[64:128, 64:128], 1.0), [], [C.blk1])
    C.pf = [k.ps("pf%d" % i, [128, 512], F32) for i in range(6)]
    C.pb = [k.ps("pb%d" % i, [128, 1024], BF16) for i in range(2)]
    C.pfi = 0
    C.pfn = 6
    C.pbi = 0
    C.evi = 0


def psf(C):
    C.pfi = (C.pfi + 1) % C.pfn
    return C.pf[C.pfi]


def psb(C):
    C.pbi = (C.pbi + 1) % len(C.pb)
    return C.pb[C.pbi]


class Ring:
    def __init__(self, bufs):
        self.bufs = bufs
        self.i = -1

    def next(self):
        self.i = (self.i + 1) % len(self.bufs)
        return self.bufs[self.i]


def evac_eng(C):
    C.evi += 1
    return "act" if (C.evi % 2) else "dve"


def copy_op(k, eng, out_ap, in_ap, reads, writes):
    if eng == "act":
        k.op("act", lambda e: e.copy(out_ap, in_ap), reads=reads, writes=writes)
    else:
        k.op(eng, lambda e: e.tensor_copy(out_ap, in_ap), reads=reads, writes=writes)


def load_w(k, Wd2, row0, nk, col0, ncols, wt):
    pairs = [(wt[:, kc, 0:ncols], Wd2[row0 + kc * 128: row0 + (kc + 1) * 128, col0:col0 + ncols]) for kc in range(nk)]
    k.dma("pool", pairs, writes=[wt], sembuf=wt)


def load_cols(k, C, dst_fn, rows_ap2, K, nch):
    rw = k.sb("rowsbuf", [K, nch * 128], F32)
    k.dma("sp", [(rw[:, :], rows_ap2)], writes=[rw], sembuf=rw)
    for c in range(nch):
        ps = psf(C)
        k.op("pe", lambda e, ps=ps, c=c: e.transpose(ps[:, 0:K], rw[0:K, c * 128:(c + 1) * 128], C.identf[0:K, 0:K]),
             reads=[rw, C.identf], writes=[ps])
        k.op("dve", lambda e, ps=ps, c=c: e.tensor_copy(dst_fn(c), ps[:, 0:K]), reads=[ps], writes=[])


def norm_to_hT(k, C, src_d, tok0, ntok, gain_ap, hT, hcol0=0):
    gbc = k.sb("gbc", [128, D], F32)
    k.dma("sp", [(gbc[:, :], gain_ap.partition_broadcast(128))], writes=[gbc], sembuf=gbc)
    xr = Ring([k.sb("nx%d" % i, [128, D], F32) for i in range(4)])
    junk = k.sb("njunk", [128, D], BF16)
    hbr = Ring([k.sb("nhb%d" % i, [128, D], BF16) for i in range(3)])
    ssr = Ring([k.sb("nss%d" % i, [128, 2], F32) for i in range(4)])
    for tt in range(ntok // 128):
        xt = xr.next(); hb = hbr.next(); ss = ssr.next()
        r0 = tok0 + tt * 128
        k.dma("sp" if tt % 2 == 0 else "pool", [(xt[:, :], src_d[r0:r0 + 128, :])], writes=[xt], sembuf=xt)
        k.op("act", lambda e, ss=ss: e.memzero(ss[:, :]), writes=[ss])
        k.op("act", lambda e, xt=xt, ss=ss: e.activation(junk[:, :], xt[:, :], AF.Square, accum_out=ss[:, 0:1]),
             reads=[xt, ss], writes=[junk, ss])
        k.op("act", lambda e, ss=ss: e.activation(ss[:, 1:2], ss[:, 0:1], AF.Sqrt, bias=EPS, scale=1.0 / D),
             reads=[ss], writes=[ss])
        k.op("dve", lambda e, ss=ss: e.reciprocal(ss[:, 1:2], ss[:, 1:2]),
             reads=[ss], writes=[ss])
        k.op("dve", lambda e, xt=xt, ss=ss, hb=hb: e.scalar_tensor_tensor(hb[:, :], xt[:, :], ss[:, 1:2], gbc[:, :],
                                                                       op0=ALU.mult, op1=ALU.mult),
             reads=[xt, ss, gbc], writes=[hb])
        pt = psb(C)
        for kc in range(8):
            k.op("pe", lambda e, kc=kc, hb=hb, pt=pt: e.transpose(pt[:, kc * 128:(kc + 1) * 128], hb[:, kc * 128:(kc + 1) * 128], C.identb[:, :]),
                 reads=[hb, C.identb], writes=[pt])
        c0 = hcol0 + tt * 128
        hb_ = hT[c0 // 512]
        copy_op(k, evac_eng(C), hb_[:, :, c0 % 512:c0 % 512 + 128], pt[:, :].rearrange("p (a b) -> p a b", a=8), [pt], [hb_])


def linear_tm(k, C, lhsT_fn, nk, Wd2, row0, col0, ncols, ntt, evac, wring, cbw=512):
    for cb in range(0, ncols, cbw):
        n = min(cbw, ncols - cb)
        wt = wring.next()
        load_w(k, Wd2, row0, nk, col0 + cb, n, wt)
        for tt in range(ntt):
            ps = psf(C)
            for kc in range(nk):
                lb, lap = lhsT_fn(kc, tt)
                k.op("pe", lambda e, ps=ps, lap=lap, wt=wt, kc=kc, n=n: e.matmul(ps[:, 0:n], lap, wt[:, kc, 0:n], start=(kc == 0), stop=(kc == nk - 1)),
                     reads=[lb, wt], writes=[ps])
            evac(cb, n, tt, ps)


def linear_fm(k, C, rhs_fn, nk, Wd2, row0, col0, ncols, ntb, evac, wring):
    for cb in range(0, ncols, 512):
        n = min(512, ncols - cb)
        wt = wring.next()
        load_w(k, Wd2, row0, nk, col0 + cb, n, wt)
        for cc in range(0, n, 128):
            m = min(128, n - cc)
            for tb in range(ntb):
                ps = psf(C)
                for kc in range(nk):
                    rb, rap = rhs_fn(kc, tb)
                    k.op("pe", lambda e, ps=ps, rap=rap, wt=wt, kc=kc, cc=cc, m=m: e.matmul(ps[0:m, :], wt[:, kc, cc:cc + m], rap, start=(kc == 0), stop=(kc == nk - 1)),
                         reads=[rb, wt], writes=[ps])
                evac(cb + cc, m, tb, ps)


def stage_resid_linear(k, C, aT_d, nk, Wd2, x_src, x_dst):
    with Stage(k):
        wres = k.sb("wres", [128, nk, D], BF16)
        for half in range(2):
            k.dma("pool", [(wres[:, kc, half * 512:(half + 1) * 512], Wd2[kc * 128:(kc + 1) * 128, half * 512:(half + 1) * 512])
                           for kc in range(nk)], writes=[wres], sembuf=wres)
        abr = Ring([k.sb("ab%d" % i, [128, nk, 512], BF16) for i in range(2)])
        xr = Ring([k.sb("ox%d" % i, [128, D], F32) for i in range(3)])
        for tb in range(NTOK // 512):
            ab = abr.next()
            k.dma("sp", [(ab[:, :, :], aT_d[:, tb * 512:(tb + 1) * 512].rearrange("(kc p) t -> p kc t", p=128))],
                  writes=[ab], sembuf=ab)
            for t4 in range(4):
                tt = tb * 4 + t4
                xt = xr.next()
                k.dma("sp", [(xt[:, :], x_src[tt * 128:(tt + 1) * 128, :])], writes=[xt], sembuf=xt)
                for cb in range(2):
                    ps = psf(C)
                    for kc in range(nk):
                        k.op("pe", lambda e, ps=ps, ab=ab, kc=kc, t4=t4, cb=cb: e.matmul(
                            ps[:, :], ab[:, kc, t4 * 128:(t4 + 1) * 128], wres[:, kc, cb * 512:(cb + 1) * 512],
                            start=(kc == 0), stop=(kc == nk - 1)), reads=[ab, wres], writes=[ps])
                    k.op("dve", lambda e, ps=ps, xt=xt, cb=cb: e.tensor_tensor(
                        xt[:, cb * 512:(cb + 1) * 512], xt[:, cb * 512:(cb + 1) * 512], ps[:, :], op=ALU.add),
                        reads=[xt, ps], writes=[xt])
                k.dma("act", [(x_dst[tt * 128:(tt + 1) * 128, :], xt[:, :])], reads=[xt], sembuf=xt)


def stage_ffn_a(k, C, x_src, gain_ap, Wg2, Wu2, convw2, convb1, actT_d):
    with Stage(k):
        hT = [k.sb("hT%d" % i, [128, 8, 512], BF16) for i in range(NTOK // 512)]
        norm_to_hT(k, C, x_src, 0, NTOK, gain_ap, hT)
        cw = k.sb("cw", [128, 22, 3], F32)
        cbias = k.sb("cbias", [128, 22], F32)
        load_cols(k, C, lambda c: cw[:, c, :], convw2, 3, 22)
        load_cols(k, C, lambda c: cbias[:, c:c + 1], convb1.rearrange("(o c) -> o c", o=1), 1, 22)
        k.barrier()
        wgr = Ring([k.sb("wg%d" % i, [128, 8, 512], BF16) for i in range(2)])
        wur = Ring([k.sb("wu%d" % i, [128, 8, 512], BF16) for i in range(2)])
        grr = Ring([k.sb("graw%d" % i, [128, NSEQ, 2 + S], BF16) for i in range(2)])
        for g in grr.bufs:
            k.op("pool", lambda e, g=g: e.memset(g[:, :, 0:2], 0.0), writes=[g])
        dgr = Ring([k.sb("dg%d" % i, [128, 3, 128], BF16) for i in range(2)])
        sgr = Ring([k.sb("sg%d" % i, [128, 512], F32) for i in range(2)])
        str_ = Ring([k.sb("fst%d" % i, [128, 512], BF16) for i in range(3)])
        for cb512 in range(0, DFF, 512):
            n = min(512, DFF - cb512)
            wg = wgr.next(); wu = wur.next()
            load_w(k, Wg2, 0, 8, cb512, n, wg)
            load_w(k, Wu2, 0, 8, cb512, n, wu)
            for cc in range(0, n, 128):
                c = (cb512 + cc) // 128
                g = grr.next(); dg = dgr.next()
                for tap in range(3):
                    k.op("pool", lambda e, dg=dg, tap=tap, c=c: e.tensor_scalar(dg[:, tap, :], C.identb[:, :], cw[:, c, tap:tap + 1], None, op0=ALU.mult),
                         reads=[C.identb, cw], writes=[dg])
                for tb in range(8):
                    ps = psf(C)
                    for kc in range(8):
                        k.op("pe", lambda e, ps=ps, wg=wg, kc=kc, cc=cc, tb=tb: e.matmul(
                            ps[:, :], wg[:, kc, cc:cc + 128], hT[tb][:, kc, :], start=(kc == 0), stop=(kc == 7)),
                            reads=[wg, hT[tb]], writes=[ps])
                    sq, t4 = tb // 4, tb % 4
                    copy_op(k, evac_eng(C), g[:, sq, 2 + t4 * 512: 2 + (t4 + 1) * 512], ps[:, :], [ps], [g])
                for tb in range(8):
                    sq, t4 = tb // 4, tb % 4
                    pu = psf(C)
                    for kc in range(8):
                        k.op("pe", lambda e, pu=pu, wu=wu, kc=kc, cc=cc, tb=tb: e.matmul(
                            pu[:, :], wu[:, kc, cc:cc + 128], hT[tb][:, kc, :], start=(kc == 0), stop=(kc == 7)),
                            reads=[wu, hT[tb]], writes=[pu])
                    pc = psf(C)
                    for tap in range(3):
                        k.op("pe", lambda e, pc=pc, dg=dg, tap=tap, g=g, sq=sq, t4=t4: e.matmul(
                            pc[:, :], dg[:, tap, :], g[:, sq, t4 * 512 + tap: t4 * 512 + tap + 512], start=(tap == 0), stop=(tap == 2)),
                            reads=[dg, g], writes=[pc])
                    sg = sgr.next(); st = str_.next()
                    k.op("act", lambda e, sg=sg, pc=pc, c=c: e.activation(sg[:, :], pc[:, :], AF.Silu, bias=cbias[:, c:c + 1], scale=1.0),
                         reads=[pc, cbias], writes=[sg])
                    k.op("dve", lambda e, st=st, sg=sg, pu=pu: e.tensor_tensor(st[:, :], sg[:, :], pu[:, :], op=ALU.mult),
                         reads=[sg, pu], writes=[st])
                    k.dma("sp", [(actT_d[c * 128:(c + 1) * 128, tb * 512:(tb + 1) * 512], st[:, :])], reads=[st], sembuf=st)


def stage_ple(k, C, x_src, gain_ap, Wgate2, Wproj2, p_d, x_dst):
    with Stage(k):
        hT = [k.sb("hT%d" % i, [128, 8, 512], BF16) for i in range(NTOK // 512)]
        norm_to_hT(k, C, x_src, 0, NTOK, gain_ap, hT)
        pT = k.sb("pT", [128, 2, NTOK], BF16)
        pr = Ring([k.sb("pl%d" % i, [128, 256], F32) for i in range(2)])
        pbr = Ring([k.sb("plb%d" % i, [128, 256], BF16) for i in range(2)])
        for tt in range(NTOK // 128):
            pt_ = pr.next(); pb_ = pbr.next()
            k.dma("sp", [(pt_[:, :], p_d[tt * 128:(tt + 1) * 128, :])], writes=[pt_], sembuf=pt_)
            k.op("pool", lambda e, pt_=pt_, pb_=pb_: e.tensor_copy(pb_[:, :], pt_[:, :]), reads=[pt_], writes=[pb_])
            pp = psb(C)
            for j in range(2):
                k.op("pe", lambda e, pp=pp, pb_=pb_, j=j: e.transpose(pp[:, j * 128:(j + 1) * 128], pb_[:, j * 128:(j + 1) * 128], C.identb[:, :]),
                     reads=[pb_, C.identb], writes=[pp])
            copy_op(k, evac_eng(C), pT[:, :, tt * 128:(tt + 1) * 128], pp[:, 0:256].rearrange("p (a b) -> p a b", a=2), [pp], [pT])
        wgr = Ring([k.sb("wpg%d" % i, [128, 8, 512], BF16) for i in range(2)])
        wpr = Ring([k.sb("wpp%d" % i, [128, 2, 512], BF16) for i in range(2)])
        xr = Ring([k.sb("px%d" % i, [128, 512], F32) for i in range(3)])
        sgr = Ring([k.sb("psg%d" % i, [128, 512], F32) for i in range(2)])
        for cb in range(2):
            wg = wgr.next(); wp = wpr.next()
            load_w(k, Wgate2, 0, 8, cb * 512, 512, wg)
            load_w(k, Wproj2, 0, 2, cb * 512, 512, wp)
            for tt in range(NTOK // 128):
                xt = xr.next(); sg = sgr.next()
                k.dma("sp", [(xt[:, :], x_src[tt * 128:(tt + 1) * 128, cb * 512:(cb + 1) * 512])], writes=[xt], sembuf=xt)
                p1 = psf(C)
                for kc in range(8):
                    k.op("pe", lambda e, p1=p1, wg=wg, kc=kc, tt=tt: e.matmul(
                        p1[:, :], hT[tt // 4][:, kc, (tt % 4) * 128:(tt % 4 + 1) * 128], wg[:, kc, :], start=(kc == 0), stop=(kc == 7)),
                        reads=[hT[tt // 4], wg], writes=[p1])
                p2 = psf(C)
                for kc in range(2):
                    k.op("pe", lambda e, p2=p2, wp=wp, kc=kc, tt=tt: e.matmul(
                        p2[:, :], pT[:, kc, tt * 128:(tt + 1) * 128], wp[:, kc, :], start=(kc == 0), stop=(kc == 1)),
                        reads=[pT, wp], writes=[p2])
                k.op("act", lambda e, sg=sg, p1=p1: e.activation(sg[:, :], p1[:, :], AF.Sigmoid), reads=[p1], writes=[sg])
                k.op("dve", lambda e, sg=sg, p2=p2: e.tensor_tensor(sg[:, :], sg[:, :], p2[:, :], op=ALU.mult), reads=[sg, p2], writes=[sg])
                k.op("pool", lambda e, sg=sg, xt=xt: e.tensor_tensor(xt[:, :], xt[:, :], sg[:, :], op=ALU.add), reads=[sg, xt], writes=[xt])
                k.dma("pool", [(x_dst[tt * 128:(tt + 1) * 128, cb * 512:(cb + 1) * 512], xt[:, :])], reads=[xt], sembuf=xt)


def stage_inproj0(k, C, x_src, gain_ap, Wd2, scr):
    with Stage(k):
        hT = [k.sb("hT%d" % i, [128, 8, 512], BF16) for i in range(NTOK // 512)]
        norm_to_hT(k, C, x_src, 0, NTOK, gain_ap, hT)
        wring = Ring([k.sb("w%d" % i, [128, 8, 512], BF16) for i in range(3)])
        stb = Ring([k.sb("stb%d" % i, [128, 512], BF16) for i in range(4)])
        stf = Ring([k.sb("stf%d" % i, [128, 512], F32) for i in range(3)])
        rhs_fn = lambda kc, tb: (hT[tb], hT[tb][:, kc, :])
        lhs_fn = lambda kc, tt: (hT[tt // 4], hT[tt // 4][:, kc, (tt % 4) * 128:(tt % 4 + 1) * 128])

        def ev_fm(dst, r0):
            def ev(c, m, tb, ps):
                st = stb.next()
                copy_op(k, evac_eng(C), st[0:m, :], ps[0:m, :], [ps], [st])
                k.dma("sp", [(dst[r0 + c:r0 + c + m, tb * 512:(tb + 1) * 512], st[0:m, :])], reads=[st], sembuf=st)
            return ev

        def ev_tm(dst, c0, ring):
            def ev(cb, n, tt, ps):
                st = ring.next()
                copy_op(k, evac_eng(C), st[:, 0:n], ps[:, 0:n], [ps], [st])
                k.dma("sp", [(dst[tt * 128:(tt + 1) * 128, c0 + cb:c0 + cb + n], st[:, 0:n])], reads=[st], sembuf=st)
            return ev

        linear_fm(k, C, rhs_fn, 8, Wd2, 0, 0, 1024, NTOK // 512, ev_fm(scr.qkT, 0), wring)
        linear_tm(k, C, lhs_fn, 8, Wd2, 0, 1024, 512, NTOK // 128, ev_tm(scr.v, 0, stb), wring)
        linear_tm(k, C, lhs_fn, 8, Wd2, 0, 1536, 1024, NTOK // 128, ev_tm(scr.z, 0, stf), wring)
        linear_fm(k, C, rhs_fn, 8, Wd2, 0, 2560, 1536, NTOK // 512, ev_fm(scr.xbcT, 0), wring)
        linear_tm(k, C, lhs_fn, 8, Wd2, 0, 4096, 16, NTOK // 128, ev_tm(scr.dt, 0, stf), wring)


def load_col(k, dst, ap1, n, reps, scale=None):
    for r in range(reps):
        k.dma("sp", [(dst[r * n:(r + 1) * n, 0:1], ap1.rearrange("(p o) -> p o", o=1))], writes=[dst], sembuf=dst)
    if scale is not None:
        k.op("dve", lambda e: e.tensor_scalar(dst[:, 0:1], dst[:, 0:1], scale, None, op0=ALU.mult), reads=[dst], writes=[dst])


def stage_attn(k, C, scr, gq_ap, gk_ap, etab_d, auxc_d):
    with Stage(k):
        C.pfn = 4
        o_ps, d_ps = C.pf[4], C.pf[5]
        E = k.sb("E", [68, 8 * 16 * 128], BF16)
        for i in range(8):
            k.dma("pool", [(E[:, i * 2048:(i + 1) * 2048], etab_d[:, i * 2048:(i + 1) * 2048])], writes=[E], sembuf=E)
        gq = k.sb("gq", [128, 1], F32)
        gk = k.sb("gk", [128, 1], F32)
        load_col(k, gq, gq_ap, 64, 2, scale=0.125)
        load_col(k, gk, gk_ap, 64, 2)
        auxT = k.sb("auxT", [68, S], BF16)
        k.op("pool", lambda e: e.memset(auxT[0:64, :], 0.0), writes=[auxT])
        k.dma("pool", [(auxT[64:68, :], auxc_d[:, :])], writes=[auxT], sembuf=auxT)
        qn = [k.sb("qn%d" % c, [128, S], BF16) for c in range(4)]
        kn = [k.sb("kn%d" % c, [128, S], BF16) for c in range(4)]
        rawr = Ring([k.sb("araw%d" % i, [128, S], BF16) for i in range(2)])
        sqr = Ring([k.sb("asq%d" % i, [128, S], BF16) for i in range(2)])
        rsr = Ring([k.sb("ars%d" % i, [128, 512], F32) for i in range(2)])
        kb32 = k.sb("kb32", [128, 4, 8], F32)
        kbhi = k.sb("kbhi", [128, 4, 8], BF16)
        kblo = k.sb("kblo", [128, 4, 8], BF16)
        kbl32 = k.sb("kbl32", [128, 4, 8], F32)
        gs = k.sb("gs", [128, 8, 8], F32)
        cmp_ = k.sb("cmp", [128, 8, 8, 8], F32)
        cnt = k.sb("cnt", [128, 8, 8], F32)
        mbr = Ring([k.sb("mb%d" % i, [128, 64], BF16) for i in range(2)])
        v_sb = k.sb("v_sb", [128, 16, 512], BF16)
        ptr = Ring([k.sb("pt%d" % i, [128, 512], BF16) for i in range(3)])
        rec = k.sb("rec", [64, 512], F32)
        osb = Ring([k.sb("osb%d" % i, [64, 512], BF16) for i in range(2)])
        for s in range(NSEQ):
            t0 = s * S
            for c8 in range(8):
                isq = c8 < 4
                dst = qn[c8] if isq else kn[c8 - 4]
                gcol = gq if isq else gk
                raw = rawr.next(); sq = sqr.next()
                k.dma("sp", [(raw[:, :], scr.qkT[c8 * 128:(c8 + 1) * 128, t0:t0 + S])], writes=[raw], sembuf=raw)
                k.op("act", lambda e, sq=sq, raw=raw: e.activation(sq[:, :], raw[:, :], AF.Square), reads=[raw], writes=[sq])
                for tb in range(4):
                    ps = psf(C); rs = rsr.next()
                    k.op("pe", lambda e, ps=ps, sq=sq, tb=tb: e.matmul(ps[:, :], C.blk1[:, :], sq[:, tb * 512:(tb + 1) * 512], start=True, stop=True),
                         reads=[C.blk1, sq], writes=[ps])
                    k.op("act", lambda e, ps=ps, rs=rs: e.activation(rs[:, :], ps[:, :], AF.Sqrt, bias=EPS, scale=1.0 / 64), reads=[ps], writes=[rs])
                    k.op("dve", lambda e, rs=rs: e.reciprocal(rs[:, :], rs[:, :]), reads=[rs], writes=[rs])
                    k.op("dve", lambda e, dst=dst, raw=raw, gcol=gcol, rs=rs, tb=tb: e.scalar_tensor_tensor(
                        dst[:, tb * 512:(tb + 1) * 512], raw[:, tb * 512:(tb + 1) * 512], gcol[:, 0:1], rs[:, :], op0=ALU.mult, op1=ALU.mult),
                        reads=[raw, gcol, rs], writes=[dst])
            for c in range(4):
                k.op("dve", lambda e, c=c: e.tensor_reduce(kb32[:, c, :], kn[c][:, :].rearrange("p (n j) -> p n j", j=256), axis=AX.X, op=ALU.add),
                     reads=[kn[c]], writes=[kb32])
            k.op("dve", lambda e: e.tensor_scalar(kb32[:, :, :], kb32[:, :, :], 1.0 / 256, None, op0=ALU.mult), reads=[kb32], writes=[kb32])
            k.op("dve", lambda e: e.tensor_copy(kbhi[:, :, :], kb32[:, :, :]), reads=[kb32], writes=[kbhi])
            k.op("dve", lambda e: e.tensor_tensor(kbl32[:, :, :], kb32[:, :, :], kbhi[:, :, :], op=ALU.subtract), reads=[kb32, kbhi], writes=[kbl32])
            k.op("dve", lambda e: e.tensor_copy(kblo[:, :, :], kbl32[:, :, :]), reads=[kbl32], writes=[kblo])
            for qt in range(8, 16):
                nb = qt // 2
                for par in range(2):
                    pg = psf(C)
                    pb_ = par * 64
                    for i4 in range(4):
                        c = i4
                        k.op("pe", lambda e, pg=pg, i4=i4, c=c, pb_=pb_, qt=qt: e.matmul(
                            pg[:, i4 * 8:(i4 + 1) * 8], qn[c][pb_:pb_ + 64, qt * 128:(qt + 1) * 128], kbhi[pb_:pb_ + 64, c, :], start=True, stop=False),
                            reads=[qn[c], kbhi], writes=[pg])
                        k.op("pe", lambda e, pg=pg, i4=i4, c=c, pb_=pb_, qt=qt: e.matmul(
                            pg[:, i4 * 8:(i4 + 1) * 8], qn[c][pb_:pb_ + 64, qt * 128:(qt + 1) * 128], kblo[pb_:pb_ + 64, c, :], start=False, stop=True),
                            reads=[qn[c], kblo], writes=[pg])
                    k.op("dve", lambda e, pg=pg, par=par: e.tensor_copy(gs[:, par * 4:(par + 1) * 4, :], pg[:, 0:32].rearrange("p (h n) -> p h n", h=4)), reads=[pg], writes=[gs])
                k.op("dve", lambda e, nb=nb: e.tensor_tensor(
                    cmp_[:, :, 0:nb, 0:nb], gs[:, :, 0:nb].unsqueeze(2).broadcast_to([128, 8, nb, nb]),
                    gs[:, :, 0:nb].unsqueeze(3).broadcast_to([128, 8, nb, nb]), op=ALU.is_gt), reads=[gs], writes=[cmp_])
                k.op("dve", lambda e, nb=nb: e.tensor_reduce(cnt[:, :, 0:nb], cmp_[:, :, 0:nb, 0:nb], axis=AX.X, op=ALU.add), reads=[cmp_], writes=[cnt])
                mb = mbr.next()
                k.op("pool", lambda e, mb=mb: e.memset(mb[:, :], 0.0), writes=[mb])
                k.op("dve", lambda e, mb=mb, nb=nb: e.tensor_scalar(
                    mb[:, :].rearrange("p (h n) -> p h n", h=8)[:, :, 0:nb], cnt[:, :, 0:nb], 3.0, NEG, op0=ALU.is_ge, op1=ALU.mult),
                    reads=[cnt, mb], writes=[mb])
                pp = psb(C)
                k.op("pe", lambda e, pp=pp, mb=mb: e.transpose(pp[0:64, 0:128], mb[:, :], C.identb[:, :]), reads=[mb, C.identb], writes=[pp])
                copy_op(k, evac_eng(C), auxT[0:64, qt * 128:(qt + 1) * 128], pp[0:64, 0:128], [pp], [auxT])
            if getattr(C, "dbg_aux", None) is not None and s == 0:
                k.dma("sp", [(C.dbg_aux[:, :], auxT[:, :])], reads=[auxT], sembuf=auxT)
            k.dma("sp", [(v_sb[:, :, :], scr.v[t0:t0 + S, :].rearrange("(kt p) c -> p kt c", p=128))], writes=[v_sb], sembuf=v_sb)
            def emit_pv(pt, kt, h, j0, n, nkt):
                k.op("pe", lambda e: e.matmul(o_ps[0:64, j0:512], v_sb[:, kt, h * 64:(h + 1) * 64], pt[:, 0:n], start=(kt == 0), stop=(kt == nkt - 1)),
                     reads=[v_sb, pt], writes=[o_ps])
                k.op("pe", lambda e: e.matmul(d_ps[0:64, j0:512], C.onesb[:, 0:64], pt[:, 0:n], start=(kt == 0), stop=(kt == nkt - 1)),
                     reads=[C.onesb, pt], writes=[d_ps])

            pend = None
            for h in range(8):
                c, pb_ = h // 2, (h % 2) * 64
                for qc in range(4):
                    nkt = 4 * qc + 4
                    for kt in range(nkt):
                        j0 = max(0, kt - 4 * qc) * 128
                        n = 512 - j0
                        q0 = qc * 512 + j0
                        ps = psf(C)
                        diag = kt >= 4 * qc
                        k.op("pe", lambda e, ps=ps, c=c, pb_=pb_, kt=kt, q0=q0, n=n: e.matmul(
                            ps[:, 0:n], kn[c][pb_:pb_ + 64, kt * 128:(kt + 1) * 128], qn[c][pb_:pb_ + 64, q0:q0 + n], start=True, stop=False),
                            reads=[kn[c], qn[c]], writes=[ps])
                        if diag:
                            k.op("pe", lambda e, ps=ps: e.matmul(ps[:, 0:128], C.identb[:, :], C.tribias[:, :], start=False, stop=False),
                                 reads=[C.identb, C.tribias], writes=[ps])
                        eo = (h * 16 + kt) * 128
                        k.op("pe", lambda e, ps=ps, eo=eo, q0=q0, n=n: e.matmul(
                            ps[:, 0:n], E[0:68, eo:eo + 128], auxT[0:68, q0:q0 + n], start=False, stop=True),
                            reads=[E, auxT], writes=[ps])
                        pt = ptr.next()
                        k.op("act", lambda e, pt=pt, ps=ps, n=n: e.activation(pt[:, 0:n], ps[:, 0:n], AF.Exp), reads=[ps], writes=[pt])
                        if pend is not None:
                            emit_pv(*pend)
                        pend = (pt, kt, h, j0, n, nkt)
                    emit_pv(*pend)
                    pend = None
                    ob = osb.next()
                    k.op("dve", lambda e: e.reciprocal(rec[:, :], d_ps[0:64, :]), reads=[d_ps], writes=[rec])
                    k.op("dve", lambda e, ob=ob: e.tensor_tensor(ob[:, :], o_ps[0:64, :], rec[:, :], op=ALU.mult), reads=[o_ps, rec], writes=[ob])
                    k.dma("sp", [(scr.mixT[h * 64:(h + 1) * 64, t0 + qc * 512:t0 + (qc + 1) * 512], ob[:, :])], reads=[ob], sembuf=ob)
        C.pfn = 6


def bc_row(k, name, ap1, n):
    t = k.sb(name, [128, n], F32)
    k.dma("sp", [(t[:, :], ap1.partition_broadcast(128))], writes=[t], sembuf=t)
    return t


def stage_ssd(k, C, scr, Wt):
    V = lambda fn, r, w: k.op("dve", fn, reads=r, writes=w)
    A = lambda fn, r, w: k.op("act", fn, reads=r, writes=w)
    G = lambda fn, r, w: k.op("pool", fn, reads=r, writes=w)
    P = lambda fn, r, w: k.op("pe", fn, reads=r, writes=w)
    with Stage(k):
        C.pfn = 3
        yd = [C.pf[3], C.pf[4]]
        yo = [C.pf[5], C.pf[5]]
        cw = k.sb("scw", [128, 12, 4], F32)
        cbias = k.sb("scb", [128, 12], F32)
        convw2 = Wt["ssm_conv_w"][0]
        load_cols(k, C, lambda c: cw[:, c, :], convw2, 4, 12)
        load_cols(k, C, lambda c: cbias[:, c:c + 1], Wt["ssm_conv_b"][0].rearrange("(o c) -> o c", o=1), 1, 12)
        k.barrier()
        dtb_bc = bc_row(k, "dtb_bc", Wt["ssm_dt_bias"][0], 16)
        a_bc = bc_row(k, "a_bc", Wt["ssm_a_log"][0], 16)
        A(lambda e: e.activation(a_bc[:, :], a_bc[:, :], AF.Exp), [a_bc], [a_bc])
        V(lambda e: e.tensor_scalar(a_bc[:, :], a_bc[:, :], -1.0, None, op0=ALU.mult), [a_bc], [a_bc])
        d_bc = bc_row(k, "d_bc", Wt["ssm_d"][0], 16)
        nw_bc = bc_row(k, "nw_bc", Wt["ssm_norm"][0], 1024)
        xc = [k.sb("xc%d" % i, [128, S], BF16) for i in range(12)]
        rawr = Ring([k.sb("sraw%d" % i, [128, 3 + S], BF16) for i in range(2)])
        for r_ in rawr.bufs:
            G(lambda e, r_=r_: e.memset(r_[:, 0:3], 0.0), [], [r_])
        dgr = Ring([k.sb("sdg%d" % i, [128, 4, 128], BF16) for i in range(2)])
        xs_tm = k.sb("xs_tm", [128, 16, 1024], BF16)
        B_tm = k.sb("B_tm", [128, 16, 256], BF16)
        dt_sp = k.sb("dt_sp", [128, 16, 16], F32)
        dta = k.sb("dta", [128, 16, 16], F32)
        gU = k.sb("gU", [128, 16, 128], F32)
        acs = k.sb("acs", [128, 32], F32)
        eac = k.sb("eac", [128, 16], F32)
        cdec = k.sb("cdec", [128, 16], F32)
        wst = k.sb("wst", [128, 16], F32)
        t1r = Ring([k.sb("st1%d" % i, [128, 512], F32) for i in range(2)])
        t2r = Ring([k.sb("st2%d" % i, [128, 512], F32) for i in range(2)])
        mtr = Ring([k.sb("smt%d" % i, [128, 512], BF16) for i in range(2)])
        cbm = [k.sb("cbm%d" % g, [128, 128], F32) for g in range(2)]
        xdtr = Ring([k.sb("xdt%d" % i, [128, 1024], BF16) for i in range(2)])
        xdtwr = Ring([k.sb("xdtw%d" % i, [128, 1024], BF16) for i in range(2)])
        yr = Ring([k.sb("sy%d" % i, [128, 1024], F32) for i in range(2)])
        tmp2 = k.sb("stmp2", [128, 1024], F32)
        zr = Ring([k.sb("sz%d" % i, [128, 1024], F32) for i in range(2)])
        junk = k.sb("sjunk", [128, 512], BF16)
        ss2 = k.sb("sss2", [128, 4], F32)
        obr = Ring([k.sb("sob%d" % i, [128, 1024], BF16) for i in range(2)])
        obTr = Ring([k.sb("sobT%d" % i, [128, 8, 128], BF16) for i in range(2)])
        prev32 = [k.sb("prev32_%d" % g, [128, 512], F32) for g in range(2)]
        prevb = [k.sb("prevb_%d" % g, [128, 512], BF16) for g in range(2)]
        for s in range(NSEQ):
            t0 = s * S
            for c in range(12):
                raw = rawr.next(); dg = dgr.next()
                k.dma("sp", [(raw[:, 3:3 + S], scr.xbcT[c * 128:(c + 1) * 128, t0:t0 + S])], writes=[raw], sembuf=raw)
                for tap in range(4):
                    G(lambda e, dg=dg, tap=tap, c=c: e.tensor_scalar(dg[:, tap, :], C.identb[:, :], cw[:, c, tap:tap + 1], None, op0=ALU.mult),
                      [C.identb, cw], [dg])
                for tb in range(4):
                    ps = psf(C)
                    for tap in range(4):
                        P(lambda e, ps=ps, dg=dg, tap=tap, raw=raw, tb=tb: e.matmul(
                            ps[:, :], dg[:, tap, :], raw[:, tb * 512 + tap: tb * 512 + tap + 512], start=(tap == 0), stop=(tap == 3)), [dg, raw], [ps])
                    A(lambda e, ps=ps, c=c, tb=tb: e.activation(xc[c][:, tb * 512:(tb + 1) * 512], ps[:, :], AF.Silu, bias=cbias[:, c:c + 1], scale=1.0),
                      [ps, cbias], [xc[c]])
            for kt in range(16):
                pp = psb(C)
                for c in range(8):
                    P(lambda e, pp=pp, c=c, kt=kt: e.transpose(pp[:, c * 128:(c + 1) * 128], xc[c][:, kt * 128:(kt + 1) * 128], C.identb[:, :]),
                      [xc[c], C.identb], [pp])
                copy_op(k, evac_eng(C), xs_tm[:, kt, :], pp[:, :], [pp], [xs_tm])
                pp = psb(C)
                for g in range(2):
                    P(lambda e, pp=pp, g=g, kt=kt: e.transpose(pp[:, g * 128:(g + 1) * 128], xc[8 + g][:, kt * 128:(kt + 1) * 128], C.identb[:, :]),
                      [xc[8 + g], C.identb], [pp])
                copy_op(k, evac_eng(C), B_tm[:, kt, :], pp[:, 0:256], [pp], [B_tm])
            k.dma("sp", [(dt_sp[:, :, :], scr.dt[t0:t0 + S, :].rearrange("(kt p) h -> p kt h", p=128))], writes=[dt_sp], sembuf=dt_sp)
            V(lambda e: e.tensor_tensor(dt_sp[:, :, :], dt_sp[:, :, :], dtb_bc[:, :].unsqueeze(1).broadcast_to([128, 16, 16]), op=ALU.add),
              [dt_sp, dtb_bc], [dt_sp])
            A(lambda e: e.activation(dt_sp[:, :, :], dt_sp[:, :, :], AF.Exp), [dt_sp], [dt_sp])
            A(lambda e: e.activation(dt_sp[:, :, :], dt_sp[:, :, :], AF.Ln, bias=1.0, scale=1.0), [dt_sp], [dt_sp])
            V(lambda e: e.tensor_tensor(dta[:, :, :], dt_sp[:, :, :], a_bc[:, :].unsqueeze(1).broadcast_to([128, 16, 16]), op=ALU.mult),
              [dt_sp, a_bc], [dta])
            for g in range(2):
                G(lambda e, g=g: e.memset(prev32[g][:, :], 0.0), [], [prev32[g]])
                G(lambda e, g=g: e.memset(prevb[g][:, :], 0.0), [], [prevb[g]])
            for c in range(16):
                xdt = xdtr.next(); xdtw = xdtwr.next(); y = yr.next(); zt = zr.next(); ob = obr.next(); obT = obTr.next()
                k.dma("sp", [(zt[:, :], scr.z[t0 + c * 128:t0 + (c + 1) * 128, :])], writes=[zt], sembuf=zt)
                ps = psf(C)
                P(lambda e, ps=ps, c=c: e.matmul(ps[:, 0:16], C.U32[:, :], dta[:, c, :], start=True, stop=True), [C.U32, dta], [ps])
                P(lambda e, ps=ps, c=c: e.matmul(ps[:, 16:32], C.ones32[:, :], dta[:, c, :], start=True, stop=True), [C.ones32, dta], [ps])
                V(lambda e, ps=ps: e.tensor_copy(acs[:, :], ps[:, 0:32]), [ps], [acs])
                A(lambda e: e.activation(eac[:, :], acs[:, 0:16], AF.Exp), [acs], [eac])
                A(lambda e: e.activation(cdec[:, :], acs[:, 16:32], AF.Exp), [acs], [cdec])
                V(lambda e: e.tensor_tensor(wst[:, :], acs[:, 16:32], acs[:, 0:16], op=ALU.subtract), [acs], [wst])
                A(lambda e: e.activation(wst[:, :], wst[:, :], AF.Exp), [wst], [wst])
                V(lambda e, c=c: e.tensor_tensor(gU[:, :, :], C.U32[:, :].unsqueeze(1).broadcast_to([128, 16, 128]),
                                                 dta[:, c, :].unsqueeze(2).broadcast_to([128, 16, 128]), op=ALU.mult), [C.U32, dta], [gU])
                V(lambda e, xdt=xdt, c=c: e.tensor_tensor(xdt[:, :].rearrange("p (h d) -> p h d", h=16), xs_tm[:, c, :].rearrange("p (h d) -> p h d", h=16),
                                                          dt_sp[:, c, :].unsqueeze(2).broadcast_to([128, 16, 64]), op=ALU.mult), [xs_tm, dt_sp], [xdt])
                G(lambda e, xdt=xdt, xdtw=xdtw: e.tensor_tensor(xdtw[:, :].rearrange("p (h d) -> p h d", h=16), xdt[:, :].rearrange("p (h d) -> p h d", h=16),
                                                                wst[:, :].unsqueeze(2).broadcast_to([128, 16, 64]), op=ALU.mult), [xdt, wst], [xdtw])
                for g in range(2):
                    ps = psf(C)
                    P(lambda e, ps=ps, g=g, c=c: e.matmul(ps[:, 0:128], xc[8 + g][:, c * 128:(c + 1) * 128], xc[10 + g][:, c * 128:(c + 1) * 128], start=True, stop=True),
                      [xc[8 + g], xc[10 + g]], [ps])
                    V(lambda e, ps=ps, g=g: e.tensor_tensor(cbm[g][:, :], ps[:, 0:128], C.U32[:, :], op=ALU.mult), [ps, C.U32], [cbm[g]])
                    P(lambda e, g=g, c=c: e.matmul(yo[g][:, :], xc[10 + g][:, c * 128:(c + 1) * 128], prevb[g][:, :], start=True, stop=True),
                      [xc[10 + g], prevb[g]], [yo[g]])
                    ysl0 = y[:, g * 512:(g + 1) * 512]
                    V(lambda e, ysl0=ysl0, g=g: e.tensor_tensor(ysl0.rearrange("p (h d) -> p h d", h=8), yo[g][:, :].rearrange("p (h d) -> p h d", h=8),
                                                              eac[:, g * 8:(g + 1) * 8].unsqueeze(2).broadcast_to([128, 8, 64]), op=ALU.mult), [yo[g], eac], [y])
                for hg in range(4):
                    g = hg // 2
                    R = psf(C); t1 = t1r.next(); t2 = t2r.next(); mt = mtr.next()
                    P(lambda e, R=R, hg=hg: e.matmul(R[:, :], C.ones32[:, :], gU[:, hg * 4:(hg + 1) * 4, :].rearrange("p a b -> p (a b)"), start=True, stop=True),
                      [C.ones32, gU], [R])
                    V(lambda e, R=R, t1=t1, hg=hg: e.tensor_tensor(t1[:, :].rearrange("p (a b) -> p a b", a=4), R[:, :].rearrange("p (a b) -> p a b", a=4),
                                                                  acs[:, hg * 4:(hg + 1) * 4].unsqueeze(2).broadcast_to([128, 4, 128]), op=ALU.subtract), [R, acs], [t1])
                    V(lambda e, t1=t1: e.tensor_scalar(t1[:, :], t1[:, :], 0.0, None, op0=ALU.min), [t1], [t1])
                    A(lambda e, t1=t1, t2=t2: e.activation(t2[:, :], t1[:, :], AF.Exp), [t1], [t2])
                    V(lambda e, t2=t2, mt=mt, g=g: e.tensor_tensor(mt[:, :].rearrange("p (a b) -> p a b", a=4), t2[:, :].rearrange("p (a b) -> p a b", a=4),
                                                                  cbm[g][:, :].unsqueeze(1).broadcast_to([128, 4, 128]), op=ALU.mult), [t2, cbm[g]], [mt])
                    for hh in range(4):
                        h = hg * 4 + hh
                        P(lambda e, mt=mt, hh=hh, h=h, g=g, xdt=xdt: e.matmul(yd[g][:, (h % 8) * 64:(h % 8 + 1) * 64], mt[:, hh * 128:(hh + 1) * 128],
                                                                             xdt[:, h * 64:(h + 1) * 64], start=True, stop=True), [mt, xdt], [yd[g]])
                for g in range(2):
                    ysl = y[:, g * 512:(g + 1) * 512]
                    V(lambda e, ysl=ysl, g=g: e.tensor_tensor(ysl, ysl, yd[g][:, :], op=ALU.add), [y, yd[g]], [y])
                G(lambda e, c=c: e.tensor_tensor(tmp2[:, :].rearrange("p (h d) -> p h d", h=16), xs_tm[:, c, :].rearrange("p (h d) -> p h d", h=16),
                                                 d_bc[:, :].unsqueeze(2).broadcast_to([128, 16, 64]), op=ALU.mult), [xs_tm, d_bc], [tmp2])
                G(lambda e, y=y: e.tensor_tensor(y[:, :], y[:, :], tmp2[:, :], op=ALU.add), [y, tmp2], [y])
                for g in range(2):
                    st = psf(C)
                    P(lambda e, st=st, g=g, c=c, xdtw=xdtw: e.matmul(st[:, :], B_tm[:, c, g * 128:(g + 1) * 128], xdtw[:, g * 512:(g + 1) * 512], start=True, stop=True),
                      [B_tm, xdtw], [st])
                    V(lambda e, g=g: e.tensor_tensor(prev32[g][:, :].rearrange("p (h d) -> p h d", h=8), prev32[g][:, :].rearrange("p (h d) -> p h d", h=8),
                                                     cdec[:, g * 8:(g + 1) * 8].unsqueeze(2).broadcast_to([128, 8, 64]), op=ALU.mult), [prev32[g], cdec], [prev32[g]])
                    V(lambda e, g=g, st=st: e.tensor_tensor(prev32[g][:, :], prev32[g][:, :], st[:, :], op=ALU.add), [prev32[g], st], [prev32[g]])
                    G(lambda e, g=g: e.tensor_copy(prevb[g][:, :], prev32[g][:, :]), [prev32[g]], [prevb[g]])
                A(lambda e, zt=zt: e.activation(zt[:, :], zt[:, :], AF.Silu), [zt], [zt])
                V(lambda e, y=y, zt=zt: e.tensor_tensor(y[:, :], y[:, :], zt[:, :], op=ALU.mult), [y, zt], [y])
                G(lambda e: e.memset(ss2[:, :], 0.0), [], [ss2])
                for g in range(2):
                    A(lambda e, y=y, g=g: e.activation(junk[:, :], y[:, g * 512:(g + 1) * 512], AF.Square, accum_out=ss2[:, g:g + 1]), [y, ss2], [junk, ss2])
                A(lambda e: e.activation(ss2[:, 2:4], ss2[:, 0:2], AF.Sqrt, bias=EPS, scale=1.0 / 512), [ss2], [ss2])
                V(lambda e: e.reciprocal(ss2[:, 2:4], ss2[:, 2:4]), [ss2], [ss2])
                for g in range(2):
                    V(lambda e, y=y, ob=ob, g=g: e.scalar_tensor_tensor(ob[:, g * 512:(g + 1) * 512], y[:, g * 512:(g + 1) * 512], ss2[:, 2 + g:3 + g],
                                                                       nw_bc[:, g * 512:(g + 1) * 512], op0=ALU.mult, op1=ALU.mult), [y, ss2, nw_bc], [ob])
                pp = psb(C)
                for j in range(8):
                    P(lambda e, pp=pp, ob=ob, j=j: e.transpose(pp[:, j * 128:(j + 1) * 128], ob[:, j * 128:(j + 1) * 128], C.identb[:, :]), [ob, C.identb], [pp])
                copy_op(k, evac_eng(C), obT[:, :, :], pp[:, :].rearrange("p (a b) -> p a b", a=8), [pp], [obT])
                k.dma("sp", [(scr.mixT[512:1536, t0 + c * 128:t0 + (c + 1) * 128].rearrange("(j p) t -> p j t", p=128), obT[:, :, :])], reads=[obT], sembuf=obT)
        C.pfn = 6


def host_consts():
    slopes = np.array([2.0 ** (-(h + 1)) for h in range(8)], dtype=np.float64)
    etab = np.zeros((68, 8, 16, 128), dtype=np.float32)
    for h in range(8):
        for kt in range(16):
            etab[((h % 2) * 4 + h // 2) * 8 + kt // 2, h, kt, :] = 1.0
            etab[64, h, kt, :] = -128.0 * slopes[h]
            etab[65, h, kt, :] = -slopes[h]
            etab[66, h, kt, :] = slopes[h] * np.arange(128)
            etab[67, h, kt, :] = 128.0 * slopes[h] * kt
    auxc = np.zeros((4, S), dtype=np.float32)
    pos = np.arange(S)
    auxc[0] = pos // 128
    auxc[1] = pos % 128
    auxc[2] = 1.0
    auxc[3] = 1.0
    return {"etab": etab.reshape(68, 8 * 16 * 128), "auxc": auxc}


def build(upto="full", dumps=()):
    nc = bass.Bass("TRN2", target_bir_lowering=False)
    root = ExitStack()
    k = K(nc, root)
    _k_init_extra(k)
    k.cur_dsems = []
    C = Ctx()
    x_d = k.dram("x", [NTOK, D], F32, kind="ExternalInput").t
    p_d = [k.dram("p%d" % i, [NTOK, 256], F32, kind="ExternalInput").t for i in range(2)]
    Wt = {n: k.dram(n, shp, F32, kind="ExternalInput").t for n, shp in WSHAPES.items()}
    etab_d = k.dram("etab", [68, 8 * 16 * 128], F32, kind="ExternalInput").t
    auxc_d = k.dram("auxc", [4, S], F32, kind="ExternalInput").t
    y_d = k.dram("y", [NTOK, D], F32, kind="ExternalOutput").t
    scr = Ctx()
    scr.qkT = k.dram("s_qkT", [1024, NTOK], BF16).t
    scr.v = k.dram("s_v", [NTOK, 512], BF16).t
    scr.z = k.dram("s_z", [NTOK, 1024], F32).t
    scr.xbcT = k.dram("s_xbcT", [1536, NTOK], BF16).t
    scr.dt = k.dram("s_dt", [NTOK, 16], F32).t
    scr.mixT = k.dram("s_mixT", [2048, NTOK], BF16).t
    scr.actT = k.dram("s_actT", [DFF, NTOK], BF16).t
    scr.qkvT = k.dram("s_qkvT", [4096, NTOK], BF16).t
    scr.zT = k.dram("s_zT", [2048, NTOK], F32).t
    scr.ba = k.dram("s_ba", [NTOK, 32], F32).t
    xa = k.dram("s_xa", [NTOK, D], F32).t
    xb = k.dram("s_xb", [NTOK, D], F32).t
    mk_consts(k, C)
    k.barrier()
    if "auxT" in dumps:
        C.dbg_aux = k.dram("dbg_auxT", [68, S], BF16, kind="ExternalOutput").t
        dumps = [d_ for d_ in dumps if d_ != "auxT"]
    order = ["in0", "attn", "ssd", "x1", "x2", "x3", "in1", "gdn", "x4", "x5", "full"]
    lim = order.index(upto)
    last = x_d

    def done(name):
        return order.index(name) > lim

    stage_inproj0(k, C, x_d, Wt["norm_mix"][0], Wt["w_in_even"][0], scr)
    if not done("attn"):
        stage_attn(k, C, scr, Wt["moba_q_norm"][0], Wt["moba_k_norm"][0], etab_d, auxc_d)
    if not done("ssd"):
        stage_ssd(k, C, scr, Wt)
    if not done("x1"):
        stage_resid_linear(k, C, scr.mixT[0:1536, :], 12, Wt["w_out_even"][0], x_d, xa)
        last = xa
    if not done("x2"):
        stage_ffn_a(k, C, xa, Wt["norm_ffn"][0], Wt["ffn_w_gate"][0], Wt["ffn_w_up"][0], Wt["ffn_conv_w"][0], Wt["ffn_conv_b"][0], scr.actT)
        stage_resid_linear(k, C, scr.actT, 22, Wt["ffn_w_down"][0], xa, xb)
        last = xb
    if not done("x3"):
        stage_ple(k, C, xb, Wt["norm_ple"][0], Wt["ple_w_gate"][0], Wt["ple_w_proj"][0], p_d[0], xa)
        last = xa
    if not done("in1"):
        stage_inproj1(k, C, xa, Wt["norm_mix"][1], Wt["w_in_odd"][0], scr)
    if not done("gdn"):
        stage_gdn(k, C, scr, Wt)
    if not done("x4"):
        stage_resid_linear(k, C, scr.mixT, 16, Wt["w_out_odd"][0], xa, xb)
        last = xb
    if not done("x5"):
        stage_ffn_a(k, C, xb, Wt["norm_ffn"][1], Wt["ffn_w_gate"][1], Wt["ffn_w_up"][1], Wt["ffn_conv_w"][1], Wt["ffn_conv_b"][1], scr.actT)
        stage_resid_linear(k, C, scr.actT, 22, Wt["ffn_w_down"][1], xb, xa)
        last = xa
    if not done("full"):
        stage_ple(k, C, xa, Wt["norm_ple"][1], Wt["ple_w_gate"][1], Wt["ple_w_proj"][1], p_d[1], xb)
        last = xb
    with Stage(k):
        cp = k.sb("cpbuf", [128, 4, D], F32)
        for i in range(NTOK // 512):
            k.dma("sp", [(cp[:, :, :], last[i * 512:(i + 1) * 512, :].rearrange("(a p) d -> p a d", p=128))], writes=[cp], sembuf=cp)
            k.dma("sp", [(y_d[i * 512:(i + 1) * 512, :].rearrange("(a p) d -> p a d", p=128), cp[:, :, :])], reads=[cp], sembuf=cp)
        for name in dumps:
            src = getattr(scr, name)
            shp = list(src.shape)
            dd = k.dram("dbg_" + name, shp, src.dtype, kind="ExternalOutput").t
            rows = shp[0]
            cb2 = k.sb("cpb_" + name, [128, shp[1]], src.dtype)
            for i in range(rows // 128):
                k.dma("sp", [(cb2[:, :], src[i * 128:(i + 1) * 128, :])], writes=[cb2], sembuf=cb2)
                k.dma("sp", [(dd[i * 128:(i + 1) * 128, :], cb2[:, :])], reads=[cb2], sembuf=cb2)
    k.finish([])
    root.close()
    return nc


_NC_CACHE = {}


def kernel(**inputs):
    n = 8
    hc = host_consts()
    x = np.ascontiguousarray(inputs["x"], dtype=np.float32).reshape(n, NTOK, D)
    p = np.ascontiguousarray(inputs["p"], dtype=np.float32)
    in_maps = []
    for c in range(n):
        m = {"x": x[c], "p0": np.ascontiguousarray(p[0, 2 * c:2 * c + 2].reshape(NTOK, 256)),
             "p1": np.ascontiguousarray(p[1, 2 * c:2 * c + 2].reshape(NTOK, 256)),
             "etab": hc["etab"], "auxc": hc["auxc"]}
        for nme in WSHAPES:
            m[nme] = np.ascontiguousarray(inputs[nme], dtype=np.float32)
        in_maps.append(m)
    if "full" not in _NC_CACHE:
        _NC_CACHE["full"] = build("full")
    res = run_bass_kernel_spmd(_NC_CACHE["full"], in_maps, core_ids=list(range(n)))
    out = np.stack([r["y"] for r in res.results], axis=0)
    return out.reshape(16, S, D).astype(np.float32)


def stage_inproj1(k, C, x_src, gain_ap, Wd2, scr):
    with Stage(k):
        hT = [k.sb("hT%d" % i, [128, 8, 512], BF16) for i in range(NTOK // 512)]
        norm_to_hT(k, C, x_src, 0, NTOK, gain_ap, hT)
        wring = Ring([k.sb("w%d" % i, [128, 8, 512], BF16) for i in range(3)])
        stb = Ring([k.sb("stb%d" % i, [128, 512], BF16) for i in range(4)])
        stf = Ring([k.sb("stf%d" % i, [128, 512], F32) for i in range(3)])
        rhs_fn = lambda kc, tb: (hT[tb], hT[tb][:, kc, :])
        lhs_fn = lambda kc, tt: (hT[tt // 4], hT[tt // 4][:, kc, (tt % 4) * 128:(tt % 4 + 1) * 128])

        def ev_fm(dst, ring):
            def ev(c, m, tb, ps):
                st = ring.next()
                copy_op(k, evac_eng(C), st[0:m, :], ps[0:m, :], [ps], [st])
                k.dma("sp", [(dst[c:c + m, tb * 512:(tb + 1) * 512], st[0:m, :])], reads=[st], sembuf=st)
            return ev

        def ev_tm(cb, n, tt, ps):
            st = stf.next()
            copy_op(k, evac_eng(C), st[:, 0:n], ps[:, 0:n], [ps], [st])
            k.dma("sp", [(scr.ba[tt * 128:(tt + 1) * 128, cb:cb + n], st[:, 0:n])], reads=[st], sembuf=st)

        linear_fm(k, C, rhs_fn, 8, Wd2, 0, 0, 4096, NTOK // 512, ev_fm(scr.qkvT, stb), wring)
        linear_fm(k, C, rhs_fn, 8, Wd2, 0, 4096, 2048, NTOK // 512, ev_fm(scr.zT, stf), wring)
        linear_tm(k, C, lhs_fn, 8, Wd2, 0, 6144, 32, NTOK // 128, ev_tm, wring)


def stage_gdn(k, C, scr, Wt, nseq=NSEQ, nkh=8, nck=None, phases=(1, 2)):
    V = lambda fn, r, w: k.op("dve", fn, reads=r, writes=w)
    A = lambda fn, r, w: k.op("act", fn, reads=r, writes=w)
    G = lambda fn, r, w: k.op("pool", fn, reads=r, writes=w)
    P = lambda fn, r, w: k.op("pe", fn, reads=r, writes=w)
    NCK = S // 128
    with Stage(k):
        cw = k.sb("gcw", [128, 32, 4], F32)
        convw2 = Wt["gdn_conv_w"][0]
        load_cols(k, C, lambda c: cw[:, c, :], convw2, 4, 32)
        k.barrier()
        dtb_bc = bc_row(k, "gdtb", Wt["gdn_dt_bias"][0], 16)
        a_bc = bc_row(k, "ga", Wt["gdn_a_log"][0], 16)
        A(lambda e: e.activation(a_bc[:, :], a_bc[:, :], AF.Exp), [a_bc], [a_bc])
        V(lambda e: e.tensor_scalar(a_bc[:, :], a_bc[:, :], -1.0, None, op0=ALU.mult), [a_bc], [a_bc])
        nwc = k.sb("gnw", [128, 1], F32)
        load_col(k, nwc, Wt["gdn_norm"][0], 128, 1)
        ba = k.sb("gba", [128, NCK, 32], F32)
        bet = k.sb("gbet", [128, NCK, 16], F32)
        nbet = k.sb("gnbet", [128, NCK, 16], F32)
        gg = k.sb("ggg", [128, NCK, 16], F32)
        rawr = Ring([k.sb("graw%d" % i, [128, 3 + S], BF16) for i in range(2)])
        for r_ in rawr.bufs:
            G(lambda e, r_=r_: e.memset(r_[:, 0:3], 0.0), [], [r_])
        dgr = Ring([k.sb("gdg%d" % i, [128, 4, 128], BF16) for i in range(2)])
        cvr = Ring([k.sb("gcv%d" % i, [128, S], BF16) for i in range(2)])
        sqr = Ring([k.sb("gsq%d" % i, [128, 512], BF16) for i in range(2)])
        rsr = Ring([k.sb("grs%d" % i, [128, 512], F32) for i in range(2)])
        QT = k.sb("gQT", [128, S], BF16)
        KT = k.sb("gKT", [128, S], BF16)
        K_tm = k.sb("gK_tm", [128, NCK, 128], BF16)
        V_tm = k.sb("gV_tm", [128, NCK, 256], BF16)
        u0b = k.sb("gu0b", [128, NCK, 2, 128], BF16)
        w0T = k.sb("gw0T", [128, NCK, 2, 128], BF16)
        qkT = k.sb("gqkT", [128, NCK, 2, 128], BF16)
        QdT = k.sb("gQdT", [128, NCK, 2, 128], BF16)
        kdec = k.sb("gkdec", [128, NCK, 2, 128], BF16)
        egl = k.sb("gegl", [128, NCK, 2], F32)
        acsA = k.sb("gacsA", [128, NCK, 4], F32)
        ecolA = k.sb("gecolA", [128, NCK, 4], F32)
        gUall = k.sb("ggUall", [128, 8, 2, 128], F32)
        t1r = Ring([k.sb("gt1%d" % i, [128, 512], F32) for i in range(2)])
        decr = Ring([k.sb("gdec%d" % i, [128, 512], F32) for i in range(2)])
        eRr = Ring([k.sb("geR%d" % i, [128, 512], F32) for i in range(2)])
        tmpr = Ring([k.sb("gtmp%d" % i, [128, 512], F32) for i in range(2)])
        Ybuf = [[k.sb("gY%d_%d" % (g_, i), [128, 4, 128], BF16) for i in range(2)] for g_ in range(4)]
        Wbuf = [[k.sb("gW%d_%d" % (g_, i), [128, 4, 128], BF16) for i in range(2)] for g_ in range(4)]
        Tbuf = [[k.sb("gT%d_%d" % (g_, i), [128, 4, 128], BF16) for i in range(2)] for g_ in range(4)]
        kegA = [k.sb("gkeg%d" % g_, [128, 2, 2, 128], BF16) for g_ in range(4)]
        S32 = [k.sb("gS32_%d" % i, [128, 128], F32) for i in range(2)]
        Sb = [k.sb("gSb_%d" % i, [128, 128], BF16) for i in range(2)]
        vnr = Ring([k.sb("gvn%d" % i, [128, 128], BF16) for i in range(3)])
        szT = [k.sb("gsz%d" % i, [128, S], F32) for i in range(2)]
        outT = [k.sb("gout%d" % i, [128, S], BF16) for i in range(2)]
        osq = Ring([k.sb("gosq%d" % i, [128, 128], BF16) for i in range(6)])
        ocpr = Ring([k.sb("gocp%d" % i, [128, 128], F32) for i in range(6)])
        ors = Ring([k.sb("gors%d" % i, [128, 128], F32) for i in range(4)])
        otm = Ring([k.sb("gotm%d" % i, [128, 128], F32) for i in range(4)])

        def conv_chunk(ch, t0, dst):
            raw = rawr.next(); dg = dgr.next()
            k.dma("sp", [(raw[:, 3:3 + S], scr.qkvT[ch * 128:(ch + 1) * 128, t0:t0 + S])], writes=[raw], sembuf=raw)
            for tap in range(4):
                G(lambda e, dg=dg, tap=tap: e.tensor_scalar(dg[:, tap, :], C.identb[:, :], cw[:, ch, tap:tap + 1], None, op0=ALU.mult), [C.identb, cw], [dg])
            for tb in range(4):
                ps = psf(C)
                for tap in range(4):
                    P(lambda e, ps=ps, dg=dg, tap=tap, raw=raw, tb=tb: e.matmul(
                        ps[:, :], dg[:, tap, :], raw[:, tb * 512 + tap: tb * 512 + tap + 512], start=(tap == 0), stop=(tap == 3)), [dg, raw], [ps])
                A(lambda e, ps=ps, tb=tb: e.activation(dst[:, tb * 512:(tb + 1) * 512], ps[:, :], AF.Silu), [ps], [dst])

        def l2norm(src, dst, scale):
            for tb in range(4):
                sq = sqr.next(); rs = rsr.next(); ps = psf(C)
                A(lambda e, sq=sq, tb=tb: e.activation(sq[:, :], src[:, tb * 512:(tb + 1) * 512], AF.Square), [src], [sq])
                P(lambda e, ps=ps, sq=sq: e.matmul(ps[:, :], C.onesb[:, :], sq[:, :], start=True, stop=True), [C.onesb, sq], [ps])
                A(lambda e, ps=ps, rs=rs: e.activation(rs[:, :], ps[:, :], AF.Sqrt, bias=EPS, scale=1.0), [ps], [rs])
                V(lambda e, rs=rs: e.reciprocal(rs[:, :], rs[:, :]), [rs], [rs])
                V(lambda e, rs=rs, tb=tb: e.scalar_tensor_tensor(dst[:, tb * 512:(tb + 1) * 512], src[:, tb * 512:(tb + 1) * 512], scale, rs[:, :],
                                                                 op0=ALU.mult, op1=ALU.mult), [src, rs], [dst])

        C.pfn = 5
        for s in range(nseq):
            t0 = s * S
            k.dma("sp", [(ba[:, :, :], scr.ba[t0:t0 + S, :].rearrange("(c p) h -> p c h", p=128))], writes=[ba], sembuf=ba)
            A(lambda e: e.activation(bet[:, :, :], ba[:, :, 0:16], AF.Sigmoid), [ba], [bet])
            V(lambda e: e.tensor_scalar(nbet[:, :, :], bet[:, :, :], -1.0, None, op0=ALU.mult), [bet], [nbet])
            V(lambda e: e.tensor_tensor(gg[:, :, :], ba[:, :, 16:32], dtb_bc[:, :].unsqueeze(1).broadcast_to([128, NCK, 16]), op=ALU.add), [ba, dtb_bc], [gg])
            A(lambda e: e.activation(gg[:, :, :], gg[:, :, :], AF.Exp), [gg], [gg])
            A(lambda e: e.activation(gg[:, :, :], gg[:, :, :], AF.Ln, bias=1.0, scale=1.0), [gg], [gg])
            V(lambda e: e.tensor_tensor(gg[:, :, :], gg[:, :, :], a_bc[:, :].unsqueeze(1).broadcast_to([128, NCK, 16]), op=ALU.mult), [gg, a_bc], [gg])
            for kh in range(nkh):
                cq = cvr.next(); conv_chunk(kh, t0, cq); l2norm(cq, QT, 128.0 ** -0.5)
                ck = cvr.next(); conv_chunk(8 + kh, t0, ck); l2norm(ck, KT, 1.0)
                for c in range(0, NCK, 8):
                    pp = psb(C)
                    for j in range(8):
                        P(lambda e, pp=pp, j=j, c=c: e.transpose(pp[:, j * 128:(j + 1) * 128], KT[:, (c + j) * 128:(c + j + 1) * 128], C.identb[:, :]), [KT, C.identb], [pp])
                    copy_op(k, evac_eng(C), K_tm[:, c:c + 8, :], pp[:, :].rearrange("p (a b) -> p a b", a=8), [pp], [K_tm])
                for hv in range(2):
                    cv = cvr.next(); conv_chunk(16 + 2 * kh + hv, t0, cv)
                    for c in range(0, NCK, 8):
                        pp = psb(C)
                        for j in range(8):
                            P(lambda e, pp=pp, j=j, c=c, cv=cv: e.transpose(pp[:, j * 128:(j + 1) * 128], cv[:, (c + j) * 128:(c + j + 1) * 128], C.identb[:, :]), [cv, C.identb], [pp])
                        copy_op(k, evac_eng(C), V_tm[:, c:c + 8, hv * 128:(hv + 1) * 128], pp[:, :].rearrange("p (a b) -> p a b", a=8), [pp], [V_tm])
                    h = 2 * kh + hv
                    k.dma("sp", [(szT[hv][:, :], scr.zT[h * 128:(h + 1) * 128, t0:t0 + S])], writes=[szT[hv]], sembuf=szT[hv])
                    A(lambda e, hv=hv: e.activation(szT[hv][:, :], szT[hv][:, :], AF.Silu), [szT[hv]], [szT[hv]])
                NC1 = (nck or NCK) if 1 in phases else 0
                if NC1:
                    pa = psf(C)
                    for c in range(NC1):
                        P(lambda e, pa=pa, c=c, kh=kh: e.matmul(pa[:, c * 4:c * 4 + 2], C.U32[:, :], gg[:, c, 2 * kh:2 * kh + 2], start=True, stop=True), [C.U32, gg], [pa])
                        P(lambda e, pa=pa, c=c, kh=kh: e.matmul(pa[:, c * 4 + 2:c * 4 + 4], C.ones32[:, :], gg[:, c, 2 * kh:2 * kh + 2], start=True, stop=True), [C.ones32, gg], [pa])
                    V(lambda e, pa=pa: e.tensor_copy(acsA[:, 0:NC1, :], pa[:, 0:NC1 * 4].rearrange("p (c f) -> p c f", f=4)), [pa], [acsA])
                    A(lambda e: e.activation(ecolA[:, 0:NC1, 0:2], acsA[:, 0:NC1, 0:2], AF.Exp), [acsA], [ecolA])
                    V(lambda e: e.tensor_tensor(ecolA[:, 0:NC1, 2:4], acsA[:, 0:NC1, 2:4], acsA[:, 0:NC1, 0:2], op=ALU.subtract), [acsA], [ecolA])
                    A(lambda e: e.activation(ecolA[:, 0:NC1, 2:4], ecolA[:, 0:NC1, 2:4], AF.Exp), [ecolA], [ecolA])
                    A(lambda e: e.activation(egl[:, 0:NC1, :], acsA[:, 0:NC1, 2:4], AF.Exp), [acsA], [egl])
                for half0 in range(0, NC1, 8):
                    ncs = min(8, NC1 - half0)
                    ngr = ncs // 2
                    G(lambda e, half0=half0, ncs=ncs, kh=kh: e.tensor_tensor(
                        gUall[:, 0:ncs, :, :], C.U32[:, :].unsqueeze(1).unsqueeze(1).broadcast_to([128, ncs, 2, 128]),
                        gg[:, half0:half0 + ncs, 2 * kh:2 * kh + 2].unsqueeze(3).broadcast_to([128, ncs, 2, 128]), op=ALU.mult), [C.U32, gg], [gUall])
                    Yc = [None] * ngr; Wc = [None] * ngr; Tc = [None] * ngr
                    for gi in range(ngr):
                        c0 = half0 + 2 * gi
                        bsl = bet[:, c0:c0 + 2, 2 * kh:2 * kh + 2].unsqueeze(3).broadcast_to([128, 2, 2, 128])
                        v4 = lambda t: t[:, :].rearrange("p (a b c) -> p a b c", a=2, b=2)
                        R = psf(C); t1 = t1r.next(); dec = decr.next(); eR = eRr.next(); tmp = tmpr.next()
                        P(lambda e, R=R, gi=gi: e.matmul(R[:, :], C.ones32[:, :], gUall[:, 2 * gi:2 * gi + 2, :, :].rearrange("p a b c -> p (a b c)"), start=True, stop=True),
                          [C.ones32, gUall], [R])
                        V(lambda e, R=R, t1=t1, c0=c0: e.tensor_tensor(v4(t1), v4(R), acsA[:, c0:c0 + 2, 0:2].unsqueeze(3).broadcast_to([128, 2, 2, 128]), op=ALU.subtract), [R, acsA], [t1])
                        V(lambda e, t1=t1: e.tensor_scalar(t1[:, :], t1[:, :], 0.0, None, op0=ALU.min), [t1], [t1])
                        A(lambda e, R=R, eR=eR: e.activation(eR[:, :], R[:, :], AF.Exp), [R], [eR])
                        A(lambda e, t1=t1, dec=dec: e.activation(dec[:, :], t1[:, :], AF.Exp), [t1], [dec])
                        G(lambda e, dec=dec: e.tensor_tensor(dec[:, :].rearrange("p (a c) -> p a c", a=4), dec[:, :].rearrange("p (a c) -> p a c", a=4),
                                                             C.U32[:, :].unsqueeze(1).broadcast_to([128, 4, 128]), op=ALU.mult), [dec, C.U32], [dec])
                        pk = C.pf[5]
                        for cl in range(2):
                            cs = slice((c0 + cl) * 128, (c0 + cl + 1) * 128)
                            P(lambda e, cl=cl, cs=cs: e.matmul(pk[:, cl * 256:cl * 256 + 128], KT[:, cs], KT[:, cs], start=True, stop=True), [KT], [pk])
                            P(lambda e, cl=cl, cs=cs: e.matmul(pk[:, cl * 256 + 128:cl * 256 + 256], KT[:, cs], QT[:, cs], start=True, stop=True), [KT, QT], [pk])
                        pk3 = pk[:, :].rearrange("p (a f) -> p a f", a=2)
                        V(lambda e, tmp=tmp, dec=dec, pk3=pk3: e.tensor_tensor(v4(tmp), pk3[:, :, 0:128].unsqueeze(2).broadcast_to([128, 2, 2, 128]), v4(dec), op=ALU.mult), [pk, dec], [tmp])
                        V(lambda e, tmp=tmp, bsl=bsl: e.tensor_tensor(v4(tmp), v4(tmp), bsl, op=ALU.mult), [tmp, bet], [tmp])
                        X = Ybuf[gi][0]
                        G(lambda e, X=X, tmp=tmp: e.tensor_tensor(X[:, :, :], tmp[:, :].rearrange("p (a c) -> p a c", a=4), C.SU32[:, :].unsqueeze(1).broadcast_to([128, 4, 128]), op=ALU.mult),
                          [tmp, C.SU32], [X])
                        V(lambda e, dec=dec, pk3=pk3, c0=c0: e.tensor_tensor(qkT[:, c0:c0 + 2, :, :], pk3[:, :, 128:256].unsqueeze(2).broadcast_to([128, 2, 2, 128]), v4(dec), op=ALU.mult), [pk, dec], [qkT])
                        G(lambda e, eR=eR, c0=c0: e.tensor_tensor(QdT[:, c0:c0 + 2, :, :], QT[:, c0 * 128:(c0 + 2) * 128].rearrange("p (a c) -> p a c", a=2).unsqueeze(2).broadcast_to([128, 2, 2, 128]),
                                                                 v4(eR), op=ALU.mult), [QT, eR], [QdT])
                        kb4 = K_tm[:, c0:c0 + 2, :].unsqueeze(2).broadcast_to([128, 2, 2, 128])
                        G(lambda e, gi=gi, c0=c0, kb4=kb4: e.tensor_tensor(kegA[gi][:, :, :, :], kb4, ecolA[:, c0:c0 + 2, 0:2].unsqueeze(3).broadcast_to([128, 2, 2, 128]), op=ALU.mult), [K_tm, ecolA], [kegA[gi]])
                        G(lambda e, c0=c0, kb4=kb4: e.tensor_tensor(kdec[:, c0:c0 + 2, :, :], kb4, ecolA[:, c0:c0 + 2, 2:4].unsqueeze(3).broadcast_to([128, 2, 2, 128]), op=ALU.mult), [K_tm, ecolA], [kdec])
                        pp = psb(C)
                        for p4 in range(4):
                            P(lambda e, pp=pp, X=X, p4=p4: e.transpose(pp[:, p4 * 128:(p4 + 1) * 128], X[:, p4, :], C.identb[:, :]), [X, C.identb], [pp])
                        W = Wbuf[gi][0]
                        copy_op(k, evac_eng(C), W[:, :, :], pp[:, 0:512].rearrange("p (a c) -> p a c", a=4), [pp], [W])
                        Tt = Tbuf[gi][0]
                        V(lambda e, Tt=Tt, X=X: e.tensor_tensor(Tt[:, :, :], C.identb[:, :].unsqueeze(1).broadcast_to([128, 4, 128]), X[:, :, :], op=ALU.subtract), [C.identb, X], [Tt])
                        Yc[gi], Wc[gi], Tc[gi] = X, W, Tt
                    for lvl in range(1, 7):
                        nb_ = lvl % 2
                        for gi in range(ngr):
                            Y, W = Yc[gi], Wc[gi]
                            pw = psf(C)
                            for p4 in range(4):
                                P(lambda e, pw=pw, Y=Y, W=W, p4=p4: e.matmul(pw[:, p4 * 128:(p4 + 1) * 128], Y[:, p4, :], W[:, p4, :], start=True, stop=True), [Y, W], [pw])
                            if lvl < 6:
                                py = psf(C)
                                for p4 in range(4):
                                    P(lambda e, py=py, Y=Y, W=W, p4=p4: e.matmul(py[:, p4 * 128:(p4 + 1) * 128], W[:, p4, :], Y[:, p4, :], start=True, stop=True), [Y, W], [py])
                            W2 = Wbuf[gi][nb_]
                            copy_op(k, "act", W2[:, :, :], pw[:, :].rearrange("p (a c) -> p a c", a=4), [pw], [W2])
                            if lvl < 6:
                                Y2 = Ybuf[gi][nb_]
                                copy_op(k, "dve", Y2[:, :, :], py[:, :].rearrange("p (a c) -> p a c", a=4), [py], [Y2])
                                Yc[gi] = Y2
                            Wc[gi] = W2
                        for gi in range(ngr):
                            W, Tt = Wc[gi], Tc[gi]
                            pt_ = psf(C)
                            for p4 in range(4):
                                P(lambda e, pt_=pt_, W=W, Tt=Tt, p4=p4: e.matmul(pt_[:, p4 * 128:(p4 + 1) * 128], W[:, p4, :], Tt[:, p4, :], start=True, stop=False), [W, Tt], [pt_])
                                P(lambda e, pt_=pt_, Tt=Tt, p4=p4: e.matmul(pt_[:, p4 * 128:(p4 + 1) * 128], C.identb[:, :], Tt[:, p4, :], start=False, stop=True), [C.identb, Tt], [pt_])
                            Tt2 = Tbuf[gi][nb_]
                            copy_op(k, evac_eng(C), Tt2[:, :, :], pt_[:, :].rearrange("p (a c) -> p a c", a=4), [pt_], [Tt2])
                            Tc[gi] = Tt2
                    for gi in range(ngr):
                        c0 = half0 + 2 * gi
                        Tt = Tc[gi]
                        pu = psf(C); pw_ = psf(C)
                        for cl in range(2):
                            for hv in range(2):
                                p4 = cl * 2 + hv
                                P(lambda e, pu=pu, Tt=Tt, p4=p4, cl=cl, hv=hv, c0=c0: e.matmul(pu[:, p4 * 128:(p4 + 1) * 128], Tt[:, p4, :], V_tm[:, c0 + cl, hv * 128:(hv + 1) * 128], start=True, stop=True), [Tt, V_tm], [pu])
                                P(lambda e, pw_=pw_, Tt=Tt, p4=p4, cl=cl, hv=hv, gi=gi: e.matmul(pw_[:, p4 * 128:(p4 + 1) * 128], kegA[gi][:, cl, hv, :], Tt[:, p4, :], start=True, stop=True), [Tt, kegA[gi]], [pw_])
                        V(lambda e, pu=pu, c0=c0, kh=kh: e.tensor_tensor(u0b[:, c0:c0 + 2, :, :], pu[:, :].rearrange("p (a b c) -> p a b c", a=2, b=2),
                                                                      bet[:, c0:c0 + 2, 2 * kh:2 * kh + 2].unsqueeze(3).broadcast_to([128, 2, 2, 128]), op=ALU.mult), [pu, bet], [u0b])
                        copy_op(k, "act", w0T[:, c0:c0 + 2, :, :], pw_[:, :].rearrange("p (a b c) -> p a b c", a=2, b=2), [pw_], [w0T])
                for hv in range(2):
                    G(lambda e, hv=hv: e.memset(S32[hv][:, :], 0.0), [], [S32[hv]])
                    G(lambda e, hv=hv: e.memset(Sb[hv][:, :], 0.0), [], [Sb[hv]])
                pend_out = []

                def emit_post(c, hv, sq, ocp):
                    cs = slice(c * 128, (c + 1) * 128)
                    rs = ors.next(); tm_ = otm.next(); pn = psf(C)
                    P(lambda e, pn=pn, sq=sq: e.matmul(pn[:, 0:128], C.onesb[:, :], sq[:, :], start=True, stop=True), [C.onesb, sq], [pn])
                    A(lambda e, pn=pn, rs=rs: e.activation(rs[:, :], pn[:, 0:128], AF.Sqrt, bias=EPS, scale=1.0 / 128), [pn], [rs])
                    V(lambda e, rs=rs: e.reciprocal(rs[:, :], rs[:, :]), [rs], [rs])
                    V(lambda e, tm_=tm_, ocp=ocp, rs=rs: e.scalar_tensor_tensor(tm_[:, :], ocp[:, :], nwc[:, 0:1], rs[:, :], op0=ALU.mult, op1=ALU.mult), [ocp, nwc, rs], [tm_])
                    G(lambda e, tm_=tm_, hv=hv, cs=cs: e.tensor_tensor(outT[hv][:, cs], tm_[:, :], szT[hv][:, cs], op=ALU.mult), [tm_, szT[hv]], [outT[hv]])

                for c in range((nck or NCK) if 2 in phases else 0):
                    cur_out = []
                    for hv in range(2):
                        h = 2 * kh + hv
                        p1 = psf(C); vn = vnr.next()
                        P(lambda e, p1=p1, c=c, hv=hv: e.matmul(p1[:, 0:128], w0T[:, c, hv, :], Sb[hv][:, :], start=True, stop=True), [w0T, Sb[hv]], [p1])
                        V(lambda e, p1=p1, vn=vn, c=c, hv=hv, h=h: e.scalar_tensor_tensor(vn[:, :], p1[:, 0:128], nbet[:, c, h:h + 1], u0b[:, c, hv, :], op0=ALU.mult, op1=ALU.add),
                          [p1, nbet, u0b], [vn])
                        po = psf(C)
                        P(lambda e, po=po, c=c, hv=hv: e.matmul(po[:, 0:128], Sb[hv][:, :], QdT[:, c, hv, :], start=True, stop=False), [Sb[hv], QdT], [po])
                        P(lambda e, po=po, c=c, hv=hv, vn=vn: e.matmul(po[:, 0:128], vn[:, :], qkT[:, c, hv, :], start=False, stop=True), [vn, qkT], [po])
                        p2 = psf(C)
                        P(lambda e, p2=p2, c=c, hv=hv, vn=vn: e.matmul(p2[:, 0:128], kdec[:, c, hv, :], vn[:, :], start=True, stop=True), [kdec, vn], [p2])
                        V(lambda e, p2=p2, c=c, hv=hv: e.scalar_tensor_tensor(S32[hv][:, :], S32[hv][:, :], egl[:, c, hv:hv + 1], p2[:, 0:128], op0=ALU.mult, op1=ALU.add),
                          [S32[hv], egl, p2], [S32[hv]])
                        G(lambda e, hv=hv: e.tensor_copy(Sb[hv][:, :], S32[hv][:, :]), [S32[hv]], [Sb[hv]])
                        sq = osq.next(); ocp = ocpr.next()
                        A(lambda e, sq=sq, po=po: e.activation(sq[:, :], po[:, 0:128], AF.Square), [po], [sq])
                        A(lambda e, ocp=ocp, po=po: e.copy(ocp[:, :], po[:, 0:128]), [po], [ocp])
                        cur_out.append((c, hv, sq, ocp))
                    for args in pend_out:
                        emit_post(*args)
                    pend_out = cur_out
                for args in pend_out:
                    emit_post(*args)
                for hv in range(2):
                    h = 2 * kh + hv
                    k.dma("sp", [(scr.mixT[h * 128:(h + 1) * 128, t0:t0 + S], outT[hv][:, :])], reads=[outT[hv]], sembuf=outT[hv])
        C.pfn = 6
```

```python
from contextlib import ExitStack
import numpy as np
import os
GCUT = int(os.environ.get('GCUT', '99'))
import concourse.bass as bass
import concourse.mybir as mybir
from concourse.bass_utils import run_bass_kernel_spmd

F32 = mybir.dt.float32
BF16 = mybir.dt.bfloat16
AF = mybir.ActivationFunctionType
ALU = mybir.AluOpType
AX = mybir.AxisListType

ENGS = ("pe", "act", "dve", "pool", "sp")
EPOCH = 12000


class Buf:
    def __init__(self, k, name, t):
        self.k = k
        self.name = name
        self.t = t
        self.w = None
        self.r = []
        self.dsem = None
        self.dcnt = 0
        self.psum = False

    def __getitem__(self, idx):
        return self.t[idx]


class K:
    def __init__(self, nc, es):
        self.nc = nc
        self.es = es
        self.ops = {e: [] for e in ENGS}
        self.sems = {}
        self.ecnt = {e: 0 for e in ENGS}
        self.waited = {e: {} for e in ENGS}
        self.stack = [es]
        self.nbuf = 0
        self.ninstr = 0

    def sb(self, name, shape, dt=F32):
        self.uid = getattr(self, "uid", 0) + 1
        name = "%s_u%d" % (name, self.uid)
        t = self.es.enter_context(self.nc.sbuf_tensor(name, list(shape), dt))
        return Buf(self, name, t)

    def ps(self, name, shape, dt=F32):
        t = self.es.enter_context(self.nc.psum_tensor(name, list(shape), dt))
        b = Buf(self, name, t)
        b.psum = True
        return b

    def dram(self, name, shape, dt=F32, kind=None):
        if kind is None:
            t = self.nc.dram_tensor(name, list(shape), dt)
        else:
            t = self.nc.dram_tensor(name, list(shape), dt, kind=kind)
        return Buf(self, name, t.ap())

    def region(self, name):
        return Buf(self, name, None)

    def _dsem(self, b):
        if b.dsem is None:
            key = "d_" + b.name + "_%d" % self.nbuf
            self.nbuf += 1
            self.sems[key] = self.es.enter_context(self.nc.semaphore(key[:40]))
            b.dsem = key
        return b.dsem

    def _deps(self, eng, reads, writes):
        need = {}

        def add(ev, is_war=False):
            if ev is None:
                return
            key, val = ev
            if key.split("#")[0] == eng:
                if eng in ("pe", "sp") or is_war:
                    return
            if need.get(key, 0) < val:
                need[key] = val

        for b in reads:
            add(b.w)
            if b.psum:
                for ev in b.r:
                    if ev[0].split("#")[0] != eng:
                        add(ev)
        for b in writes:
            add(b.w, is_war=True)
            for ev in b.r:
                add(ev, is_war=True)
        out = []
        wd = self.waited[eng]
        for key, val in need.items():
            if wd.get(key, 0) >= val:
                continue
            wd[key] = val
            out.append((key, val))
        return out

    def _commit(self, ev, reads, writes):
        for b in reads:
            b.r.append(ev)
            if len(b.r) > 64:
                m = {}
                for kk, vv in b.r:
                    if m.get(kk, 0) < vv:
                        m[kk] = vv
                b.r = list(m.items())
        for b in writes:
            b.w = ev
            b.r = []

    def op(self, eng, fn, reads=(), writes=()):
        waits = self._deps(eng, reads, writes)
        self.ecnt[eng] += 1
        ep = (self.ecnt[eng] - 1) // EPOCH
        val = self.ecnt[eng] - ep * EPOCH
        ekey = "%s#%d" % (eng, ep)
        if ekey not in self.sems:
            self.sems[ekey] = self.stack[0].enter_context(self.nc.semaphore("es_%s_%d" % (eng, ep)))
        sems = self.sems
        wl = [(sems[k], v) for k, v in waits]
        mysem = sems[ekey]

        def emit(e, fn=fn, wl=wl, mysem=mysem):
            for s, v in wl:
                e.wait_ge(s, v)
            fn(e).then_inc(mysem, 1)

        self.ops[eng].append(emit)
        self._commit((ekey, val), reads, writes)
        self.ninstr += 1

    def dma(self, q, pairs, reads=(), writes=(), sembuf=None):
        assert sembuf is not None
        key = self._dsem(sembuf)
        waits = self._deps(q, reads, writes)
        sems = self.sems
        wl = [(sems[k], v) for k, v in waits]
        sembuf.dcnt += 16 * len(pairs)
        val = sembuf.dcnt
        dsem = sems[key]

        def emit(e, pairs=pairs, wl=wl, dsem=dsem):
            for s, v in wl:
                e.wait_ge(s, v)
            for o, i in pairs:
                e.dma_start(out=o, in_=i).then_inc(dsem, 16)

        self.ops[q].append(emit)
        self._commit((key, val), reads, writes)
        self.ninstr += len(pairs)

    def finish(self, final_bufs):
        nc = self.nc
        finals = []
        for b in final_bufs:
            if b.w is not None:
                finals.append(b.w)
        sems = self.sems

        def fin(e):
            for key, val in finals:
                e.wait_ge(sems[key], val)

        self.ops["sp"].append(fin)
        with nc.Block() as block:
            @block.tensor
            def _(e):
                for f in self.ops["pe"]:
                    f(e)

            @block.scalar
            def _(e):
                for f in self.ops["act"]:
                    f(e)

            @block.vector
            def _(e):
                for f in self.ops["dve"]:
                    f(e)

            @block.gpsimd
            def _(e):
                for f in self.ops["pool"]:
                    f(e)

            @block.sync
            def _(e):
                for f in self.ops["sp"]:
                    f(e)

    def barrier(self):
        targets = []
        for e in ("pe", "act", "dve", "pool"):
            if self.ecnt[e] > 0:
                ep = (self.ecnt[e] - 1) // EPOCH
                targets.append(("%s#%d" % (e, ep), self.ecnt[e] - ep * EPOCH))
        targets += [(key, cnt) for key, cnt in self.dsem_cnt.items() if cnt > 0]
        sems = self.sems
        for eng in ENGS:
            wd = self.waited[eng]
            wl = []
            for key, val in targets:
                if key.split("#")[0] == eng or wd.get(key, 0) >= val:
                    continue
                wd[key] = val
                wl.append((sems[key], val))

            def emit(e, wl=wl):
                for s, v in wl:
                    e.wait_ge(s, v)

            if wl:
                self.ops[eng].append(emit)


def _k_init_extra(self):
    self.dsem_cnt = {}
    self.free_dsems = []
    self.stack = [self.es]
    self._psf = []
    self._psf_i = 0


def _k_dsem(self, b):
    if b.dsem is None:
        if self.free_dsems:
            key = self.free_dsems.pop()
        else:
            key = "d%d" % self.nbuf
            self.nbuf += 1
            self.sems[key] = self.stack[0].enter_context(self.nc.semaphore(key))
            self.dsem_cnt[key] = 0
        b.dsem = key
        b.dcnt = self.dsem_cnt[key]
        self.cur_dsems.append(key)
    return b.dsem


K._dsem = _k_dsem


def _k_dma(self, q, pairs, reads=(), writes=(), sembuf=None, **kw):
    assert sembuf is not None
    key = self._dsem(sembuf)
    waits = self._deps(q, reads, writes)
    sems = self.sems
    wl = [(sems[k_], v) for k_, v in waits]
    sembuf.dcnt += 16 * len(pairs)
    val = sembuf.dcnt
    self.dsem_cnt[key] = val
    dsem = sems[key]

    def emit(e, pairs=pairs, wl=wl, dsem=dsem, kw=kw):
        for s, v in wl:
            e.wait_ge(s, v)
        for o, i in pairs:
            e.dma_start(out=o, in_=i, **kw).then_inc(dsem, 16)

    self.ops[q].append(emit)
    self._commit((key, val), reads, writes)
    self.ninstr += len(pairs)


K.dma = _k_dma


class Stage:
    def __init__(self, k):
        self.k = k

    def __enter__(self):
        k = self.k
        self.es = ExitStack()
        self.prev = k.es
        k.es = self.es
        self.prev_dsems = getattr(k, "cur_dsems", [])
        k.cur_dsems = []
        return self

    def __exit__(self, *a):
        k = self.k
        k.barrier()
        k.free_dsems.extend(k.cur_dsems)
        k.cur_dsems = self.prev_dsems
        k.es = self.prev
        self.es.close()
        return False


EPS = 1e-6
S = 2048
NSEQ = 2
NTOK = NSEQ * S
D = 1024
DFF = 2816
NEG = -30000.0

WSHAPES = {
    "norm_mix": [2, 1024], "norm_ffn": [2, 1024], "norm_ple": [2, 1024],
    "w_in_even": [1, 1024, 4112], "moba_q_norm": [1, 64], "moba_k_norm": [1, 64],
    "ssm_conv_w": [1, 4, 1536], "ssm_conv_b": [1, 1536], "ssm_dt_bias": [1, 16],
    "ssm_a_log": [1, 16], "ssm_d": [1, 16], "ssm_norm": [1, 1024],
    "w_out_even": [1, 1536, 1024], "w_in_odd": [1, 1024, 6176], "gdn_conv_w": [1, 4, 4096],
    "gdn_dt_bias": [1, 16], "gdn_a_log": [1, 16], "gdn_norm": [1, 128],
    "w_out_odd": [1, 2048, 1024], "ffn_w_gate": [2, 1024, 2816], "ffn_w_up": [2, 1024, 2816],
    "ffn_conv_w": [2, 3, 2816], "ffn_conv_b": [2, 2816], "ffn_w_down": [2, 2816, 1024],
    "ple_w_proj": [2, 256, 1024], "ple_w_gate": [2, 1024, 1024],
}


class Ctx:
    pass


def mk_consts(k, C):
    C.identf = k.sb("identf", [128, 128], F32)
    C.identb = k.sb("identb", [128, 128], BF16)
    C.ones32 = k.sb("ones32", [128, 128], F32)
    C.onesb = k.sb("onesb", [128, 128], BF16)
    C.U32 = k.sb("U32", [128, 128], F32)
    C.Ub = k.sb("Ub", [128, 128], BF16)
    C.SU32 = k.sb("SU32", [128, 128], F32)
    C.tribias = k.sb("tribias", [128, 128], BF16)
    C.blk1 = k.sb("blk1", [128, 128], BF16)
    tb32 = k.sb("tb32", [128, 128], F32)
    G = lambda fn, r, w: k.op("pool", fn, reads=r, writes=w)
    V = lambda fn, r, w: k.op("dve", fn, reads=r, writes=w)
    G(lambda e: e.memset(C.ones32[:, :], 1.0), [], [C.ones32])
    G(lambda e: e.affine_select(C.identf[:, :], C.ones32[:, :], pattern=[[-1, 128]], compare_op=ALU.is_equal,
                                fill=0.0, base=0, channel_multiplier=1), [C.ones32], [C.identf])
    G(lambda e: e.affine_select(C.U32[:, :], C.ones32[:, :], pattern=[[1, 128]], compare_op=ALU.is_ge,
                                fill=0.0, base=0, channel_multiplier=-1), [C.ones32], [C.U32])
    G(lambda e: e.affine_select(C.SU32[:, :], C.ones32[:, :], pattern=[[1, 128]], compare_op=ALU.is_gt,
                                fill=0.0, base=0, channel_multiplier=-1), [C.ones32], [C.SU32])
    V(lambda e: e.tensor_scalar(tb32[:, :], C.U32[:, :], -1.0, -NEG, op0=ALU.add, op1=ALU.mult), [C.U32], [tb32])
    V(lambda e: e.tensor_copy(C.tribias[:, :], tb32[:, :]), [tb32], [C.tribias])
    V(lambda e: e.tensor_copy(C.identb[:, :], C.identf[:, :]), [C.identf], [C.identb])
    V(lambda e: e.tensor_copy(C.onesb[:, :], C.ones32[:, :]), [C.ones32], [C.onesb])
    V(lambda e: e.tensor_copy(C.Ub[:, :], C.U32[:, :]), [C.U32], [C.Ub])
    G(lambda e: e.memset(C.blk1[:, :], 0.0), [], [C.blk1])
    G(lambda e: e.memset(C.blk1[0:64, 0:64], 1.0), [], [C.blk1])
    G(lambda e: e.memset(C.blk1[64:128, 64:128], 1.0), [], [C.blk1])
    C.pf = [k.ps("pf%d" % i, [128, 512], F32) for i in range(6)]
    C.pb = [k.ps("pb%d" % i, [128, 1024], BF16) for i in range(2)]
    C.pfi = 0
    C.pfn = 6
    C.pbi = 0
    C.evi = 0


def psf(C):
    C.pfi = (C.pfi + 1) % C.pfn
    return C.pf[C.pfi]


def psb(C):
    C.pbi = (C.pbi + 1) % len(C.pb)
    return C.pb[C.pbi]


class Ring:
    def __init__(self, bufs):
        self.bufs = bufs
        self.i = -1

    def next(self):
        self.i = (self.i + 1) % len(self.bufs)
        return self.bufs[self.i]


def evac_eng(C):
    C.evi += 1
    return "act" if (C.evi % 2) else "dve"


def copy_op(k, eng, out_ap, in_ap, reads, writes):
    if eng == "act":
        k.op("act", lambda e: e.copy(out_ap, in_ap), reads=reads, writes=writes)
    else:
        k.op(eng, lambda e: e.tensor_copy(out_ap, in_ap), reads=reads, writes=writes)


def load_w(k, Wd2, row0, nk, col0, ncols, wt):
    pairs = [(wt[:, kc, 0:ncols], Wd2[row0 + kc * 128: row0 + (kc + 1) * 128, col0:col0 + ncols]) for kc in range(nk)]
    k.dma("pool", pairs, writes=[wt], sembuf=wt)


def load_cols(k, C, dst_fn, rows_ap2, K, nch):
    rw = k.sb("rowsbuf", [K, nch * 128], F32)
    k.dma("sp", [(rw[:, :], rows_ap2)], writes=[rw], sembuf=rw)
    for c in range(nch):
        ps = psf(C)
        k.op("pe", lambda e, ps=ps, c=c: e.transpose(ps[:, 0:K], rw[0:K, c * 128:(c + 1) * 128], C.identf[0:K, 0:K]),
             reads=[rw, C.identf], writes=[ps])
        k.op("dve", lambda e, ps=ps, c=c: e.tensor_copy(dst_fn(c), ps[:, 0:K]), reads=[ps], writes=[])


def norm_to_hT(k, C, src_d, tok0, ntok, gain_ap, hT, hcol0=0):
    gbc = k.sb("gbc", [128, D], F32)
    k.dma("sp", [(gbc[:, :], gain_ap.partition_broadcast(128))], writes=[gbc], sembuf=gbc)
    xr = Ring([k.sb("nx%d" % i, [128, D], F32) for i in range(4)])
    junk = k.sb("njunk", [128, D], BF16)
    hbr = Ring([k.sb("nhb%d" % i, [128, D], BF16) for i in range(3)])
    ssr = Ring([k.sb("nss%d" % i, [128, 2], F32) for i in range(4)])
    def emit_tr(hb, c0):
        pt = psb(C)
        for kc in range(8):
            k.op("pe", lambda e, kc=kc, hb=hb, pt=pt: e.transpose(pt[:, kc * 128:(kc + 1) * 128], hb[:, kc * 128:(kc + 1) * 128], C.identb[:, :]),
                 reads=[hb, C.identb], writes=[pt])
        hb_ = hT[c0 // 512]
        copy_op(k, evac_eng(C), hb_[:, :, c0 % 512:c0 % 512 + 128], pt[:, :].rearrange("p (a b) -> p a b", a=8), [pt], [hb_])

    pend = None
    for tt in range(ntok // 128):
        xt = xr.next(); hb = hbr.next(); ss = ssr.next()
        r0 = tok0 + tt * 128
        k.dma("sp" if tt % 2 == 0 else "pool", [(xt[:, :], src_d[r0:r0 + 128, :])], writes=[xt], sembuf=xt)
        k.op("act", lambda e, ss=ss: e.memzero(ss[:, :]), writes=[ss])
        k.op("act", lambda e, xt=xt, ss=ss: e.activation(junk[:, :], xt[:, :], AF.Square, accum_out=ss[:, 0:1]),
             reads=[xt, ss], writes=[junk, ss])
        k.op("act", lambda e, ss=ss: e.activation(ss[:, 1:2], ss[:, 0:1], AF.Sqrt, bias=EPS, scale=1.0 / D),
             reads=[ss], writes=[ss])
        k.op("dve", lambda e, ss=ss: e.reciprocal(ss[:, 1:2], ss[:, 1:2]),
             reads=[ss], writes=[ss])
        k.op("dve", lambda e, xt=xt, ss=ss, hb=hb: e.scalar_tensor_tensor(hb[:, :], xt[:, :], ss[:, 1:2], gbc[:, :],
                                                                       op0=ALU.mult, op1=ALU.mult),
             reads=[xt, ss, gbc], writes=[hb])
        if pend is not None:
            emit_tr(*pend)
        pend = (hb, hcol0 + tt * 128)
    if pend is not None:
        emit_tr(*pend)


def linear_tm(k, C, lhsT_fn, nk, Wd2, row0, col0, ncols, ntt, evac, wring, cbw=512):
    for cb in range(0, ncols, cbw):
        n = min(cbw, ncols - cb)
        wt = wring.next()
        load_w(k, Wd2, row0, nk, col0 + cb, n, wt)
        for tt in range(ntt):
            ps = psf(C)
            for kc in range(nk):
                lb, lap = lhsT_fn(kc, tt)
                k.op("pe", lambda e, ps=ps, lap=lap, wt=wt, kc=kc, n=n: e.matmul(ps[:, 0:n], lap, wt[:, kc, 0:n], start=(kc == 0), stop=(kc == nk - 1)),
                     reads=[lb, wt], writes=[ps])
            evac(cb, n, tt, ps)


def linear_fm(k, C, rhs_fn, nk, Wd2, row0, col0, ncols, ntb, evac, wring):
    for cb in range(0, ncols, 512):
        n = min(512, ncols - cb)
        wt = wring.next()
        load_w(k, Wd2, row0, nk, col0 + cb, n, wt)
        for cc in range(0, n, 128):
            m = min(128, n - cc)
            for tb in range(ntb):
                ps = psf(C)
                for kc in range(nk):
                    rb, rap = rhs_fn(kc, tb)
                    k.op("pe", lambda e, ps=ps, rap=rap, wt=wt, kc=kc, cc=cc, m=m: e.matmul(ps[0:m, :], wt[:, kc, cc:cc + m], rap, start=(kc == 0), stop=(kc == nk - 1)),
                         reads=[rb, wt], writes=[ps])
                evac(cb + cc, m, tb, ps)


def stage_resid_linear(k, C, aT_d, nk, Wd2, x_src, x_dst):
    with Stage(k):
        wres = k.sb("wres", [128, nk, D], BF16)
        for half in range(2):
            k.dma("pool", [(wres[:, kc, half * 512:(half + 1) * 512], Wd2[kc * 128:(kc + 1) * 128, half * 512:(half + 1) * 512])
                           for kc in range(nk)], writes=[wres], sembuf=wres)
        abr = Ring([k.sb("ab%d" % i, [128, nk, 512], BF16) for i in range(2)])
        xr = Ring([k.sb("ox%d" % i, [128, D], F32) for i in range(3)])
        for tb in range(NTOK // 512):
            ab = abr.next()
            k.dma("sp", [(ab[:, :, :], aT_d[:, tb * 512:(tb + 1) * 512].rearrange("(kc p) t -> p kc t", p=128))],
                  writes=[ab], sembuf=ab)
            for t4 in range(4):
                tt = tb * 4 + t4
                xt = xr.next()
                k.dma("sp", [(xt[:, :], x_src[tt * 128:(tt + 1) * 128, :])], writes=[xt], sembuf=xt)
                for cb in range(2):
                    ps = psf(C)
                    for kc in range(nk):
                        k.op("pe", lambda e, ps=ps, ab=ab, kc=kc, t4=t4, cb=cb: e.matmul(
                            ps[:, :], ab[:, kc, t4 * 128:(t4 + 1) * 128], wres[:, kc, cb * 512:(cb + 1) * 512],
                            start=(kc == 0), stop=(kc == nk - 1)), reads=[ab, wres], writes=[ps])
                    k.op("dve", lambda e, ps=ps, xt=xt, cb=cb: e.tensor_tensor(
                        xt[:, cb * 512:(cb + 1) * 512], xt[:, cb * 512:(cb + 1) * 512], ps[:, :], op=ALU.add),
                        reads=[xt, ps], writes=[xt])
                k.dma("act", [(x_dst[tt * 128:(tt + 1) * 128, :], xt[:, :])], reads=[xt], sembuf=xt)


def stage_ffn_a(k, C, x_src, gain_ap, Wg2, Wu2, convw2, convb1, actT_d):
    with Stage(k):
        hT = [k.sb("hT%d" % i, [128, 8, 512], BF16) for i in range(NTOK // 512)]
        norm_to_hT(k, C, x_src, 0, NTOK, gain_ap, hT)
        cw = k.sb("cw", [128, 22, 3], F32)
        cbias = k.sb("cbias", [128, 22], F32)
        load_cols(k, C, lambda c: cw[:, c, :], convw2, 3, 22)
        load_cols(k, C, lambda c: cbias[:, c:c + 1], convb1.rearrange("(o c) -> o c", o=1), 1, 22)
        k.barrier()
        wgr = Ring([k.sb("wg%d" % i, [128, 8, 512], BF16) for i in range(2)])
        wur = Ring([k.sb("wu%d" % i, [128, 8, 512], BF16) for i in range(2)])
        grr = Ring([k.sb("graw%d" % i, [128, NSEQ, 2 + S], BF16) for i in range(2)])
        for g in grr.bufs:
            k.op("pool", lambda e, g=g: e.memset(g[:, :, 0:2], 0.0), writes=[g])
        dgr = Ring([k.sb("dg%d" % i, [128, 3, 128], BF16) for i in range(2)])
        sgr = Ring([k.sb("sg%d" % i, [128, 512], F32) for i in range(2)])
        str_ = Ring([k.sb("fst%d" % i, [128, 512], BF16) for i in range(3)])
        for cb512 in range(0, DFF, 512):
            n = min(512, DFF - cb512)
            wg = wgr.next(); wu = wur.next()
            load_w(k, Wg2, 0, 8, cb512, n, wg)
            load_w(k, Wu2, 0, 8, cb512, n, wu)
            for cc in range(0, n, 128):
                c = (cb512 + cc) // 128
                g = grr.next(); dg = dgr.next()
                for tap in range(3):
                    k.op("pool", lambda e, dg=dg, tap=tap, c=c: e.tensor_scalar(dg[:, tap, :], C.identb[:, :], cw[:, c, tap:tap + 1], None, op0=ALU.mult),
                         reads=[C.identb, cw], writes=[dg])
                for tb in range(8):
                    ps = psf(C)
                    for kc in range(8):
                        k.op("pe", lambda e, ps=ps, wg=wg, kc=kc, cc=cc, tb=tb: e.matmul(
                            ps[:, :], wg[:, kc, cc:cc + 128], hT[tb][:, kc, :], start=(kc == 0), stop=(kc == 7)),
                            reads=[wg, hT[tb]], writes=[ps])
                    sq, t4 = tb // 4, tb % 4
                    copy_op(k, evac_eng(C), g[:, sq, 2 + t4 * 512: 2 + (t4 + 1) * 512], ps[:, :], [ps], [g])
                for tb in range(8):
                    sq, t4 = tb // 4, tb % 4
                    pu = psf(C)
                    for kc in range(8):
                        k.op("pe", lambda e, pu=pu, wu=wu, kc=kc, cc=cc, tb=tb: e.matmul(
                            pu[:, :], wu[:, kc, cc:cc + 128], hT[tb][:, kc, :], start=(kc == 0), stop=(kc == 7)),
                            reads=[wu, hT[tb]], writes=[pu])
                    pc = psf(C)
                    for tap in range(3):
                        k.op("pe", lambda e, pc=pc, dg=dg, tap=tap, g=g, sq=sq, t4=t4: e.matmul(
                            pc[:, :], dg[:, tap, :], g[:, sq, t4 * 512 + tap: t4 * 512 + tap + 512], start=(tap == 0), stop=(tap == 2)),
                            reads=[dg, g], writes=[pc])
                    sg = sgr.next(); st = str_.next()
                    k.op("act", lambda e, sg=sg, pc=pc, c=c: e.activation(sg[:, :], pc[:, :], AF.Silu, bias=cbias[:, c:c + 1], scale=1.0),
                         reads=[pc, cbias], writes=[sg])
                    k.op("dve", lambda e, st=st, sg=sg, pu=pu: e.tensor_tensor(st[:, :], sg[:, :], pu[:, :], op=ALU.mult),
                         reads=[sg, pu], writes=[st])
                    k.dma("sp", [(actT_d[c * 128:(c + 1) * 128, tb * 512:(tb + 1) * 512], st[:, :])], reads=[st], sembuf=st)


def stage_ple(k, C, x_src, gain_ap, Wgate2, Wproj2, p_d, x_dst):
    with Stage(k):
        hT = [k.sb("hT%d" % i, [128, 8, 512], BF16) for i in range(NTOK // 512)]
        norm_to_hT(k, C, x_src, 0, NTOK, gain_ap, hT)
        pT = k.sb("pT", [128, 2, NTOK], BF16)
        pr = Ring([k.sb("pl%d" % i, [128, 256], F32) for i in range(2)])
        pbr = Ring([k.sb("plb%d" % i, [128, 256], BF16) for i in range(2)])
        for tt in range(NTOK // 128):
            pt_ = pr.next(); pb_ = pbr.next()
            k.dma("sp", [(pt_[:, :], p_d[tt * 128:(tt + 1) * 128, :])], writes=[pt_], sembuf=pt_)
            k.op("pool", lambda e, pt_=pt_, pb_=pb_: e.tensor_copy(pb_[:, :], pt_[:, :]), reads=[pt_], writes=[pb_])
            pp = psb(C)
            for j in range(2):
                k.op("pe", lambda e, pp=pp, pb_=pb_, j=j: e.transpose(pp[:, j * 128:(j + 1) * 128], pb_[:, j * 128:(j + 1) * 128], C.identb[:, :]),
                     reads=[pb_, C.identb], writes=[pp])
            copy_op(k, evac_eng(C), pT[:, :, tt * 128:(tt + 1) * 128], pp[:, 0:256].rearrange("p (a b) -> p a b", a=2), [pp], [pT])
        wgr = Ring([k.sb("wpg%d" % i, [128, 8, 512], BF16) for i in range(2)])
        wpr = Ring([k.sb("wpp%d" % i, [128, 2, 512], BF16) for i in range(2)])
        xr = Ring([k.sb("px%d" % i, [128, 512], F32) for i in range(3)])
        sgr = Ring([k.sb("psg%d" % i, [128, 512], F32) for i in range(2)])
        for cb in range(2):
            wg = wgr.next(); wp = wpr.next()
            load_w(k, Wgate2, 0, 8, cb * 512, 512, wg)
            load_w(k, Wproj2, 0, 2, cb * 512, 512, wp)
            for tt in range(NTOK // 128):
                xt = xr.next(); sg = sgr.next()
                k.dma("sp", [(xt[:, :], x_src[tt * 128:(tt + 1) * 128, cb * 512:(cb + 1) * 512])], writes=[xt], sembuf=xt)
                p1 = psf(C)
                for kc in range(8):
                    k.op("pe", lambda e, p1=p1, wg=wg, kc=kc, tt=tt: e.matmul(
                        p1[:, :], hT[tt // 4][:, kc, (tt % 4) * 128:(tt % 4 + 1) * 128], wg[:, kc, :], start=(kc == 0), stop=(kc == 7)),
                        reads=[hT[tt // 4], wg], writes=[p1])
                p2 = psf(C)
                for kc in range(2):
                    k.op("pe", lambda e, p2=p2, wp=wp, kc=kc, tt=tt: e.matmul(
                        p2[:, :], pT[:, kc, tt * 128:(tt + 1) * 128], wp[:, kc, :], start=(kc == 0), stop=(kc == 1)),
                        reads=[pT, wp], writes=[p2])
                k.op("act", lambda e, sg=sg, p1=p1: e.activation(sg[:, :], p1[:, :], AF.Sigmoid), reads=[p1], writes=[sg])
                k.op("dve", lambda e, sg=sg, p2=p2: e.tensor_tensor(sg[:, :], sg[:, :], p2[:, :], op=ALU.mult), reads=[sg, p2], writes=[sg])
                k.op("pool", lambda e, sg=sg, xt=xt: e.tensor_tensor(xt[:, :], xt[:, :], sg[:, :], op=ALU.add), reads=[sg, xt], writes=[xt])
                k.dma("pool", [(x_dst[tt * 128:(tt + 1) * 128, cb * 512:(cb + 1) * 512], xt[:, :])], reads=[xt], sembuf=xt)


def stage_inproj0(k, C, x_src, gain_ap, Wd2, scr):
    with Stage(k):
        hT = [k.sb("hT%d" % i, [128, 8, 512], BF16) for i in range(NTOK // 512)]
        norm_to_hT(k, C, x_src, 0, NTOK, gain_ap, hT)
        wring = Ring([k.sb("w%d" % i, [128, 8, 512], BF16) for i in range(3)])
        stb = Ring([k.sb("stb%d" % i, [128, 512], BF16) for i in range(4)])
        stf = Ring([k.sb("stf%d" % i, [128, 512], F32) for i in range(3)])
        rhs_fn = lambda kc, tb: (hT[tb], hT[tb][:, kc, :])
        lhs_fn = lambda kc, tt: (hT[tt // 4], hT[tt // 4][:, kc, (tt % 4) * 128:(tt % 4 + 1) * 128])

        def ev_fm(dst, r0):
            def ev(c, m, tb, ps):
                st = stb.next()
                copy_op(k, evac_eng(C), st[0:m, :], ps[0:m, :], [ps], [st])
                k.dma("sp", [(dst[r0 + c:r0 + c + m, tb * 512:(tb + 1) * 512], st[0:m, :])], reads=[st], sembuf=st)
            return ev

        def ev_tm(dst, c0, ring):
            def ev(cb, n, tt, ps):
                st = ring.next()
                copy_op(k, evac_eng(C), st[:, 0:n], ps[:, 0:n], [ps], [st])
                k.dma("sp", [(dst[tt * 128:(tt + 1) * 128, c0 + cb:c0 + cb + n], st[:, 0:n])], reads=[st], sembuf=st)
            return ev

        linear_fm(k, C, rhs_fn, 8, Wd2, 0, 0, 1024, NTOK // 512, ev_fm(scr.qkT, 0), wring)
        linear_tm(k, C, lhs_fn, 8, Wd2, 0, 1024, 512, NTOK // 128, ev_tm(scr.v, 0, stb), wring)
        linear_tm(k, C, lhs_fn, 8, Wd2, 0, 1536, 1024, NTOK // 128, ev_tm(scr.z, 0, stf), wring)
        linear_fm(k, C, rhs_fn, 8, Wd2, 0, 2560, 1536, NTOK // 512, ev_fm(scr.xbcT, 0), wring)
        linear_tm(k, C, lhs_fn, 8, Wd2, 0, 4096, 16, NTOK // 128, ev_tm(scr.dt, 0, stf), wring)


def load_col(k, dst, ap1, n, reps, scale=None):
    for r in range(reps):
        k.dma("sp", [(dst[r * n:(r + 1) * n, 0:1], ap1.rearrange("(p o) -> p o", o=1))], writes=[dst], sembuf=dst)
    if scale is not None:
        k.op("dve", lambda e: e.tensor_scalar(dst[:, 0:1], dst[:, 0:1], scale, None, op0=ALU.mult), reads=[dst], writes=[dst])


def stage_attn(k, C, scr, gq_ap, gk_ap, etab_d, auxc_d):
    with Stage(k):
        C.pfn = 4
        o_ps, d_ps = C.pf[4], C.pf[5]
        E = k.sb("E", [68, 8 * 16 * 128], BF16)
        for i in range(8):
            k.dma("pool", [(E[:, i * 2048:(i + 1) * 2048], etab_d[:, i * 2048:(i + 1) * 2048])], writes=[E], sembuf=E)
        gq = k.sb("gq", [128, 1], F32)
        gk = k.sb("gk", [128, 1], F32)
        load_col(k, gq, gq_ap, 64, 2, scale=0.125)
        load_col(k, gk, gk_ap, 64, 2)
        auxT = k.sb("auxT", [68, S], BF16)
        k.op("pool", lambda e: e.memset(auxT[0:64, :], 0.0), writes=[auxT])
        k.dma("pool", [(auxT[64:68, :], auxc_d[:, :])], writes=[auxT], sembuf=auxT)
        qn = [k.sb("qn%d" % c, [128, S], BF16) for c in range(4)]
        kn = [k.sb("kn%d" % c, [128, S], BF16) for c in range(4)]
        rawr = Ring([k.sb("araw%d" % i, [128, S], BF16) for i in range(2)])
        sqr = Ring([k.sb("asq%d" % i, [128, S], BF16) for i in range(2)])
        rsr = Ring([k.sb("ars%d" % i, [128, 512], F32) for i in range(2)])
        kb32 = k.sb("kb32", [128, 4, 8], F32)
        kbhi = k.sb("kbhi", [128, 4, 8], BF16)
        kblo = k.sb("kblo", [128, 4, 8], BF16)
        kbl32 = k.sb("kbl32", [128, 4, 8], F32)
        gs = k.sb("gs", [128, 8, 8], F32)
        cmp_ = k.sb("cmp", [128, 8, 8, 8], F32)
        cnt = k.sb("cnt", [128, 8, 8], F32)
        mbr = Ring([k.sb("mb%d" % i, [128, 64], BF16) for i in range(2)])
        v_sb = k.sb("v_sb", [128, 16, 512], BF16)
        ptr = Ring([k.sb("pt%d" % i, [128, 512], BF16) for i in range(3)])
        rec = k.sb("rec", [64, 512], F32)
        osb = Ring([k.sb("osb%d" % i, [64, 512], BF16) for i in range(2)])
        for s in range(NSEQ):
            t0 = s * S
            for c8 in range(8):
                isq = c8 < 4
                dst = qn[c8] if isq else kn[c8 - 4]
                gcol = gq if isq else gk
                raw = rawr.next(); sq = sqr.next()
                k.dma("sp", [(raw[:, :], scr.qkT[c8 * 128:(c8 + 1) * 128, t0:t0 + S])], writes=[raw], sembuf=raw)
                k.op("act", lambda e, sq=sq, raw=raw: e.activation(sq[:, :], raw[:, :], AF.Square), reads=[raw], writes=[sq])
                for tb in range(4):
                    ps = psf(C); rs = rsr.next()
                    k.op("pe", lambda e, ps=ps, sq=sq, tb=tb: e.matmul(ps[:, :], C.blk1[:, :], sq[:, tb * 512:(tb + 1) * 512], start=True, stop=True),
                         reads=[C.blk1, sq], writes=[ps])
                    k.op("act", lambda e, ps=ps, rs=rs: e.activation(rs[:, :], ps[:, :], AF.Sqrt, bias=EPS, scale=1.0 / 64), reads=[ps], writes=[rs])
                    k.op("dve", lambda e, rs=rs: e.reciprocal(rs[:, :], rs[:, :]), reads=[rs], writes=[rs])
                    k.op("dve", lambda e, dst=dst, raw=raw, gcol=gcol, rs=rs, tb=tb: e.scalar_tensor_tensor(
                        dst[:, tb * 512:(tb + 1) * 512], raw[:, tb * 512:(tb + 1) * 512], gcol[:, 0:1], rs[:, :], op0=ALU.mult, op1=ALU.mult),
                        reads=[raw, gcol, rs], writes=[dst])
            for c in range(4):
                k.op("dve", lambda e, c=c: e.tensor_reduce(kb32[:, c, :], kn[c][:, :].rearrange("p (n j) -> p n j", j=256), axis=AX.X, op=ALU.add),
                     reads=[kn[c]], writes=[kb32])
            k.op("dve", lambda e: e.tensor_scalar(kb32[:, :, :], kb32[:, :, :], 1.0 / 256, None, op0=ALU.mult), reads=[kb32], writes=[kb32])
            k.op("dve", lambda e: e.tensor_copy(kbhi[:, :, :], kb32[:, :, :]), reads=[kb32], writes=[kbhi])
            k.op("dve", lambda e: e.tensor_tensor(kbl32[:, :, :], kb32[:, :, :], kbhi[:, :, :], op=ALU.subtract), reads=[kb32, kbhi], writes=[kbl32])
            k.op("dve", lambda e: e.tensor_copy(kblo[:, :, :], kbl32[:, :, :]), reads=[kbl32], writes=[kblo])
            for qt in range(8, 16):
                nb = qt // 2
                for par in range(2):
                    pg = psf(C)
                    pb_ = par * 64
                    for i4 in range(4):
                        c = i4
                        k.op("pe", lambda e, pg=pg, i4=i4, c=c, pb_=pb_, qt=qt: e.matmul(
                            pg[:, i4 * 8:(i4 + 1) * 8], qn[c][pb_:pb_ + 64, qt * 128:(qt + 1) * 128], kbhi[pb_:pb_ + 64, c, :], start=True, stop=False),
                            reads=[qn[c], kbhi], writes=[pg])
                        k.op("pe", lambda e, pg=pg, i4=i4, c=c, pb_=pb_, qt=qt: e.matmul(
                            pg[:, i4 * 8:(i4 + 1) * 8], qn[c][pb_:pb_ + 64, qt * 128:(qt + 1) * 128], kblo[pb_:pb_ + 64, c, :], start=False, stop=True),
                            reads=[qn[c], kblo], writes=[pg])
                    k.op("dve", lambda e, pg=pg, par=par: e.tensor_copy(gs[:, par * 4:(par + 1) * 4, :], pg[:, 0:32].rearrange("p (h n) -> p h n", h=4)), reads=[pg], writes=[gs])
                k.op("dve", lambda e, nb=nb: e.tensor_tensor(
                    cmp_[:, :, 0:nb, 0:nb], gs[:, :, 0:nb].unsqueeze(2).broadcast_to([128, 8, nb, nb]),
                    gs[:, :, 0:nb].unsqueeze(3).broadcast_to([128, 8, nb, nb]), op=ALU.is_gt), reads=[gs], writes=[cmp_])
                k.op("dve", lambda e, nb=nb: e.tensor_reduce(cnt[:, :, 0:nb], cmp_[:, :, 0:nb, 0:nb], axis=AX.X, op=ALU.add), reads=[cmp_], writes=[cnt])
                mb = mbr.next()
                k.op("pool", lambda e, mb=mb: e.memset(mb[:, :], 0.0), writes=[mb])
                k.op("dve", lambda e, mb=mb, nb=nb: e.tensor_scalar(
                    mb[:, :].rearrange("p (h n) -> p h n", h=8)[:, :, 0:nb], cnt[:, :, 0:nb], 3.0, NEG, op0=ALU.is_ge, op1=ALU.mult),
                    reads=[cnt, mb], writes=[mb])
                pp = psb(C)
                k.op("pe", lambda e, pp=pp, mb=mb: e.transpose(pp[0:64, 0:128], mb[:, :], C.identb[:, :]), reads=[mb, C.identb], writes=[pp])
                copy_op(k, evac_eng(C), auxT[0:64, qt * 128:(qt + 1) * 128], pp[0:64, 0:128], [pp], [auxT])
            if getattr(C, "dbg_aux", None) is not None and s == 0:
                k.dma("sp", [(C.dbg_aux[:, :], auxT[:, :])], reads=[auxT], sembuf=auxT)
            k.dma("sp", [(v_sb[:, :, :], scr.v[t0:t0 + S, :].rearrange("(kt p) c -> p kt c", p=128))], writes=[v_sb], sembuf=v_sb)
            def emit_pv(pt, kt, h, j0, n, nkt):
                k.op("pe", lambda e: e.matmul(o_ps[0:64, j0:512], v_sb[:, kt, h * 64:(h + 1) * 64], pt[:, 0:n], start=(kt == 0), stop=(kt == nkt - 1)),
                     reads=[v_sb, pt], writes=[o_ps])
                k.op("pe", lambda e: e.matmul(d_ps[0:64, j0:512], C.onesb[:, 0:64], pt[:, 0:n], start=(kt == 0), stop=(kt == nkt - 1)),
                     reads=[C.onesb, pt], writes=[d_ps])

            pend = None
            for h in range(8):
                c, pb_ = h // 2, (h % 2) * 64
                for qc in range(4):
                    nkt = 4 * qc + 4
                    for kt in range(nkt):
                        j0 = max(0, kt - 4 * qc) * 128
                        n = 512 - j0
                        q0 = qc * 512 + j0
                        ps = psf(C)
                        diag = kt >= 4 * qc
                        k.op("pe", lambda e, ps=ps, c=c, pb_=pb_, kt=kt, q0=q0, n=n: e.matmul(
                            ps[:, 0:n], kn[c][pb_:pb_ + 64, kt * 128:(kt + 1) * 128], qn[c][pb_:pb_ + 64, q0:q0 + n], start=True, stop=False),
                            reads=[kn[c], qn[c]], writes=[ps])
                        if diag:
                            k.op("pe", lambda e, ps=ps: e.matmul(ps[:, 0:128], C.identb[:, :], C.tribias[:, :], start=False, stop=False),
                                 reads=[C.identb, C.tribias], writes=[ps])
                        eo = (h * 16 + kt) * 128
                        k.op("pe", lambda e, ps=ps, eo=eo, q0=q0, n=n: e.matmul(
                            ps[:, 0:n], E[0:68, eo:eo + 128], auxT[0:68, q0:q0 + n], start=False, stop=True),
                            reads=[E, auxT], writes=[ps])
                        pt = ptr.next()
                        k.op("act", lambda e, pt=pt, ps=ps, n=n: e.activation(pt[:, 0:n], ps[:, 0:n], AF.Exp), reads=[ps], writes=[pt])
                        if pend is not None:
                            emit_pv(*pend)
                        pend = (pt, kt, h, j0, n, nkt)
                    emit_pv(*pend)
                    pend = None
                    ob = osb.next()
                    k.op("dve", lambda e: e.reciprocal(rec[:, :], d_ps[0:64, :]), reads=[d_ps], writes=[rec])
                    k.op("dve", lambda e, ob=ob: e.tensor_tensor(ob[:, :], o_ps[0:64, :], rec[:, :], op=ALU.mult), reads=[o_ps, rec], writes=[ob])
                    k.dma("sp", [(scr.mixT[h * 64:(h + 1) * 64, t0 + qc * 512:t0 + (qc + 1) * 512], ob[:, :])], reads=[ob], sembuf=ob)
        C.pfn = 6


def bc_row(k, name, ap1, n):
    t = k.sb(name, [128, n], F32)
    k.dma("sp", [(t[:, :], ap1.partition_broadcast(128))], writes=[t], sembuf=t)
    return t


def stage_ssd(k, C, scr, Wt):
    V = lambda fn, r, w: k.op("dve", fn, reads=r, writes=w)
    A = lambda fn, r, w: k.op("act", fn, reads=r, writes=w)
    G = lambda fn, r, w: k.op("pool", fn, reads=r, writes=w)
    P = lambda fn, r, w: k.op("pe", fn, reads=r, writes=w)
    with Stage(k):
        C.pfn = 3
        yd = [C.pf[3], C.pf[4]]
        yo = [C.pf[5], C.pf[5]]
        cw = k.sb("scw", [128, 12, 4], F32)
        cbias = k.sb("scb", [128, 12], F32)
        convw2 = Wt["ssm_conv_w"][0]
        load_cols(k, C, lambda c: cw[:, c, :], convw2, 4, 12)
        load_cols(k, C, lambda c: cbias[:, c:c + 1], Wt["ssm_conv_b"][0].rearrange("(o c) -> o c", o=1), 1, 12)
        k.barrier()
        dtb_bc = bc_row(k, "dtb_bc", Wt["ssm_dt_bias"][0], 16)
        a_bc = bc_row(k, "a_bc", Wt["ssm_a_log"][0], 16)
        A(lambda e: e.activation(a_bc[:, :], a_bc[:, :], AF.Exp), [a_bc], [a_bc])
        V(lambda e: e.tensor_scalar(a_bc[:, :], a_bc[:, :], -1.0, None, op0=ALU.mult), [a_bc], [a_bc])
        d_bc = bc_row(k, "d_bc", Wt["ssm_d"][0], 16)
        nw_bc = bc_row(k, "nw_bc", Wt["ssm_norm"][0], 1024)
        xc = [k.sb("xc%d" % i, [128, S], BF16) for i in range(12)]
        rawr = Ring([k.sb("sraw%d" % i, [128, 3 + S], BF16) for i in range(2)])
        for r_ in rawr.bufs:
            G(lambda e, r_=r_: e.memset(r_[:, 0:3], 0.0), [], [r_])
        dgr = Ring([k.sb("sdg%d" % i, [128, 4, 128], BF16) for i in range(2)])
        xs_tm = k.sb("xs_tm", [128, 16, 1024], BF16)
        B_tm = k.sb("B_tm", [128, 16, 256], BF16)
        dt_sp = k.sb("dt_sp", [128, 16, 16], F32)
        dta = k.sb("dta", [128, 16, 16], F32)
        gU = k.sb("gU", [128, 16, 128], F32)
        acs = k.sb("acs", [128, 32], F32)
        eac = k.sb("eac", [128, 16], F32)
        cdec = k.sb("cdec", [128, 16], F32)
        wst = k.sb("wst", [128, 16], F32)
        t1r = Ring([k.sb("st1%d" % i, [128, 512], F32) for i in range(2)])
        t2r = Ring([k.sb("st2%d" % i, [128, 512], F32) for i in range(2)])
        mtr = Ring([k.sb("smt%d" % i, [128, 512], BF16) for i in range(2)])
        cbm = [k.sb("cbm%d" % g, [128, 128], F32) for g in range(2)]
        xdtr = Ring([k.sb("xdt%d" % i, [128, 1024], BF16) for i in range(2)])
        xdtwr = Ring([k.sb("xdtw%d" % i, [128, 1024], BF16) for i in range(2)])
        yr = Ring([k.sb("sy%d" % i, [128, 1024], F32) for i in range(2)])
        tmp2 = k.sb("stmp2", [128, 1024], F32)
        zr = Ring([k.sb("sz%d" % i, [128, 1024], F32) for i in range(2)])
        junk = k.sb("sjunk", [128, 512], BF16)
        ss2 = k.sb("sss2", [128, 4], F32)
        obr = Ring([k.sb("sob%d" % i, [128, 1024], BF16) for i in range(2)])
        obTr = Ring([k.sb("sobT%d" % i, [128, 8, 128], BF16) for i in range(2)])
        prev32 = [k.sb("prev32_%d" % g, [128, 512], F32) for g in range(2)]
        prevb = [k.sb("prevb_%d" % g, [128, 512], BF16) for g in range(2)]
        for s in range(NSEQ):
            t0 = s * S
            for c in range(12):
                raw = rawr.next(); dg = dgr.next()
                k.dma("sp", [(raw[:, 3:3 + S], scr.xbcT[c * 128:(c + 1) * 128, t0:t0 + S])], writes=[raw], sembuf=raw)
                for tap in range(4):
                    G(lambda e, dg=dg, tap=tap, c=c: e.tensor_scalar(dg[:, tap, :], C.identb[:, :], cw[:, c, tap:tap + 1], None, op0=ALU.mult),
                      [C.identb, cw], [dg])
                for tb in range(4):
                    ps = psf(C)
                    for tap in range(4):
                        P(lambda e, ps=ps, dg=dg, tap=tap, raw=raw, tb=tb: e.matmul(
                            ps[:, :], dg[:, tap, :], raw[:, tb * 512 + tap: tb * 512 + tap + 512], start=(tap == 0), stop=(tap == 3)), [dg, raw], [ps])
                    A(lambda e, ps=ps, c=c, tb=tb: e.activation(xc[c][:, tb * 512:(tb + 1) * 512], ps[:, :], AF.Silu, bias=cbias[:, c:c + 1], scale=1.0),
                      [ps, cbias], [xc[c]])
            for kt in range(16):
                pp = psb(C)
                for c in range(8):
                    P(lambda e, pp=pp, c=c, kt=kt: e.transpose(pp[:, c * 128:(c + 1) * 128], xc[c][:, kt * 128:(kt + 1) * 128], C.identb[:, :]),
                      [xc[c], C.identb], [pp])
                copy_op(k, evac_eng(C), xs_tm[:, kt, :], pp[:, :], [pp], [xs_tm])
                pp = psb(C)
                for g in range(2):
                    P(lambda e, pp=pp, g=g, kt=kt: e.transpose(pp[:, g * 128:(g + 1) * 128], xc[8 + g][:, kt * 128:(kt + 1) * 128], C.identb[:, :]),
                      [xc[8 + g], C.identb], [pp])
                copy_op(k, evac_eng(C), B_tm[:, kt, :], pp[:, 0:256], [pp], [B_tm])
            k.dma("sp", [(dt_sp[:, :, :], scr.dt[t0:t0 + S, :].rearrange("(kt p) h -> p kt h", p=128))], writes=[dt_sp], sembuf=dt_sp)
            V(lambda e: e.tensor_tensor(dt_sp[:, :, :], dt_sp[:, :, :], dtb_bc[:, :].unsqueeze(1).broadcast_to([128, 16, 16]), op=ALU.add),
              [dt_sp, dtb_bc], [dt_sp])
            A(lambda e: e.activation(dt_sp[:, :, :], dt_sp[:, :, :], AF.Exp), [dt_sp], [dt_sp])
            A(lambda e: e.activation(dt_sp[:, :, :], dt_sp[:, :, :], AF.Ln, bias=1.0, scale=1.0), [dt_sp], [dt_sp])
            V(lambda e: e.tensor_tensor(dta[:, :, :], dt_sp[:, :, :], a_bc[:, :].unsqueeze(1).broadcast_to([128, 16, 16]), op=ALU.mult),
              [dt_sp, a_bc], [dta])
            for g in range(2):
                G(lambda e, g=g: e.memset(prev32[g][:, :], 0.0), [], [prev32[g]])
                G(lambda e, g=g: e.memset(prevb[g][:, :], 0.0), [], [prevb[g]])
            for c in range(16):
                xdt = xdtr.next(); xdtw = xdtwr.next(); y = yr.next(); zt = zr.next(); ob = obr.next(); obT = obTr.next()
                k.dma("sp", [(zt[:, :], scr.z[t0 + c * 128:t0 + (c + 1) * 128, :])], writes=[zt], sembuf=zt)
                ps = psf(C)
                P(lambda e, ps=ps, c=c: e.matmul(ps[:, 0:16], C.U32[:, :], dta[:, c, :], start=True, stop=True), [C.U32, dta], [ps])
                P(lambda e, ps=ps, c=c: e.matmul(ps[:, 16:32], C.ones32[:, :], dta[:, c, :], start=True, stop=True), [C.ones32, dta], [ps])
                V(lambda e, ps=ps: e.tensor_copy(acs[:, :], ps[:, 0:32]), [ps], [acs])
                A(lambda e: e.activation(eac[:, :], acs[:, 0:16], AF.Exp), [acs], [eac])
                A(lambda e: e.activation(cdec[:, :], acs[:, 16:32], AF.Exp), [acs], [cdec])
                V(lambda e: e.tensor_tensor(wst[:, :], acs[:, 16:32], acs[:, 0:16], op=ALU.subtract), [acs], [wst])
                A(lambda e: e.activation(wst[:, :], wst[:, :], AF.Exp), [wst], [wst])
                V(lambda e, c=c: e.tensor_tensor(gU[:, :, :], C.U32[:, :].unsqueeze(1).broadcast_to([128, 16, 128]),
                                                 dta[:, c, :].unsqueeze(2).broadcast_to([128, 16, 128]), op=ALU.mult), [C.U32, dta], [gU])
                V(lambda e, xdt=xdt, c=c: e.tensor_tensor(xdt[:, :].rearrange("p (h d) -> p h d", h=16), xs_tm[:, c, :].rearrange("p (h d) -> p h d", h=16),
                                                          dt_sp[:, c, :].unsqueeze(2).broadcast_to([128, 16, 64]), op=ALU.mult), [xs_tm, dt_sp], [xdt])
                G(lambda e, xdt=xdt, xdtw=xdtw: e.tensor_tensor(xdtw[:, :].rearrange("p (h d) -> p h d", h=16), xdt[:, :].rearrange("p (h d) -> p h d", h=16),
                                                                wst[:, :].unsqueeze(2).broadcast_to([128, 16, 64]), op=ALU.mult), [xdt, wst], [xdtw])
                for g in range(2):
                    ps = psf(C)
                    P(lambda e, ps=ps, g=g, c=c: e.matmul(ps[:, 0:128], xc[8 + g][:, c * 128:(c + 1) * 128], xc[10 + g][:, c * 128:(c + 1) * 128], start=True, stop=True),
                      [xc[8 + g], xc[10 + g]], [ps])
                    V(lambda e, ps=ps, g=g: e.tensor_tensor(cbm[g][:, :], ps[:, 0:128], C.U32[:, :], op=ALU.mult), [ps, C.U32], [cbm[g]])
                    P(lambda e, g=g, c=c: e.matmul(yo[g][:, :], xc[10 + g][:, c * 128:(c + 1) * 128], prevb[g][:, :], start=True, stop=True),
                      [xc[10 + g], prevb[g]], [yo[g]])
                    ysl0 = y[:, g * 512:(g + 1) * 512]
                    V(lambda e, ysl0=ysl0, g=g: e.tensor_tensor(ysl0.rearrange("p (h d) -> p h d", h=8), yo[g][:, :].rearrange("p (h d) -> p h d", h=8),
                                                              eac[:, g * 8:(g + 1) * 8].unsqueeze(2).broadcast_to([128, 8, 64]), op=ALU.mult), [yo[g], eac], [y])
                for hg in range(4):
                    g = hg // 2
                    R = psf(C); t1 = t1r.next(); t2 = t2r.next(); mt = mtr.next()
                    P(lambda e, R=R, hg=hg: e.matmul(R[:, :], C.ones32[:, :], gU[:, hg * 4:(hg + 1) * 4, :].rearrange("p a b -> p (a b)"), start=True, stop=True),
                      [C.ones32, gU], [R])
                    V(lambda e, R=R, t1=t1, hg=hg: e.tensor_tensor(t1[:, :].rearrange("p (a b) -> p a b", a=4), R[:, :].rearrange("p (a b) -> p a b", a=4),
                                                                  acs[:, hg * 4:(hg + 1) * 4].unsqueeze(2).broadcast_to([128, 4, 128]), op=ALU.subtract), [R, acs], [t1])
                    V(lambda e, t1=t1: e.tensor_scalar(t1[:, :], t1[:, :], 0.0, None, op0=ALU.min), [t1], [t1])
                    A(lambda e, t1=t1, t2=t2: e.activation(t2[:, :], t1[:, :], AF.Exp), [t1], [t2])
                    V(lambda e, t2=t2, mt=mt, g=g: e.tensor_tensor(mt[:, :].rearrange("p (a b) -> p a b", a=4), t2[:, :].rearrange("p (a b) -> p a b", a=4),
                                                                  cbm[g][:, :].unsqueeze(1).broadcast_to([128, 4, 128]), op=ALU.mult), [t2, cbm[g]], [mt])
                    for hh in range(4):
                        h = hg * 4 + hh
                        P(lambda e, mt=mt, hh=hh, h=h, g=g, xdt=xdt: e.matmul(yd[g][:, (h % 8) * 64:(h % 8 + 1) * 64], mt[:, hh * 128:(hh + 1) * 128],
                                                                             xdt[:, h * 64:(h + 1) * 64], start=True, stop=True), [mt, xdt], [yd[g]])
                for g in range(2):
                    ysl = y[:, g * 512:(g + 1) * 512]
                    V(lambda e, ysl=ysl, g=g: e.tensor_tensor(ysl, ysl, yd[g][:, :], op=ALU.add), [y, yd[g]], [y])
                G(lambda e, c=c: e.tensor_tensor(tmp2[:, :].rearrange("p (h d) -> p h d", h=16), xs_tm[:, c, :].rearrange("p (h d) -> p h d", h=16),
                                                 d_bc[:, :].unsqueeze(2).broadcast_to([128, 16, 64]), op=ALU.mult), [xs_tm, d_bc], [tmp2])
                G(lambda e, y=y: e.tensor_tensor(y[:, :], y[:, :], tmp2[:, :], op=ALU.add), [y, tmp2], [y])
                for g in range(2):
                    st = psf(C)
                    P(lambda e, st=st, g=g, c=c, xdtw=xdtw: e.matmul(st[:, :], B_tm[:, c, g * 128:(g + 1) * 128], xdtw[:, g * 512:(g + 1) * 512], start=True, stop=True),
                      [B_tm, xdtw], [st])
                    V(lambda e, g=g: e.tensor_tensor(prev32[g][:, :].rearrange("p (h d) -> p h d", h=8), prev32[g][:, :].rearrange("p (h d) -> p h d", h=8),
                                                     cdec[:, g * 8:(g + 1) * 8].unsqueeze(2).broadcast_to([128, 8, 64]), op=ALU.mult), [prev32[g], cdec], [prev32[g]])
                    V(lambda e, g=g, st=st: e.tensor_tensor(prev32[g][:, :], prev32[g][:, :], st[:, :], op=ALU.add), [prev32[g], st], [prev32[g]])
                    G(lambda e, g=g: e.tensor_copy(prevb[g][:, :], prev32[g][:, :]), [prev32[g]], [prevb[g]])
                A(lambda e, zt=zt: e.activation(zt[:, :], zt[:, :], AF.Silu), [zt], [zt])
                V(lambda e, y=y, zt=zt: e.tensor_tensor(y[:, :], y[:, :], zt[:, :], op=ALU.mult), [y, zt], [y])
                G(lambda e: e.memset(ss2[:, :], 0.0), [], [ss2])
                for g in range(2):
                    A(lambda e, y=y, g=g: e.activation(junk[:, :], y[:, g * 512:(g + 1) * 512], AF.Square, accum_out=ss2[:, g:g + 1]), [y, ss2], [junk, ss2])
                A(lambda e: e.activation(ss2[:, 2:4], ss2[:, 0:2], AF.Sqrt, bias=EPS, scale=1.0 / 512), [ss2], [ss2])
                V(lambda e: e.reciprocal(ss2[:, 2:4], ss2[:, 2:4]), [ss2], [ss2])
                for g in range(2):
                    V(lambda e, y=y, ob=ob, g=g: e.scalar_tensor_tensor(ob[:, g * 512:(g + 1) * 512], y[:, g * 512:(g + 1) * 512], ss2[:, 2 + g:3 + g],
                                                                       nw_bc[:, g * 512:(g + 1) * 512], op0=ALU.mult, op1=ALU.mult), [y, ss2, nw_bc], [ob])
                pp = psb(C)
                for j in range(8):
                    P(lambda e, pp=pp, ob=ob, j=j: e.transpose(pp[:, j * 128:(j + 1) * 128], ob[:, j * 128:(j + 1) * 128], C.identb[:, :]), [ob, C.identb], [pp])
                copy_op(k, evac_eng(C), obT[:, :, :], pp[:, :].rearrange("p (a b) -> p a b", a=8), [pp], [obT])
                k.dma("sp", [(scr.mixT[512:1536, t0 + c * 128:t0 + (c + 1) * 128].rearrange("(j p) t -> p j t", p=128), obT[:, :, :])], reads=[obT], sembuf=obT)
        C.pfn = 6


def host_consts():
    slopes = np.array([2.0 ** (-(h + 1)) for h in range(8)], dtype=np.float64)
    etab = np.zeros((68, 8, 16, 128), dtype=np.float32)
    for h in range(8):
        for kt in range(16):
            etab[((h % 2) * 4 + h // 2) * 8 + kt // 2, h, kt, :] = 1.0
            etab[64, h, kt, :] = -128.0 * slopes[h]
            etab[65, h, kt, :] = -slopes[h]
            etab[66, h, kt, :] = slopes[h] * np.arange(128)
            etab[67, h, kt, :] = 128.0 * slopes[h] * kt
    auxc = np.zeros((4, S), dtype=np.float32)
    pos = np.arange(S)
    auxc[0] = pos // 128
    auxc[1] = pos % 128
    auxc[2] = 1.0
    auxc[3] = 1.0
    return {"etab": etab.reshape(68, 8 * 16 * 128), "auxc": auxc}


def build(upto="full", dumps=()):
    nc = bass.Bass("TRN2", target_bir_lowering=False)
    root = ExitStack()
    k = K(nc, root)
    _k_init_extra(k)
    k.cur_dsems = []
    C = Ctx()
    x_d = k.dram("x", [NTOK, D], F32, kind="ExternalInput").t
    p_d = [k.dram("p%d" % i, [NTOK, 256], F32, kind="ExternalInput").t for i in range(2)]
    Wt = {n: k.dram(n, shp, F32, kind="ExternalInput").t for n, shp in WSHAPES.items()}
    etab_d = k.dram("etab", [68, 8 * 16 * 128], F32, kind="ExternalInput").t
    auxc_d = k.dram("auxc", [4, S], F32, kind="ExternalInput").t
    y_d = k.dram("y", [NTOK, D], F32, kind="ExternalOutput").t
    scr = Ctx()
    scr.qkT = k.dram("s_qkT", [1024, NTOK], BF16).t
    scr.v = k.dram("s_v", [NTOK, 512], BF16).t
    scr.z = k.dram("s_z", [NTOK, 1024], F32).t
    scr.xbcT = k.dram("s_xbcT", [1536, NTOK], BF16).t
    scr.dt = k.dram("s_dt", [NTOK, 16], F32).t
    scr.mixT = k.dram("s_mixT", [2048, NTOK], BF16).t
    scr.actT = k.dram("s_actT", [DFF, NTOK], BF16).t
    scr.qkvT = k.dram("s_qkvT", [4096, NTOK], BF16).t
    scr.zT = k.dram("s_zT", [2048, NTOK], F32).t
    scr.ba = k.dram("s_ba", [NTOK, 32], F32).t
    xa = k.dram("s_xa", [NTOK, D], F32).t
    xb = k.dram("s_xb", [NTOK, D], F32).t
    mk_consts(k, C)
    k.barrier()
    if "auxT" in dumps:
        C.dbg_aux = k.dram("dbg_auxT", [68, S], BF16, kind="ExternalOutput").t
        dumps = [d_ for d_ in dumps if d_ != "auxT"]
    order = ["in0", "attn", "ssd", "x1", "x2", "x3", "in1", "gdn", "x4", "x5", "full"]
    lim = order.index(upto)
    last = x_d

    def done(name):
        return order.index(name) > lim

    stage_inproj0(k, C, x_d, Wt["norm_mix"][0], Wt["w_in_even"][0], scr)
    if not done("attn"):
        stage_attn(k, C, scr, Wt["moba_q_norm"][0], Wt["moba_k_norm"][0], etab_d, auxc_d)
    if not done("ssd"):
        stage_ssd(k, C, scr, Wt)
    if not done("x1"):
        stage_resid_linear(k, C, scr.mixT[0:1536, :], 12, Wt["w_out_even"][0], x_d, xa)
        last = xa
    if not done("x2"):
        stage_ffn_a(k, C, xa, Wt["norm_ffn"][0], Wt["ffn_w_gate"][0], Wt["ffn_w_up"][0], Wt["ffn_conv_w"][0], Wt["ffn_conv_b"][0], scr.actT)
        stage_resid_linear(k, C, scr.actT, 22, Wt["ffn_w_down"][0], xa, xb)
        last = xb
    if not done("x3"):
        stage_ple(k, C, xb, Wt["norm_ple"][0], Wt["ple_w_gate"][0], Wt["ple_w_proj"][0], p_d[0], xa)
        last = xa
    if not done("in1"):
        stage_inproj1(k, C, xa, Wt["norm_mix"][1], Wt["w_in_odd"][0], scr)
    if not done("gdn"):
        stage_gdn(k, C, scr, Wt)
    if not done("x4"):
        stage_resid_linear(k, C, scr.mixT, 16, Wt["w_out_odd"][0], xa, xb)
        last = xb
    if not done("x5"):
        stage_ffn_a(k, C, xb, Wt["norm_ffn"][1], Wt["ffn_w_gate"][1], Wt["ffn_w_up"][1], Wt["ffn_conv_w"][1], Wt["ffn_conv_b"][1], scr.actT)
        stage_resid_linear(k, C, scr.actT, 22, Wt["ffn_w_down"][1], xb, xa)
        last = xa
    if not done("full"):
        stage_ple(k, C, xa, Wt["norm_ple"][1], Wt["ple_w_gate"][1], Wt["ple_w_proj"][1], p_d[1], xb)
        last = xb
    with Stage(k):
        cp = k.sb("cpbuf", [128, 4, D], F32)
        for i in range(NTOK // 512):
            k.dma("sp", [(cp[:, :, :], last[i * 512:(i + 1) * 512, :].rearrange("(a p) d -> p a d", p=128))], writes=[cp], sembuf=cp)
            k.dma("sp", [(y_d[i * 512:(i + 1) * 512, :].rearrange("(a p) d -> p a d", p=128), cp[:, :, :])], reads=[cp], sembuf=cp)
        for name in dumps:
            src = getattr(scr, name)
            shp = list(src.shape)
            dd = k.dram("dbg_" + name, shp, src.dtype, kind="ExternalOutput").t
            rows = shp[0]
            cb2 = k.sb("cpb_" + name, [128, shp[1]], src.dtype)
            for i in range(rows // 128):
                k.dma("sp", [(cb2[:, :], src[i * 128:(i + 1) * 128, :])], writes=[cb2], sembuf=cb2)
                k.dma("sp", [(dd[i * 128:(i + 1) * 128, :], cb2[:, :])], reads=[cb2], sembuf=cb2)
    k.finish([])
    root.close()
    return nc


_NC_CACHE = {}


def kernel(**inputs):
    n = 8
    hc = host_consts()
    x = np.ascontiguousarray(inputs["x"], dtype=np.float32).reshape(n, NTOK, D)
    p = np.ascontiguousarray(inputs["p"], dtype=np.float32)
    in_maps = []
    for c in range(n):
        m = {"x": x[c], "p0": np.ascontiguousarray(p[0, 2 * c:2 * c + 2].reshape(NTOK, 256)),
             "p1": np.ascontiguousarray(p[1, 2 * c:2 * c + 2].reshape(NTOK, 256)),
             "etab": hc["etab"], "auxc": hc["auxc"]}
        for nme in WSHAPES:
            m[nme] = np.ascontiguousarray(inputs[nme], dtype=np.float32)
        in_maps.append(m)
    if "full" not in _NC_CACHE:
        _NC_CACHE["full"] = build("full")
    res = run_bass_kernel_spmd(_NC_CACHE["full"], in_maps, core_ids=list(range(n)))
    out = np.stack([r["y"] for r in res.results], axis=0)
    return out.reshape(16, S, D).astype(np.float32)


def stage_inproj1(k, C, x_src, gain_ap, Wd2, scr):
    with Stage(k):
        hT = [k.sb("hT%d" % i, [128, 8, 512], BF16) for i in range(NTOK // 512)]
        norm_to_hT(k, C, x_src, 0, NTOK, gain_ap, hT)
        wring = Ring([k.sb("w%d" % i, [128, 8, 512], BF16) for i in range(3)])
        stb = Ring([k.sb("stb%d" % i, [128, 512], BF16) for i in range(4)])
        stf = Ring([k.sb("stf%d" % i, [128, 512], F32) for i in range(3)])
        rhs_fn = lambda kc, tb: (hT[tb], hT[tb][:, kc, :])
        lhs_fn = lambda kc, tt: (hT[tt // 4], hT[tt // 4][:, kc, (tt % 4) * 128:(tt % 4 + 1) * 128])

        def ev_fm(dst, ring):
            def ev(c, m, tb, ps):
                st = ring.next()
                copy_op(k, evac_eng(C), st[0:m, :], ps[0:m, :], [ps], [st])
                k.dma("sp", [(dst[c:c + m, tb * 512:(tb + 1) * 512], st[0:m, :])], reads=[st], sembuf=st)
            return ev

        def ev_tm(cb, n, tt, ps):
            st = stf.next()
            copy_op(k, evac_eng(C), st[:, 0:n], ps[:, 0:n], [ps], [st])
            k.dma("sp", [(scr.ba[tt * 128:(tt + 1) * 128, cb:cb + n], st[:, 0:n])], reads=[st], sembuf=st)

        linear_fm(k, C, rhs_fn, 8, Wd2, 0, 0, 4096, NTOK // 512, ev_fm(scr.qkvT, stb), wring)
        linear_fm(k, C, rhs_fn, 8, Wd2, 0, 4096, 2048, NTOK // 512, ev_fm(scr.zT, stf), wring)
        linear_tm(k, C, lhs_fn, 8, Wd2, 0, 6144, 32, NTOK // 128, ev_tm, wring)


def stage_gdn(k, C, scr, Wt, nseq=NSEQ, nkh=8, nck=None, phases=(1, 2)):
    V = lambda fn, r, w: k.op("dve", fn, reads=r, writes=w)
    A = lambda fn, r, w: k.op("act", fn, reads=r, writes=w)
    G = lambda fn, r, w: k.op("pool", fn, reads=r, writes=w)
    P = lambda fn, r, w: k.op("pe", fn, reads=r, writes=w)
    NCK = S // 128
    with Stage(k):
        cw = k.sb("gcw", [128, 32, 4], F32)
        convw2 = Wt["gdn_conv_w"][0]
        load_cols(k, C, lambda c: cw[:, c, :], convw2, 4, 32)
        k.barrier()
        dtb_bc = bc_row(k, "gdtb", Wt["gdn_dt_bias"][0], 16)
        a_bc = bc_row(k, "ga", Wt["gdn_a_log"][0], 16)
        A(lambda e: e.activation(a_bc[:, :], a_bc[:, :], AF.Exp), [a_bc], [a_bc])
        V(lambda e: e.tensor_scalar(a_bc[:, :], a_bc[:, :], -1.0, None, op0=ALU.mult), [a_bc], [a_bc])
        nwc = k.sb("gnw", [128, 1], F32)
        load_col(k, nwc, Wt["gdn_norm"][0], 128, 1)
        ba = k.sb("gba", [128, NCK, 32], F32)
        bet = k.sb("gbet", [128, NCK, 16], F32)
        nbet = k.sb("gnbet", [128, NCK, 16], F32)
        gg = k.sb("ggg", [128, NCK, 16], F32)
        rawr = Ring([k.sb("graw%d" % i, [128, 3 + S], BF16) for i in range(2)])
        for r_ in rawr.bufs:
            G(lambda e, r_=r_: e.memset(r_[:, 0:3], 0.0), [], [r_])
        dgr = Ring([k.sb("gdg%d" % i, [128, 4, 128], BF16) for i in range(2)])
        cvr = Ring([k.sb("gcv%d" % i, [128, S], BF16) for i in range(2)])
        sqr = Ring([k.sb("gsq%d" % i, [128, 512], BF16) for i in range(2)])
        rsr = Ring([k.sb("grs%d" % i, [128, 512], F32) for i in range(2)])
        QT = k.sb("gQT", [128, S], BF16)
        KT = k.sb("gKT", [128, S], BF16)
        K_tm = k.sb("gK_tm", [128, NCK, 128], BF16)
        V_tm = k.sb("gV_tm", [128, NCK, 256], BF16)
        u0b = k.sb("gu0b", [128, NCK, 2, 128], BF16)
        w0T = k.sb("gw0T", [128, NCK, 2, 128], BF16)
        qkT = k.sb("gqkT", [128, NCK, 2, 128], BF16)
        QdT = k.sb("gQdT", [128, NCK, 2, 128], BF16)
        kdec = k.sb("gkdec", [128, NCK, 2, 128], BF16)
        egl = k.sb("gegl", [128, NCK, 2], F32)
        acsA = k.sb("gacsA", [128, NCK, 4], F32)
        ecolA = k.sb("gecolA", [128, NCK, 4], F32)
        gUall = k.sb("ggUall", [128, 8, 2, 128], F32)
        t1r = Ring([k.sb("gt1%d" % i, [128, 512], F32) for i in range(2)])
        decr = Ring([k.sb("gdec%d" % i, [128, 512], F32) for i in range(2)])
        eRr = Ring([k.sb("geR%d" % i, [128, 512], F32) for i in range(2)])
        tmpr = Ring([k.sb("gtmp%d" % i, [128, 512], F32) for i in range(2)])
        Ybuf = [[k.sb("gY%d_%d" % (g_, i), [128, 4, 128], BF16) for i in range(2)] for g_ in range(4)]
        Wbuf = [[k.sb("gW%d_%d" % (g_, i), [128, 4, 128], BF16) for i in range(2)] for g_ in range(4)]
        Tbuf = [[k.sb("gT%d_%d" % (g_, i), [128, 4, 128], BF16) for i in range(2)] for g_ in range(4)]
        kegA = [k.sb("gkeg%d" % g_, [128, 2, 2, 128], BF16) for g_ in range(4)]
        S32 = [k.sb("gS32_%d" % i, [128, 128], F32) for i in range(2)]
        Sb = [k.sb("gSb_%d" % i, [128, 128], BF16) for i in range(2)]
        vnr = Ring([k.sb("gvn%d" % i, [128, 128], BF16) for i in range(3)])
        szT = [k.sb("gsz%d" % i, [128, S], F32) for i in range(2)]
        outT = [k.sb("gout%d" % i, [128, S], BF16) for i in range(2)]
        osq = Ring([k.sb("gosq%d" % i, [128, 128], BF16) for i in range(6)])
        ocpr = Ring([k.sb("gocp%d" % i, [128, 128], F32) for i in range(6)])
        ors = Ring([k.sb("gors%d" % i, [128, 128], F32) for i in range(4)])
        otm = Ring([k.sb("gotm%d" % i, [128, 128], F32) for i in range(4)])

        def conv_chunk(ch, t0, dst):
            raw = rawr.next(); dg = dgr.next()
            k.dma("sp", [(raw[:, 3:3 + S], scr.qkvT[ch * 128:(ch + 1) * 128, t0:t0 + S])], writes=[raw], sembuf=raw)
            for tap in range(4):
                G(lambda e, dg=dg, tap=tap: e.tensor_scalar(dg[:, tap, :], C.identb[:, :], cw[:, ch, tap:tap + 1], None, op0=ALU.mult), [C.identb, cw], [dg])
            for tb in range(4):
                ps = psf(C)
                for tap in range(4):
                    P(lambda e, ps=ps, dg=dg, tap=tap, raw=raw, tb=tb: e.matmul(
                        ps[:, :], dg[:, tap, :], raw[:, tb * 512 + tap: tb * 512 + tap + 512], start=(tap == 0), stop=(tap == 3)), [dg, raw], [ps])
                A(lambda e, ps=ps, tb=tb: e.activation(dst[:, tb * 512:(tb + 1) * 512], ps[:, :], AF.Silu), [ps], [dst])

        def l2norm(src, dst, scale):
            for tb in range(4):
                sq = sqr.next(); rs = rsr.next(); ps = psf(C)
                A(lambda e, sq=sq, tb=tb: e.activation(sq[:, :], src[:, tb * 512:(tb + 1) * 512], AF.Square), [src], [sq])
                P(lambda e, ps=ps, sq=sq: e.matmul(ps[:, :], C.onesb[:, :], sq[:, :], start=True, stop=True), [C.onesb, sq], [ps])
                A(lambda e, ps=ps, rs=rs: e.activation(rs[:, :], ps[:, :], AF.Sqrt, bias=EPS, scale=1.0), [ps], [rs])
                V(lambda e, rs=rs: e.reciprocal(rs[:, :], rs[:, :]), [rs], [rs])
                V(lambda e, rs=rs, tb=tb: e.scalar_tensor_tensor(dst[:, tb * 512:(tb + 1) * 512], src[:, tb * 512:(tb + 1) * 512], scale, rs[:, :],
                                                                 op0=ALU.mult, op1=ALU.mult), [src, rs], [dst])

        C.pfn = 5
        for s in range(nseq):
            t0 = s * S
            k.dma("sp", [(ba[:, :, :], scr.ba[t0:t0 + S, :].rearrange("(c p) h -> p c h", p=128))], writes=[ba], sembuf=ba)
            A(lambda e: e.activation(bet[:, :, :], ba[:, :, 0:16], AF.Sigmoid), [ba], [bet])
            V(lambda e: e.tensor_scalar(nbet[:, :, :], bet[:, :, :], -1.0, None, op0=ALU.mult), [bet], [nbet])
            V(lambda e: e.tensor_tensor(gg[:, :, :], ba[:, :, 16:32], dtb_bc[:, :].unsqueeze(1).broadcast_to([128, NCK, 16]), op=ALU.add), [ba, dtb_bc], [gg])
            A(lambda e: e.activation(gg[:, :, :], gg[:, :, :], AF.Exp), [gg], [gg])
            A(lambda e: e.activation(gg[:, :, :], gg[:, :, :], AF.Ln, bias=1.0, scale=1.0), [gg], [gg])
            V(lambda e: e.tensor_tensor(gg[:, :, :], gg[:, :, :], a_bc[:, :].unsqueeze(1).broadcast_to([128, NCK, 16]), op=ALU.mult), [gg, a_bc], [gg])
            for kh in range(nkh):
                cq = cvr.next(); conv_chunk(kh, t0, cq); l2norm(cq, QT, 128.0 ** -0.5)
                ck = cvr.next(); conv_chunk(8 + kh, t0, ck); l2norm(ck, KT, 1.0)
                for c in range(0, NCK, 8):
                    pp = psb(C)
                    for j in range(8):
                        P(lambda e, pp=pp, j=j, c=c: e.transpose(pp[:, j * 128:(j + 1) * 128], KT[:, (c + j) * 128:(c + j + 1) * 128], C.identb[:, :]), [KT, C.identb], [pp])
                    copy_op(k, evac_eng(C), K_tm[:, c:c + 8, :], pp[:, :].rearrange("p (a b) -> p a b", a=8), [pp], [K_tm])
                for hv in range(2):
                    cv = cvr.next(); conv_chunk(16 + 2 * kh + hv, t0, cv)
                    for c in range(0, NCK, 8):
                        pp = psb(C)
                        for j in range(8):
                            P(lambda e, pp=pp, j=j, c=c, cv=cv: e.transpose(pp[:, j * 128:(j + 1) * 128], cv[:, (c + j) * 128:(c + j + 1) * 128], C.identb[:, :]), [cv, C.identb], [pp])
                        copy_op(k, evac_eng(C), V_tm[:, c:c + 8, hv * 128:(hv + 1) * 128], pp[:, :].rearrange("p (a b) -> p a b", a=8), [pp], [V_tm])
                    h = 2 * kh + hv
                    k.dma("sp", [(szT[hv][:, :], scr.zT[h * 128:(h + 1) * 128, t0:t0 + S])], writes=[szT[hv]], sembuf=szT[hv])
                    A(lambda e, hv=hv: e.activation(szT[hv][:, :], szT[hv][:, :], AF.Silu), [szT[hv]], [szT[hv]])
                NC1 = (nck or NCK) if 1 in phases else 0
                if NC1:
                    pa = psf(C)
                    for c in range(NC1):
                        P(lambda e, pa=pa, c=c, kh=kh: e.matmul(pa[:, c * 4:c * 4 + 2], C.U32[:, :], gg[:, c, 2 * kh:2 * kh + 2], start=True, stop=True), [C.U32, gg], [pa])
                        P(lambda e, pa=pa, c=c, kh=kh: e.matmul(pa[:, c * 4 + 2:c * 4 + 4], C.ones32[:, :], gg[:, c, 2 * kh:2 * kh + 2], start=True, stop=True), [C.ones32, gg], [pa])
                    V(lambda e, pa=pa: e.tensor_copy(acsA[:, 0:NC1, :], pa[:, 0:NC1 * 4].rearrange("p (c f) -> p c f", f=4)), [pa], [acsA])
                    A(lambda e: e.activation(ecolA[:, 0:NC1, 0:2], acsA[:, 0:NC1, 0:2], AF.Exp), [acsA], [ecolA])
                    V(lambda e: e.tensor_tensor(ecolA[:, 0:NC1, 2:4], acsA[:, 0:NC1, 2:4], acsA[:, 0:NC1, 0:2], op=ALU.subtract), [acsA], [ecolA])
                    A(lambda e: e.activation(ecolA[:, 0:NC1, 2:4], ecolA[:, 0:NC1, 2:4], AF.Exp), [ecolA], [ecolA])
                    A(lambda e: e.activation(egl[:, 0:NC1, :], acsA[:, 0:NC1, 2:4], AF.Exp), [acsA], [egl])
                for half0 in range(0, NC1, 8):
                    ncs = min(8, NC1 - half0)
                    ngr = ncs // 2
                    G(lambda e, half0=half0, ncs=ncs, kh=kh: e.tensor_tensor(
                        gUall[:, 0:ncs, :, :], C.U32[:, :].unsqueeze(1).unsqueeze(1).broadcast_to([128, ncs, 2, 128]),
                        gg[:, half0:half0 + ncs, 2 * kh:2 * kh + 2].unsqueeze(3).broadcast_to([128, ncs, 2, 128]), op=ALU.mult), [C.U32, gg], [gUall])
                    Yc = [None] * ngr; Wc = [None] * ngr; Tc = [None] * ngr
                    for gi in range(ngr):
                        c0 = half0 + 2 * gi
                        bsl = bet[:, c0:c0 + 2, 2 * kh:2 * kh + 2].unsqueeze(3).broadcast_to([128, 2, 2, 128])
                        v4 = lambda t: t[:, :].rearrange("p (a b c) -> p a b c", a=2, b=2)
                        R = psf(C); t1 = t1r.next(); dec = decr.next(); eR = eRr.next(); tmp = tmpr.next()
                        P(lambda e, R=R, gi=gi: e.matmul(R[:, :], C.ones32[:, :], gUall[:, 2 * gi:2 * gi + 2, :, :].rearrange("p a b c -> p (a b c)"), start=True, stop=True),
                          [C.ones32, gUall], [R])
                        V(lambda e, R=R, t1=t1, c0=c0: e.tensor_tensor(v4(t1), v4(R), acsA[:, c0:c0 + 2, 0:2].unsqueeze(3).broadcast_to([128, 2, 2, 128]), op=ALU.subtract), [R, acsA], [t1])
                        V(lambda e, t1=t1: e.tensor_scalar(t1[:, :], t1[:, :], 0.0, None, op0=ALU.min), [t1], [t1])
                        A(lambda e, R=R, eR=eR: e.activation(eR[:, :], R[:, :], AF.Exp), [R], [eR])
                        A(lambda e, t1=t1, dec=dec: e.activation(dec[:, :], t1[:, :], AF.Exp), [t1], [dec])
                        G(lambda e, dec=dec: e.tensor_tensor(dec[:, :].rearrange("p (a c) -> p a c", a=4), dec[:, :].rearrange("p (a c) -> p a c", a=4),
                                                             C.U32[:, :].unsqueeze(1).broadcast_to([128, 4, 128]), op=ALU.mult), [dec, C.U32], [dec])
                        pk = C.pf[5]
                        for cl in range(2):
                            cs = slice((c0 + cl) * 128, (c0 + cl + 1) * 128)
                            P(lambda e, cl=cl, cs=cs: e.matmul(pk[:, cl * 256:cl * 256 + 128], KT[:, cs], KT[:, cs], start=True, stop=True), [KT], [pk])
                            P(lambda e, cl=cl, cs=cs: e.matmul(pk[:, cl * 256 + 128:cl * 256 + 256], KT[:, cs], QT[:, cs], start=True, stop=True), [KT, QT], [pk])
                        pk3 = pk[:, :].rearrange("p (a f) -> p a f", a=2)
                        V(lambda e, tmp=tmp, dec=dec, pk3=pk3: e.tensor_tensor(v4(tmp), pk3[:, :, 0:128].unsqueeze(2).broadcast_to([128, 2, 2, 128]), v4(dec), op=ALU.mult), [pk, dec], [tmp])
                        V(lambda e, tmp=tmp, bsl=bsl: e.tensor_tensor(v4(tmp), v4(tmp), bsl, op=ALU.mult), [tmp, bet], [tmp])
                        X = Ybuf[gi][0]
                        G(lambda e, X=X, tmp=tmp: e.tensor_tensor(X[:, :, :], tmp[:, :].rearrange("p (a c) -> p a c", a=4), C.SU32[:, :].unsqueeze(1).broadcast_to([128, 4, 128]), op=ALU.mult),
                          [tmp, C.SU32], [X])
                        V(lambda e, dec=dec, pk3=pk3, c0=c0: e.tensor_tensor(qkT[:, c0:c0 + 2, :, :], pk3[:, :, 128:256].unsqueeze(2).broadcast_to([128, 2, 2, 128]), v4(dec), op=ALU.mult), [pk, dec], [qkT])
                        G(lambda e, eR=eR, c0=c0: e.tensor_tensor(QdT[:, c0:c0 + 2, :, :], QT[:, c0 * 128:(c0 + 2) * 128].rearrange("p (a c) -> p a c", a=2).unsqueeze(2).broadcast_to([128, 2, 2, 128]),
                                                                 v4(eR), op=ALU.mult), [QT, eR], [QdT])
                        kb4 = K_tm[:, c0:c0 + 2, :].unsqueeze(2).broadcast_to([128, 2, 2, 128])
                        G(lambda e, gi=gi, c0=c0, kb4=kb4: e.tensor_tensor(kegA[gi][:, :, :, :], kb4, ecolA[:, c0:c0 + 2, 0:2].unsqueeze(3).broadcast_to([128, 2, 2, 128]), op=ALU.mult), [K_tm, ecolA], [kegA[gi]])
                        G(lambda e, c0=c0, kb4=kb4: e.tensor_tensor(kdec[:, c0:c0 + 2, :, :], kb4, ecolA[:, c0:c0 + 2, 2:4].unsqueeze(3).broadcast_to([128, 2, 2, 128]), op=ALU.mult), [K_tm, ecolA], [kdec])
                        pp = psb(C)
                        for p4 in range(4):
                            P(lambda e, pp=pp, X=X, p4=p4: e.transpose(pp[:, p4 * 128:(p4 + 1) * 128], X[:, p4, :], C.identb[:, :]), [X, C.identb], [pp])
                        W = Wbuf[gi][0]
                        copy_op(k, evac_eng(C), W[:, :, :], pp[:, 0:512].rearrange("p (a c) -> p a c", a=4), [pp], [W])
                        Tt = Tbuf[gi][0]
                        V(lambda e, Tt=Tt, X=X: e.tensor_tensor(Tt[:, :, :], C.identb[:, :].unsqueeze(1).broadcast_to([128, 4, 128]), X[:, :, :], op=ALU.subtract), [C.identb, X], [Tt])
                        Yc[gi], Wc[gi], Tc[gi] = X, W, Tt
                    for lvl in range(1, 7):
                        nb_ = lvl % 2
                        for gi in range(ngr):
                            Y, W = Yc[gi], Wc[gi]
                            pw = psf(C)
                            for p4 in range(4):
                                P(lambda e, pw=pw, Y=Y, W=W, p4=p4: e.matmul(pw[:, p4 * 128:(p4 + 1) * 128], Y[:, p4, :], W[:, p4, :], start=True, stop=True), [Y, W], [pw])
                            if lvl < 6:
                                py = psf(C)
                                for p4 in range(4):
                                    P(lambda e, py=py, Y=Y, W=W, p4=p4: e.matmul(py[:, p4 * 128:(p4 + 1) * 128], W[:, p4, :], Y[:, p4, :], start=True, stop=True), [Y, W], [py])
                            W2 = Wbuf[gi][nb_]
                            copy_op(k, "act", W2[:, :, :], pw[:, :].rearrange("p (a c) -> p a c", a=4), [pw], [W2])
                            if lvl < 6:
                                Y2 = Ybuf[gi][nb_]
                                copy_op(k, "dve", Y2[:, :, :], py[:, :].rearrange("p (a c) -> p a c", a=4), [py], [Y2])
                                Yc[gi] = Y2
                            Wc[gi] = W2
                        for gi in range(ngr):
                            W, Tt = Wc[gi], Tc[gi]
                            pt_ = psf(C)
                            for p4 in range(4):
                                P(lambda e, pt_=pt_, W=W, Tt=Tt, p4=p4: e.matmul(pt_[:, p4 * 128:(p4 + 1) * 128], W[:, p4, :], Tt[:, p4, :], start=True, stop=False), [W, Tt], [pt_])
                                P(lambda e, pt_=pt_, Tt=Tt, p4=p4: e.matmul(pt_[:, p4 * 128:(p4 + 1) * 128], C.identb[:, :], Tt[:, p4, :], start=False, stop=True), [C.identb, Tt], [pt_])
                            Tt2 = Tbuf[gi][nb_]
                            copy_op(k, evac_eng(C), Tt2[:, :, :], pt_[:, :].rearrange("p (a c) -> p a c", a=4), [pt_], [Tt2])
                            Tc[gi] = Tt2
                    for gi in range(ngr):
                        c0 = half0 + 2 * gi
                        Tt = Tc[gi]
                        pu = psf(C); pw_ = psf(C)
                        for cl in range(2):
                            for hv in range(2):
                                p4 = cl * 2 + hv
                                P(lambda e, pu=pu, Tt=Tt, p4=p4, cl=cl, hv=hv, c0=c0: e.matmul(pu[:, p4 * 128:(p4 + 1) * 128], Tt[:, p4, :], V_tm[:, c0 + cl, hv * 128:(hv + 1) * 128], start=True, stop=True), [Tt, V_tm], [pu])
                                P(lambda e, pw_=pw_, Tt=Tt, p4=p4, cl=cl, hv=hv, gi=gi: e.matmul(pw_[:, p4 * 128:(p4 + 1) * 128], kegA[gi][:, cl, hv, :], Tt[:, p4, :], start=True, stop=True), [Tt, kegA[gi]], [pw_])
                        V(lambda e, pu=pu, c0=c0, kh=kh: e.tensor_tensor(u0b[:, c0:c0 + 2, :, :], pu[:, :].rearrange("p (a b c) -> p a b c", a=2, b=2),
                                                                      bet[:, c0:c0 + 2, 2 * kh:2 * kh + 2].unsqueeze(3).broadcast_to([128, 2, 2, 128]), op=ALU.mult), [pu, bet], [u0b])
                        copy_op(k, "act", w0T[:, c0:c0 + 2, :, :], pw_[:, :].rearrange("p (a b c) -> p a b c", a=2, b=2), [pw_], [w0T])
                for hv in range(2):
                    G(lambda e, hv=hv: e.memset(S32[hv][:, :], 0.0), [], [S32[hv]])
                    G(lambda e, hv=hv: e.memset(Sb[hv][:, :], 0.0), [], [Sb[hv]])
                pend_out = []

                def emit_post(c, hv, sq, ocp):
                    cs = slice(c * 128, (c + 1) * 128)
                    rs = ors.next(); tm_ = otm.next(); pn = psf(C)
                    P(lambda e, pn=pn, sq=sq: e.matmul(pn[:, 0:128], C.onesb[:, :], sq[:, :], start=True, stop=True), [C.onesb, sq], [pn])
                    A(lambda e, pn=pn, rs=rs: e.activation(rs[:, :], pn[:, 0:128], AF.Sqrt, bias=EPS, scale=1.0 / 128), [pn], [rs])
                    V(lambda e, rs=rs: e.reciprocal(rs[:, :], rs[:, :]), [rs], [rs])
                    V(lambda e, tm_=tm_, ocp=ocp, rs=rs: e.scalar_tensor_tensor(tm_[:, :], ocp[:, :], nwc[:, 0:1], rs[:, :], op0=ALU.mult, op1=ALU.mult), [ocp, nwc, rs], [tm_])
                    G(lambda e, tm_=tm_, hv=hv, cs=cs: e.tensor_tensor(outT[hv][:, cs], tm_[:, :], szT[hv][:, cs], op=ALU.mult), [tm_, szT[hv]], [outT[hv]])

                for c in range((nck or NCK) if 2 in phases else 0):
                    cur_out = []
                    for hv in range(2):
                        h = 2 * kh + hv
                        p1 = psf(C); vn = vnr.next()
                        P(lambda e, p1=p1, c=c, hv=hv: e.matmul(p1[:, 0:128], w0T[:, c, hv, :], Sb[hv][:, :], start=True, stop=True), [w0T, Sb[hv]], [p1])
                        V(lambda e, p1=p1, vn=vn, c=c, hv=hv, h=h: e.scalar_tensor_tensor(vn[:, :], p1[:, 0:128], nbet[:, c, h:h + 1], u0b[:, c, hv, :], op0=ALU.mult, op1=ALU.add),
                          [p1, nbet, u0b], [vn])
                        po = psf(C)
                        P(lambda e, po=po, c=c, hv=hv: e.matmul(po[:, 0:128], Sb[hv][:, :], QdT[:, c, hv, :], start=True, stop=False), [Sb[hv], QdT], [po])
                        P(lambda e, po=po, c=c, hv=hv, vn=vn: e.matmul(po[:, 0:128], vn[:, :], qkT[:, c, hv, :], start=False, stop=True), [vn, qkT], [po])
                        p2 = psf(C)
                        P(lambda e, p2=p2, c=c, hv=hv, vn=vn: e.matmul(p2[:, 0:128], kdec[:, c, hv, :], vn[:, :], start=True, stop=True), [kdec, vn], [p2])
                        V(lambda e, p2=p2, c=c, hv=hv: e.scalar_tensor_tensor(S32[hv][:, :], S32[hv][:, :], egl[:, c, hv:hv + 1], p2[:, 0:128], op0=ALU.mult, op1=ALU.add),
                          [S32[hv], egl, p2], [S32[hv]])
                        G(lambda e, hv=hv: e.tensor_copy(Sb[hv][:, :], S32[hv][:, :]), [S32[hv]], [Sb[hv]])
                        sq = osq.next(); ocp = ocpr.next()
                        A(lambda e, sq=sq, po=po: e.activation(sq[:, :], po[:, 0:128], AF.Square), [po], [sq])
                        A(lambda e, ocp=ocp, po=po: e.copy(ocp[:, :], po[:, 0:128]), [po], [ocp])
                        cur_out.append((c, hv, sq, ocp))
                    for args in pend_out:
                        emit_post(*args)
                    pend_out = cur_out
                for args in pend_out:
                    emit_post(*args)
                for hv in range(2):
                    h = 2 * kh + hv
                    k.dma("sp", [(scr.mixT[h * 128:(h + 1) * 128, t0:t0 + S], outT[hv][:, :])], reads=[outT[hv]], sembuf=outT[hv])
        C.pfn = 6
```

```python
from contextlib import ExitStack
import numpy as np
import os
GCUT = int(os.environ.get('GCUT', '99'))
import concourse.bass as bass
import concourse.mybir as mybir
from concourse.bass_utils import run_bass_kernel_spmd

F32 = mybir.dt.float32
BF16 = mybir.dt.bfloat16
AF = mybir.ActivationFunctionType
ALU = mybir.AluOpType
AX = mybir.AxisListType

ENGS = ("pe", "act", "dve", "pool", "sp")
EPOCH = 12000


class Buf:
    def __init__(self, k, name, t):
        self.k = k
        self.name = name
        self.t = t
        self.w = None
        self.r = []
        self.dsem = None
        self.dcnt = 0
        self.psum = False

    def __getitem__(self, idx):
        return self.t[idx]


class K:
    def __init__(self, nc, es):
        self.nc = nc
        self.es = es
        self.ops = {e: [] for e in ENGS}
        self.sems = {}
        self.ecnt = {e: 0 for e in ENGS}
        self.waited = {e: {} for e in ENGS}
        self.stack = [es]
        self.nbuf = 0
        self.ninstr = 0

    def sb(self, name, shape, dt=F32):
        self.uid = getattr(self, "uid", 0) + 1
        name = "%s_u%d" % (name, self.uid)
        t = self.es.enter_context(self.nc.sbuf_tensor(name, list(shape), dt))
        return Buf(self, name, t)

    def ps(self, name, shape, dt=F32):
        t = self.es.enter_context(self.nc.psum_tensor(name, list(shape), dt))
        b = Buf(self, name, t)
        b.psum = True
        return b

    def dram(self, name, shape, dt=F32, kind=None):
        if kind is None:
            t = self.nc.dram_tensor(name, list(shape), dt)
        else:
            t = self.nc.dram_tensor(name, list(shape), dt, kind=kind)
        return Buf(self, name, t.ap())

    def region(self, name):
        return Buf(self, name, None)

    def _dsem(self, b):
        if b.dsem is None:
            key = "d_" + b.name + "_%d" % self.nbuf
            self.nbuf += 1
            self.sems[key] = self.es.enter_context(self.nc.semaphore(key[:40]))
            b.dsem = key
        return b.dsem

    def _deps(self, eng, reads, writes):
        need = {}

        def add(ev, is_war=False):
            if ev is None:
                return
            key, val = ev
            if key.split("#")[0] == eng:
                if eng in ("pe", "sp") or is_war:
                    return
            if need.get(key, 0) < val:
                need[key] = val

        for b in reads:
            add(b.w)
            if b.psum:
                for ev in b.r:
                    if ev[0].split("#")[0] != eng:
                        add(ev)
        for b in writes:
            add(b.w, is_war=True)
            for ev in b.r:
                add(ev, is_war=True)
        out = []
        wd = self.waited[eng]
        for key, val in need.items():
            if wd.get(key, 0) >= val:
                continue
            wd[key] = val
            out.append((key, val))
        return out

    def _commit(self, ev, reads, writes):
        for b in reads:
            b.r.append(ev)
            if len(b.r) > 64:
                m = {}
                for kk, vv in b.r:
                    if m.get(kk, 0) < vv:
                        m[kk] = vv
                b.r = list(m.items())
        for b in writes:
            b.w = ev
            b.r = []

    def op(self, eng, fn, reads=(), writes=()):
        waits = self._deps(eng, reads, writes)
        self.ecnt[eng] += 1
        ep = (self.ecnt[eng] - 1) // EPOCH
        val = self.ecnt[eng] - ep * EPOCH
        ekey = "%s#%d" % (eng, ep)
        if ekey not in self.sems:
            self.sems[ekey] = self.stack[0].enter_context(self.nc.semaphore("es_%s_%d" % (eng, ep)))
        sems = self.sems
        wl = [(sems[k], v) for k, v in waits]
        mysem = sems[ekey]

        def emit(e, fn=fn, wl=wl, mysem=mysem):
            for s, v in wl:
                e.wait_ge(s, v)
            fn(e).then_inc(mysem, 1)

        self.ops[eng].append(emit)
        self._commit((ekey, val), reads, writes)
        self.ninstr += 1

    def dma(self, q, pairs, reads=(), writes=(), sembuf=None):
        assert sembuf is not None
        key = self._dsem(sembuf)
        waits = self._deps(q, reads, writes)
        sems = self.sems
        wl = [(sems[k], v) for k, v in waits]
        sembuf.dcnt += 16 * len(pairs)
        val = sembuf.dcnt
        dsem = sems[key]

        def emit(e, pairs=pairs, wl=wl, dsem=dsem):
            for s, v in wl:
                e.wait_ge(s, v)
            for o, i in pairs:
                e.dma_start(out=o, in_=i).then_inc(dsem, 16)

        self.ops[q].append(emit)
        self._commit((key, val), reads, writes)
        self.ninstr += len(pairs)

    def finish(self, final_bufs):
        nc = self.nc
        finals = []
        for b in final_bufs:
            if b.w is not None:
                finals.append(b.w)
        sems = self.sems

        def fin(e):
            for key, val in finals:
                e.wait_ge(sems[key], val)

        self.ops["sp"].append(fin)
        with nc.Block() as block:
            @block.tensor
            def _(e):
                for f in self.ops["pe"]:
                    f(e)

            @block.scalar
            def _(e):
                for f in self.ops["act"]:
                    f(e)

            @block.vector
            def _(e):
                for f in self.ops["dve"]:
                    f(e)

            @block.gpsimd
            def _(e):
                for f in self.ops["pool"]:
                    f(e)

            @block.sync
            def _(e):
                for f in self.ops["sp"]:
                    f(e)

    def barrier(self):
        targets = []
        for e in ("pe", "act", "dve", "pool"):
            if self.ecnt[e] > 0:
                ep = (self.ecnt[e] - 1) // EPOCH
                targets.append(("%s#%d" % (e, ep), self.ecnt[e] - ep * EPOCH))
        targets += [(key, cnt) for key, cnt in self.dsem_cnt.items() if cnt > 0]
        sems = self.sems
        for eng in ENGS:
            wd = self.waited[eng]
            wl = []
            for key, val in targets:
                if key.split("#")[0] == eng or wd.get(key, 0) >= val:
                    continue
                wd[key] = val
                wl.append((sems[key], val))

            def emit(e, wl=wl):
                for s, v in wl:
                    e.wait_ge(s, v)

            if wl:
                self.ops[eng].append(emit)


def _k_init_extra(self):
    self.dsem_cnt = {}
    self.free_dsems = []
    self.stack = [self.es]
    self._psf = []
    self._psf_i = 0


def _k_dsem(self, b):
    if b.dsem is None:
        if self.free_dsems:
            key = self.free_dsems.pop()
        else:
            key = "d%d" % self.nbuf
            self.nbuf += 1
            self.sems[key] = self.stack[0].enter_context(self.nc.semaphore(key))
            self.dsem_cnt[key] = 0
        b.dsem = key
        b.dcnt = self.dsem_cnt[key]
        self.cur_dsems.append(key)
    return b.dsem


K._dsem = _k_dsem


def _k_dma(self, q, pairs, reads=(), writes=(), sembuf=None, **kw):
    assert sembuf is not None
    key = self._dsem(sembuf)
    waits = self._deps(q, reads, writes)
    sems = self.sems
    wl = [(sems[k_], v) for k_, v in waits]
    sembuf.dcnt += 16 * len(pairs)
    val = sembuf.dcnt
    self.dsem_cnt[key] = val
    dsem = sems[key]

    def emit(e, pairs=pairs, wl=wl, dsem=dsem, kw=kw):
        for s, v in wl:
            e.wait_ge(s, v)
        for o, i in pairs:
            e.dma_start(out=o, in_=i, **kw).then_inc(dsem, 16)

    self.ops[q].append(emit)
    self._commit((key, val), reads, writes)
    self.ninstr += len(pairs)


K.dma = _k_dma


class Stage:
    def __init__(self, k):
        self.k = k

    def __enter__(self):
        k = self.k
        self.es = ExitStack()
        self.prev = k.es
        k.es = self.es
        self.prev_dsems = getattr(k, "cur_dsems", [])
        k.cur_dsems = []
        return self

    def __exit__(self, *a):
        k = self.k
        k.barrier()
        k.free_dsems.extend(k.cur_dsems)
        k.cur_dsems = self.prev_dsems
        k.es = self.prev
        self.es.close()
        return False


EPS = 1e-6
S = 2048
NSEQ = 2
NTOK = NSEQ * S
D = 1024
DFF = 2816
NEG = -30000.0

WSHAPES = {
    "norm_mix": [2, 1024], "norm_ffn": [2, 1024], "norm_ple": [2, 1024],
    "w_in_even": [1, 1024, 4112], "moba_q_norm": [1, 64], "moba_k_norm": [1, 64],
    "ssm_conv_w": [1, 4, 1536], "ssm_conv_b": [1, 1536], "ssm_dt_bias": [1, 16],
    "ssm_a_log": [1, 16], "ssm_d": [1, 16], "ssm_norm": [1, 1024],
    "w_out_even": [1, 1536, 1024], "w_in_odd": [1, 1024, 6176], "gdn_conv_w": [1, 4, 4096],
    "gdn_dt_bias": [1, 16], "gdn_a_log": [1, 16], "gdn_norm": [1, 128],
    "w_out_odd": [1, 2048, 1024], "ffn_w_gate": [2, 1024, 2816], "ffn_w_up": [2, 1024, 2816],
    "ffn_conv_w": [2, 3, 2816], "ffn_conv_b": [2, 2816], "ffn_w_down": [2, 2816, 1024],
    "ple_w_proj": [2, 256, 1024], "ple_w_gate": [2, 1024, 1024],
}


class Ctx:
    pass


def mk_consts(k, C):
    C.identf = k.sb("identf", [128, 128], F32)
    C.identb = k.sb("identb", [128, 128], BF16)
    C.ones32 = k.sb("ones32", [128, 128], F32)
    C.onesb = k.sb("onesb", [128, 128], BF16)
    C.U32 = k.sb("U32", [128, 128], F32)
    C.Ub = k.sb("Ub", [128, 128], BF16)
    C.SU32 = k.sb("SU32", [128, 128], F32)
    C.tribias = k.sb("tribias", [128, 128], BF16)
    C.blk1 = k.sb("blk1", [128, 128], BF16)
    tb32 = k.sb("tb32", [128, 128], F32)
    G = lambda fn, r, w: k.op("pool", fn, reads=r, writes=w)
    V = lambda fn, r, w: k.op("dve", fn, reads=r, writes=w)
    G(lambda e: e.memset(C.ones32[:, :], 1.0), [], [C.ones32])
    G(lambda e: e.affine_select(C.identf[:, :], C.ones32[:, :], pattern=[[-1, 128]], compare_op=ALU.is_equal,
                                fill=0.0, base=0, channel_multiplier=1), [C.ones32], [C.identf])
    G(lambda e: e.affine_select(C.U32[:, :], C.ones32[:, :], pattern=[[1, 128]], compare_op=ALU.is_ge,
                                fill=0.0, base=0, channel_multiplier=-1), [C.ones32], [C.U32])
    G(lambda e: e.affine_select(C.SU32[:, :], C.ones32[:, :], pattern=[[1, 128]], compare_op=ALU.is_gt,
                                fill=0.0, base=0, channel_multiplier=-1), [C.ones32], [C.SU32])
    V(lambda e: e.tensor_scalar(tb32[:, :], C.U32[:, :], -1.0, -NEG, op0=ALU.add, op1=ALU.mult), [C.U32], [tb32])
    V(lambda e: e.tensor_copy(C.tribias[:, :], tb32[:, :]), [tb32], [C.tribias])
    V(lambda e: e.tensor_copy(C.identb[:, :], C.identf[:, :]), [C.identf], [C.identb])
    V(lambda e: e.tensor_copy(C.onesb[:, :], C.ones32[:, :]), [C.ones32], [C.onesb])
    V(lambda e: e.tensor_copy(C.Ub[:, :], C.U32[:, :]), [C.U32], [C.Ub])
    G(lambda e: e.memset(C.blk1[:, :], 0.0), [], [C.blk1])
    G(lambda e: e.memset(C.blk1[0:64, 0:64], 1.0), [], [C.blk1])
    G(lambda e: e.memset(C.blk1[64:128, 64:128], 1.0), [], [C.blk1])
    C.pf = [k.ps("pf%d" % i, [128, 512], F32) for i in range(6)]
    C.pb = [k.ps("pb%d" % i, [128, 1024], BF16) for i in range(2)]
    C.pfi = 0
    C.pfn = 6
    C.pbi = 0
    C.evi = 0


def psf(C):
    C.pfi = (C.pfi + 1) % C.pfn
    return C.pf[C.pfi]


def psb(C):
    C.pbi = (C.pbi + 1) % len(C.pb)
    return C.pb[C.pbi]


class Ring:
    def __init__(self, bufs):
        self.bufs = bufs
        self.i = -1

    def next(self):
        self.i = (self.i + 1) % len(self.bufs)
        return self.bufs[self.i]


def evac_eng(C):
    C.evi += 1
    return "act" if (C.evi % 2) else "dve"


def copy_op(k, eng, out_ap, in_ap, reads, writes):
    if eng == "act":
        k.op("act", lambda e: e.copy(out_ap, in_ap), reads=reads, writes=writes)
    else:
        k.op(eng, lambda e: e.tensor_copy(out_ap, in_ap), reads=reads, writes=writes)


def load_w(k, Wd2, row0, nk, col0, ncols, wt):
    pairs = [(wt[:, kc, 0:ncols], Wd2[row0 + kc * 128: row0 + (kc + 1) * 128, col0:col0 + ncols]) for kc in range(nk)]
    k.dma("pool", pairs, writes=[wt], sembuf=wt)


def load_cols(k, C, dst_fn, rows_ap2, K, nch):
    rw = k.sb("rowsbuf", [K, nch * 128], F32)
    k.dma("sp", [(rw[:, :], rows_ap2)], writes=[rw], sembuf=rw)
    for c in range(nch):
        ps = psf(C)
        k.op("pe", lambda e, ps=ps, c=c: e.transpose(ps[:, 0:K], rw[0:K, c * 128:(c + 1) * 128], C.identf[0:K, 0:K]),
             reads=[rw, C.identf], writes=[ps])
        k.op("dve", lambda e, ps=ps, c=c: e.tensor_copy(dst_fn(c), ps[:, 0:K]), reads=[ps], writes=[])


def norm_to_hT(k, C, src_d, tok0, ntok, gain_ap, hT, hcol0=0):
    gbc = k.sb("gbc", [128, D], F32)
    k.dma("sp", [(gbc[:, :], gain_ap.partition_broadcast(128))], writes=[gbc], sembuf=gbc)
    xr = Ring([k.sb("nx%d" % i, [128, D], F32) for i in range(4)])
    junk = k.sb("njunk", [128, D], BF16)
    hbr = Ring([k.sb("nhb%d" % i, [128, D], BF16) for i in range(3)])
    ssr = Ring([k.sb("nss%d" % i, [128, 2], F32) for i in range(4)])
    def emit_tr(hb, c0):
        pt = psb(C)
        for kc in range(8):
            k.op("pe", lambda e, kc=kc, hb=hb, pt=pt: e.transpose(pt[:, kc * 128:(kc + 1) * 128], hb[:, kc * 128:(kc + 1) * 128], C.identb[:, :]),
                 reads=[hb, C.identb], writes=[pt])
        hb_ = hT[c0 // 512]
        copy_op(k, evac_eng(C), hb_[:, :, c0 % 512:c0 % 512 + 128], pt[:, :].rearrange("p (a b) -> p a b", a=8), [pt], [hb_])

    pend = None
    for tt in range(ntok // 128):
        xt = xr.next(); hb = hbr.next(); ss = ssr.next()
        r0 = tok0 + tt * 128
        k.dma("sp" if tt % 2 == 0 else "pool", [(xt[:, :], src_d[r0:r0 + 128, :])], writes=[xt], sembuf=xt)
        k.op("act", lambda e, ss=ss: e.memzero(ss[:, :]), writes=[ss])
        k.op("act", lambda e, xt=xt, ss=ss: e.activation(junk[:, :], xt[:, :], AF.Square, accum_out=ss[:, 0:1]),
             reads=[xt, ss], writes=[junk, ss])
        k.op("act", lambda e, ss=ss: e.activation(ss[:, 1:2], ss[:, 0:1], AF.Sqrt, bias=EPS, scale=1.0 / D),
             reads=[ss], writes=[ss])
        k.op("dve", lambda e, ss=ss: e.reciprocal(ss[:, 1:2], ss[:, 1:2]),
             reads=[ss], writes=[ss])
        k.op("dve", lambda e, xt=xt, ss=ss, hb=hb: e.scalar_tensor_tensor(hb[:, :], xt[:, :], ss[:, 1:2], gbc[:, :],
                                                                       op0=ALU.mult, op1=ALU.mult),
             reads=[xt, ss, gbc], writes=[hb])
        if pend is not None:
            emit_tr(*pend)
        pend = (hb, hcol0 + tt * 128)
    if pend is not None:
        emit_tr(*pend)


def linear_tm(k, C, lhsT_fn, nk, Wd2, row0, col0, ncols, ntt, evac, wring, cbw=512):
    for cb in range(0, ncols, cbw):
        n = min(cbw, ncols - cb)
        wt = wring.next()
        load_w(k, Wd2, row0, nk, col0 + cb, n, wt)
        for tt in range(ntt):
            ps = psf(C)
            for kc in range(nk):
                lb, lap = lhsT_fn(kc, tt)
                k.op("pe", lambda e, ps=ps, lap=lap, wt=wt, kc=kc, n=n: e.matmul(ps[:, 0:n], lap, wt[:, kc, 0:n], start=(kc == 0), stop=(kc == nk - 1)),
                     reads=[lb, wt], writes=[ps])
            evac(cb, n, tt, ps)


def linear_fm(k, C, rhs_fn, nk, Wd2, row0, col0, ncols, ntb, evac, wring):
    for cb in range(0, ncols, 512):
        n = min(512, ncols - cb)
        wt = wring.next()
        load_w(k, Wd2, row0, nk, col0 + cb, n, wt)
        for cc in range(0, n, 128):
            m = min(128, n - cc)
            for tb in range(ntb):
                ps = psf(C)
                for kc in range(nk):
                    rb, rap = rhs_fn(kc, tb)
                    k.op("pe", lambda e, ps=ps, rap=rap, wt=wt, kc=kc, cc=cc, m=m: e.matmul(ps[0:m, :], wt[:, kc, cc:cc + m], rap, start=(kc == 0), stop=(kc == nk - 1)),
                         reads=[rb, wt], writes=[ps])
                evac(cb + cc, m, tb, ps)


def stage_resid_linear(k, C, aT_d, nk, Wd2, x_src, x_dst):
    with Stage(k):
        wres = k.sb("wres", [128, nk, D], BF16)
        for half in range(2):
            k.dma("pool", [(wres[:, kc, half * 512:(half + 1) * 512], Wd2[kc * 128:(kc + 1) * 128, half * 512:(half + 1) * 512])
                           for kc in range(nk)], writes=[wres], sembuf=wres)
        abr = Ring([k.sb("ab%d" % i, [128, nk, 512], BF16) for i in range(2)])
        xr = Ring([k.sb("ox%d" % i, [128, D], F32) for i in range(3)])
        for tb in range(NTOK // 512):
            ab = abr.next()
            k.dma("sp", [(ab[:, :, :], aT_d[:, tb * 512:(tb + 1) * 512].rearrange("(kc p) t -> p kc t", p=128))],
                  writes=[ab], sembuf=ab)
            for t4 in range(4):
                tt = tb * 4 + t4
                xt = xr.next()
                k.dma("sp", [(xt[:, :], x_src[tt * 128:(tt + 1) * 128, :])], writes=[xt], sembuf=xt)
                for cb in range(2):
                    ps = psf(C)
                    for kc in range(nk):
                        k.op("pe", lambda e, ps=ps, ab=ab, kc=kc, t4=t4, cb=cb: e.matmul(
                            ps[:, :], ab[:, kc, t4 * 128:(t4 + 1) * 128], wres[:, kc, cb * 512:(cb + 1) * 512],
                            start=(kc == 0), stop=(kc == nk - 1)), reads=[ab, wres], writes=[ps])
                    k.op("dve", lambda e, ps=ps, xt=xt, cb=cb: e.tensor_tensor(
                        xt[:, cb * 512:(cb + 1) * 512], xt[:, cb * 512:(cb + 1) * 512], ps[:, :], op=ALU.add),
                        reads=[xt, ps], writes=[xt])
                k.dma("act", [(x_dst[tt * 128:(tt + 1) * 128, :], xt[:, :])], reads=[xt], sembuf=xt)


def stage_ffn_a(k, C, x_src, gain_ap, Wg2, Wu2, convw2, convb1, actT_d):
    with Stage(k):
        hT = [k.sb("hT%d" % i, [128, 8, 512], BF16) for i in range(NTOK // 512)]
        norm_to_hT(k, C, x_src, 0, NTOK, gain_ap, hT)
        cw = k.sb("cw", [128, 22, 3], F32)
        cbias = k.sb("cbias", [128, 22], F32)
        load_cols(k, C, lambda c: cw[:, c, :], convw2, 3, 22)
        load_cols(k, C, lambda c: cbias[:, c:c + 1], convb1.rearrange("(o c) -> o c", o=1), 1, 22)
        k.barrier()
        wgr = Ring([k.sb("wg%d" % i, [128, 8, 512], BF16) for i in range(2)])
        wur = Ring([k.sb("wu%d" % i, [128, 8, 512], BF16) for i in range(2)])
        grr = Ring([k.sb("graw%d" % i, [128, NSEQ, 2 + S], BF16) for i in range(2)])
        for g in grr.bufs:
            k.op("pool", lambda e, g=g: e.memset(g[:, :, 0:2], 0.0), writes=[g])
        dgr = Ring([k.sb("dg%d" % i, [128, 3, 128], BF16) for i in range(2)])
        sgr = Ring([k.sb("sg%d" % i, [128, 512], F32) for i in range(2)])
        str_ = Ring([k.sb("fst%d" % i, [128, 512], BF16) for i in range(3)])
        for cb512 in range(0, DFF, 512):
            n = min(512, DFF - cb512)
            wg = wgr.next(); wu = wur.next()
            load_w(k, Wg2, 0, 8, cb512, n, wg)
            load_w(k, Wu2, 0, 8, cb512, n, wu)
            for cc in range(0, n, 128):
                c = (cb512 + cc) // 128
                g = grr.next(); dg = dgr.next()
                for tap in range(3):
                    k.op("pool", lambda e, dg=dg, tap=tap, c=c: e.tensor_scalar(dg[:, tap, :], C.identb[:, :], cw[:, c, tap:tap + 1], None, op0=ALU.mult),
                         reads=[C.identb, cw], writes=[dg])
                for tb in range(8):
                    ps = psf(C)
                    for kc in range(8):
                        k.op("pe", lambda e, ps=ps, wg=wg, kc=kc, cc=cc, tb=tb: e.matmul(
                            ps[:, :], wg[:, kc, cc:cc + 128], hT[tb][:, kc, :], start=(kc == 0), stop=(kc == 7)),
                            reads=[wg, hT[tb]], writes=[ps])
                    sq, t4 = tb // 4, tb % 4
                    copy_op(k, evac_eng(C), g[:, sq, 2 + t4 * 512: 2 + (t4 + 1) * 512], ps[:, :], [ps], [g])
                for tb in range(8):
                    sq, t4 = tb // 4, tb % 4
                    pu = psf(C)
                    for kc in range(8):
                        k.op("pe", lambda e, pu=pu, wu=wu, kc=kc, cc=cc, tb=tb: e.matmul(
                            pu[:, :], wu[:, kc, cc:cc + 128], hT[tb][:, kc, :], start=(kc == 0), stop=(kc == 7)),
                            reads=[wu, hT[tb]], writes=[pu])
                    pc = psf(C)
                    for tap in range(3):
                        k.op("pe", lambda e, pc=pc, dg=dg, tap=tap, g=g, sq=sq, t4=t4: e.matmul(
                            pc[:, :], dg[:, tap, :], g[:, sq, t4 * 512 + tap: t4 * 512 + tap + 512], start=(tap == 0), stop=(tap == 2)),
                            reads=[dg, g], writes=[pc])
                    sg = sgr.next(); st = str_.next()
                    k.op("act", lambda e, sg=sg, pc=pc, c=c: e.activation(sg[:, :], pc[:, :], AF.Silu, bias=cbias[:, c:c + 1], scale=1.0),
                         reads=[pc, cbias], writes=[sg])
                    k.op("dve", lambda e, st=st, sg=sg, pu=pu: e.tensor_tensor(st[:, :], sg[:, :], pu[:, :], op=ALU.mult),
                         reads=[sg, pu], writes=[st])
                    k.dma("sp", [(actT_d[c * 128:(c + 1) * 128, tb * 512:(tb + 1) * 512], st[:, :])], reads=[st], sembuf=st)


def stage_ple(k, C, x_src, gain_ap, Wgate2, Wproj2, p_d, x_dst):
    with Stage(k):
        hT = [k.sb("hT%d" % i, [128, 8, 512], BF16) for i in range(NTOK // 512)]
        norm_to_hT(k, C, x_src, 0, NTOK, gain_ap, hT)
        pT = k.sb("pT", [128, 2, NTOK], BF16)
        pr = Ring([k.sb("pl%d" % i, [128, 256], F32) for i in range(2)])
        pbr = Ring([k.sb("plb%d" % i, [128, 256], BF16) for i in range(2)])
        for tt in range(NTOK // 128):
            pt_ = pr.next(); pb_ = pbr.next()
            k.dma("sp", [(pt_[:, :], p_d[tt * 128:(tt + 1) * 128, :])], writes=[pt_], sembuf=pt_)
            k.op("pool", lambda e, pt_=pt_, pb_=pb_: e.tensor_copy(pb_[:, :], pt_[:, :]), reads=[pt_], writes=[pb_])
            pp = psb(C)
            for j in range(2):
                k.op("pe", lambda e, pp=pp, pb_=pb_, j=j: e.transpose(pp[:, j * 128:(j + 1) * 128], pb_[:, j * 128:(j + 1) * 128], C.identb[:, :]),
                     reads=[pb_, C.identb], writes=[pp])
            copy_op(k, evac_eng(C), pT[:, :, tt * 128:(tt + 1) * 128], pp[:, 0:256].rearrange("p (a b) -> p a b", a=2), [pp], [pT])
        wgr = Ring([k.sb("wpg%d" % i, [128, 8, 512], BF16) for i in range(2)])
        wpr = Ring([k.sb("wpp%d" % i, [128, 2, 512], BF16) for i in range(2)])
        xr = Ring([k.sb("px%d" % i, [128, 512], F32) for i in range(3)])
        sgr = Ring([k.sb("psg%d" % i, [128, 512], F32) for i in range(2)])
        for cb in range(2):
            wg = wgr.next(); wp = wpr.next()
            load_w(k, Wgate2, 0, 8, cb * 512, 512, wg)
            load_w(k, Wproj2, 0, 2, cb * 512, 512, wp)
            for tt in range(NTOK // 128):
                xt = xr.next(); sg = sgr.next()
                k.dma("sp", [(xt[:, :], x_src[tt * 128:(tt + 1) * 128, cb * 512:(cb + 1) * 512])], writes=[xt], sembuf=xt)
                p1 = psf(C)
                for kc in range(8):
                    k.op("pe", lambda e, p1=p1, wg=wg, kc=kc, tt=tt: e.matmul(
                        p1[:, :], hT[tt // 4][:, kc, (tt % 4) * 128:(tt % 4 + 1) * 128], wg[:, kc, :], start=(kc == 0), stop=(kc == 7)),
                        reads=[hT[tt // 4], wg], writes=[p1])
                p2 = psf(C)
                for kc in range(2):
                    k.op("pe", lambda e, p2=p2, wp=wp, kc=kc, tt=tt: e.matmul(
                        p2[:, :], pT[:, kc, tt * 128:(tt + 1) * 128], wp[:, kc, :], start=(kc == 0), stop=(kc == 1)),
                        reads=[pT, wp], writes=[p2])
                k.op("act", lambda e, sg=sg, p1=p1: e.activation(sg[:, :], p1[:, :], AF.Sigmoid), reads=[p1], writes=[sg])
                k.op("dve", lambda e, sg=sg, p2=p2: e.tensor_tensor(sg[:, :], sg[:, :], p2[:, :], op=ALU.mult), reads=[sg, p2], writes=[sg])
                k.op("pool", lambda e, sg=sg, xt=xt: e.tensor_tensor(xt[:, :], xt[:, :], sg[:, :], op=ALU.add), reads=[sg, xt], writes=[xt])
                k.dma("pool", [(x_dst[tt * 128:(tt + 1) * 128, cb * 512:(cb + 1) * 512], xt[:, :])], reads=[xt], sembuf=xt)


def stage_inproj0(k, C, x_src, gain_ap, Wd2, scr):
    with Stage(k):
        hT = [k.sb("hT%d" % i, [128, 8, 512], BF16) for i in range(NTOK // 512)]
        norm_to_hT(k, C, x_src, 0, NTOK, gain_ap, hT)
        wring = Ring([k.sb("w%d" % i, [128, 8, 512], BF16) for i in range(3)])
        stb = Ring([k.sb("stb%d" % i, [128, 512], BF16) for i in range(4)])
        stf = Ring([k.sb("stf%d" % i, [128, 512], F32) for i in range(3)])
        rhs_fn = lambda kc, tb: (hT[tb], hT[tb][:, kc, :])
        lhs_fn = lambda kc, tt: (hT[tt // 4], hT[tt // 4][:, kc, (tt % 4) * 128:(tt % 4 + 1) * 128])

        def ev_fm(dst, r0):
            def ev(c, m, tb, ps):
                st = stb.next()
                copy_op(k, evac_eng(C), st[0:m, :], ps[0:m, :], [ps], [st])
                k.dma("sp", [(dst[r0 + c:r0 + c + m, tb * 512:(tb + 1) * 512], st[0:m, :])], reads=[st], sembuf=st)
            return ev

        def ev_tm(dst, c0, ring):
            def ev(cb, n, tt, ps):
                st = ring.next()
                copy_op(k, evac_eng(C), st[:, 0:n], ps[:, 0:n], [ps], [st])
                k.dma("sp", [(dst[tt * 128:(tt + 1) * 128, c0 + cb:c0 + cb + n], st[:, 0:n])], reads=[st], sembuf=st)
            return ev

        linear_fm(k, C, rhs_fn, 8, Wd2, 0, 0, 1024, NTOK // 512, ev_fm(scr.qkT, 0), wring)
        linear_tm(k, C, lhs_fn, 8, Wd2, 0, 1024, 512, NTOK // 128, ev_tm(scr.v, 0, stb), wring)
        linear_tm(k, C, lhs_fn, 8, Wd2, 0, 1536, 1024, NTOK // 128, ev_tm(scr.z, 0, stf), wring)
        linear_fm(k, C, rhs_fn, 8, Wd2, 0, 2560, 1536, NTOK // 512, ev_fm(scr.xbcT, 0), wring)
        linear_tm(k, C, lhs_fn, 8, Wd2, 0, 4096, 16, NTOK // 128, ev_tm(scr.dt, 0, stf), wring)


def load_col(k, dst, ap1, n, reps, scale=None):
    for r in range(reps):
        k.dma("sp", [(dst[r * n:(r + 1) * n, 0:1], ap1.rearrange("(p o) -> p o", o=1))], writes=[dst], sembuf=dst)
    if scale is not None:
        k.op("dve", lambda e: e.tensor_scalar(dst[:, 0:1], dst[:, 0:1], scale, None, op0=ALU.mult), reads=[dst], writes=[dst])


def stage_attn(k, C, scr, gq_ap, gk_ap, etab_d, auxc_d):
    with Stage(k):
        C.pfn = 4
        o_ps, d_ps = C.pf[4], C.pf[5]
        E = k.sb("E", [68, 8 * 16 * 128], BF16)
        for i in range(8):
            k.dma("pool", [(E[:, i * 2048:(i + 1) * 2048], etab_d[:, i * 2048:(i + 1) * 2048])], writes=[E], sembuf=E)
        gq = k.sb("gq", [128, 1], F32)
        gk = k.sb("gk", [128, 1], F32)
        load_col(k, gq, gq_ap, 64, 2, scale=0.125)
        load_col(k, gk, gk_ap, 64, 2)
        auxT = k.sb("auxT", [68, S], BF16)
        k.op("pool", lambda e: e.memset(auxT[0:64, :], 0.0), writes=[auxT])
        k.dma("pool", [(auxT[64:68, :], auxc_d[:, :])], writes=[auxT], sembuf=auxT)
        qn = [k.sb("qn%d" % c, [128, S], BF16) for c in range(4)]
        kn = [k.sb("kn%d" % c, [128, S], BF16) for c in range(4)]
        rawr = Ring([k.sb("araw%d" % i, [128, S], BF16) for i in range(2)])
        sqr = Ring([k.sb("asq%d" % i, [128, S], BF16) for i in range(2)])
        rsr = Ring([k.sb("ars%d" % i, [128, 512], F32) for i in range(2)])
        kb32 = k.sb("kb32", [128, 4, 8], F32)
        kbhi = k.sb("kbhi", [128, 4, 8], BF16)
        kblo = k.sb("kblo", [128, 4, 8], BF16)
        kbl32 = k.sb("kbl32", [128, 4, 8], F32)
        gs = k.sb("gs", [128, 8, 8], F32)
        cmp_ = k.sb("cmp", [128, 8, 8, 8], F32)
        cnt = k.sb("cnt", [128, 8, 8], F32)
        mbr = Ring([k.sb("mb%d" % i, [128, 64], BF16) for i in range(2)])
        v_sb = k.sb("v_sb", [128, 16, 512], BF16)
        ptr = Ring([k.sb("pt%d" % i, [128, 512], BF16) for i in range(5)])
        rec = k.sb("rec", [64, 512], F32)
        osb = Ring([k.sb("osb%d" % i, [64, 512], BF16) for i in range(2)])
        for s in range(NSEQ):
            t0 = s * S
            for c8 in range(8):
                isq = c8 < 4
                dst = qn[c8] if isq else kn[c8 - 4]
                gcol = gq if isq else gk
                raw = rawr.next(); sq = sqr.next()
                k.dma("sp", [(raw[:, :], scr.qkT[c8 * 128:(c8 + 1) * 128, t0:t0 + S])], writes=[raw], sembuf=raw)
                k.op("act", lambda e, sq=sq, raw=raw: e.activation(sq[:, :], raw[:, :], AF.Square), reads=[raw], writes=[sq])
                for tb in range(4):
                    ps = psf(C); rs = rsr.next()
                    k.op("pe", lambda e, ps=ps, sq=sq, tb=tb: e.matmul(ps[:, :], C.blk1[:, :], sq[:, tb * 512:(tb + 1) * 512], start=True, stop=True),
                         reads=[C.blk1, sq], writes=[ps])
                    k.op("act", lambda e, ps=ps, rs=rs: e.activation(rs[:, :], ps[:, :], AF.Sqrt, bias=EPS, scale=1.0 / 64), reads=[ps], writes=[rs])
                    k.op("dve", lambda e, rs=rs: e.reciprocal(rs[:, :], rs[:, :]), reads=[rs], writes=[rs])
                    k.op("dve", lambda e, dst=dst, raw=raw, gcol=gcol, rs=rs, tb=tb: e.scalar_tensor_tensor(
                        dst[:, tb * 512:(tb + 1) * 512], raw[:, tb * 512:(tb + 1) * 512], gcol[:, 0:1], rs[:, :], op0=ALU.mult, op1=ALU.mult),
                        reads=[raw, gcol, rs], writes=[dst])
            for c in range(4):
                k.op("dve", lambda e, c=c: e.tensor_reduce(kb32[:, c, :], kn[c][:, :].rearrange("p (n j) -> p n j", j=256), axis=AX.X, op=ALU.add),
                     reads=[kn[c]], writes=[kb32])
            k.op("dve", lambda e: e.tensor_scalar(kb32[:, :, :], kb32[:, :, :], 1.0 / 256, None, op0=ALU.mult), reads=[kb32], writes=[kb32])
            k.op("dve", lambda e: e.tensor_copy(kbhi[:, :, :], kb32[:, :, :]), reads=[kb32], writes=[kbhi])
            k.op("dve", lambda e: e.tensor_tensor(kbl32[:, :, :], kb32[:, :, :], kbhi[:, :, :], op=ALU.subtract), reads=[kb32, kbhi], writes=[kbl32])
            k.op("dve", lambda e: e.tensor_copy(kblo[:, :, :], kbl32[:, :, :]), reads=[kbl32], writes=[kblo])
            for qt in range(8, 16):
                nb = qt // 2
                for par in range(2):
                    pg = psf(C)
                    pb_ = par * 64
                    for i4 in range(4):
                        c = i4
                        k.op("pe", lambda e, pg=pg, i4=i4, c=c, pb_=pb_, qt=qt: e.matmul(
                            pg[:, i4 * 8:(i4 + 1) * 8], qn[c][pb_:pb_ + 64, qt * 128:(qt + 1) * 128], kbhi[pb_:pb_ + 64, c, :], start=True, stop=False),
                            reads=[qn[c], kbhi], writes=[pg])
                        k.op("pe", lambda e, pg=pg, i4=i4, c=c, pb_=pb_, qt=qt: e.matmul(
                            pg[:, i4 * 8:(i4 + 1) * 8], qn[c][pb_:pb_ + 64, qt * 128:(qt + 1) * 128], kblo[pb_:pb_ + 64, c, :], start=False, stop=True),
                            reads=[qn[c], kblo], writes=[pg])
                    k.op("dve", lambda e, pg=pg, par=par: e.tensor_copy(gs[:, par * 4:(par + 1) * 4, :], pg[:, 0:32].rearrange("p (h n) -> p h n", h=4)), reads=[pg], writes=[gs])
                k.op("dve", lambda e, nb=nb: e.tensor_tensor(
                    cmp_[:, :, 0:nb, 0:nb], gs[:, :, 0:nb].unsqueeze(2).broadcast_to([128, 8, nb, nb]),
                    gs[:, :, 0:nb].unsqueeze(3).broadcast_to([128, 8, nb, nb]), op=ALU.is_gt), reads=[gs], writes=[cmp_])
                k.op("dve", lambda e, nb=nb: e.tensor_reduce(cnt[:, :, 0:nb], cmp_[:, :, 0:nb, 0:nb], axis=AX.X, op=ALU.add), reads=[cmp_], writes=[cnt])
                mb = mbr.next()
                k.op("pool", lambda e, mb=mb: e.memset(mb[:, :], 0.0), writes=[mb])
                k.op("dve", lambda e, mb=mb, nb=nb: e.tensor_scalar(
                    mb[:, :].rearrange("p (h n) -> p h n", h=8)[:, :, 0:nb], cnt[:, :, 0:nb], 3.0, NEG, op0=ALU.is_ge, op1=ALU.mult),
                    reads=[cnt, mb], writes=[mb])
                pp = psb(C)
                k.op("pe", lambda e, pp=pp, mb=mb: e.transpose(pp[0:64, 0:128], mb[:, :], C.identb[:, :]), reads=[mb, C.identb], writes=[pp])
                copy_op(k, evac_eng(C), auxT[0:64, qt * 128:(qt + 1) * 128], pp[0:64, 0:128], [pp], [auxT])
            if getattr(C, "dbg_aux", None) is not None and s == 0:
                k.dma("sp", [(C.dbg_aux[:, :], auxT[:, :])], reads=[auxT], sembuf=auxT)
            k.dma("sp", [(v_sb[:, :, :], scr.v[t0:t0 + S, :].rearrange("(kt p) c -> p kt c", p=128))], writes=[v_sb], sembuf=v_sb)
            def emit_pv(pt, kt, h, j0, n, nkt):
                k.op("pe", lambda e: e.matmul(o_ps[0:64, j0:512], v_sb[:, kt, h * 64:(h + 1) * 64], pt[:, 0:n], start=(kt == 0), stop=(kt == nkt - 1)),
                     reads=[v_sb, pt], writes=[o_ps])
                k.op("pe", lambda e: e.matmul(d_ps[0:64, j0:512], C.onesb[:, 0:64], pt[:, 0:n], start=(kt == 0), stop=(kt == nkt - 1)),
                     reads=[C.onesb, pt], writes=[d_ps])

            pendq = []
            for h in range(8):
                c, pb_ = h // 2, (h % 2) * 64
                for qc in range(4):
                    nkt = 4 * qc + 4
                    for kt in range(nkt):
                        j0 = max(0, kt - 4 * qc) * 128
                        n = 512 - j0
                        q0 = qc * 512 + j0
                        ps = psf(C)
                        diag = kt >= 4 * qc
                        k.op("pe", lambda e, ps=ps, c=c, pb_=pb_, kt=kt, q0=q0, n=n: e.matmul(
                            ps[:, 0:n], kn[c][pb_:pb_ + 64, kt * 128:(kt + 1) * 128], qn[c][pb_:pb_ + 64, q0:q0 + n], start=True, stop=False),
                            reads=[kn[c], qn[c]], writes=[ps])
                        if diag:
                            k.op("pe", lambda e, ps=ps: e.matmul(ps[:, 0:128], C.identb[:, :], C.tribias[:, :], start=False, stop=False),
                                 reads=[C.identb, C.tribias], writes=[ps])
                        eo = (h * 16 + kt) * 128
                        k.op("pe", lambda e, ps=ps, eo=eo, q0=q0, n=n: e.matmul(
                            ps[:, 0:n], E[0:68, eo:eo + 128], auxT[0:68, q0:q0 + n], start=False, stop=True),
                            reads=[E, auxT], writes=[ps])
                        pt = ptr.next()
                        k.op("act", lambda e, pt=pt, ps=ps, n=n: e.activation(pt[:, 0:n], ps[:, 0:n], AF.Exp), reads=[ps], writes=[pt])
                        pendq.append((pt, kt, h, j0, n, nkt))
                        if len(pendq) > 2:
                            emit_pv(*pendq.pop(0))
                    while pendq:
                        emit_pv(*pendq.pop(0))
                    ob = osb.next()
                    k.op("dve", lambda e: e.reciprocal(rec[:, :], d_ps[0:64, :]), reads=[d_ps], writes=[rec])
                    k.op("dve", lambda e, ob=ob: e.tensor_tensor(ob[:, :], o_ps[0:64, :], rec[:, :], op=ALU.mult), reads=[o_ps, rec], writes=[ob])
                    k.dma("sp", [(scr.mixT[h * 64:(h + 1) * 64, t0 + qc * 512:t0 + (qc + 1) * 512], ob[:, :])], reads=[ob], sembuf=ob)
        C.pfn = 6


def bc_row(k, name, ap1, n):
    t = k.sb(name, [128, n], F32)
    k.dma("sp", [(t[:, :], ap1.partition_broadcast(128))], writes=[t], sembuf=t)
    return t


def stage_ssd(k, C, scr, Wt):
    V = lambda fn, r, w: k.op("dve", fn, reads=r, writes=w)
    A = lambda fn, r, w: k.op("act", fn, reads=r, writes=w)
    G = lambda fn, r, w: k.op("pool", fn, reads=r, writes=w)
    P = lambda fn, r, w: k.op("pe", fn, reads=r, writes=w)
    with Stage(k):
        C.pfn = 3
        yd = [C.pf[3], C.pf[4]]
        yo = [C.pf[5], C.pf[5]]
        cw = k.sb("scw", [128, 12, 4], F32)
        cbias = k.sb("scb", [128, 12], F32)
        convw2 = Wt["ssm_conv_w"][0]
        load_cols(k, C, lambda c: cw[:, c, :], convw2, 4, 12)
        load_cols(k, C, lambda c: cbias[:, c:c + 1], Wt["ssm_conv_b"][0].rearrange("(o c) -> o c", o=1), 1, 12)
        k.barrier()
        dtb_bc = bc_row(k, "dtb_bc", Wt["ssm_dt_bias"][0], 16)
        a_bc = bc_row(k, "a_bc", Wt["ssm_a_log"][0], 16)
        A(lambda e: e.activation(a_bc[:, :], a_bc[:, :], AF.Exp), [a_bc], [a_bc])
        V(lambda e: e.tensor_scalar(a_bc[:, :], a_bc[:, :], -1.0, None, op0=ALU.mult), [a_bc], [a_bc])
        d_bc = bc_row(k, "d_bc", Wt["ssm_d"][0], 16)
        nw_bc = bc_row(k, "nw_bc", Wt["ssm_norm"][0], 1024)
        xc = [k.sb("xc%d" % i, [128, S], BF16) for i in range(12)]
        rawr = Ring([k.sb("sraw%d" % i, [128, 3 + S], BF16) for i in range(2)])
        for r_ in rawr.bufs:
            G(lambda e, r_=r_: e.memset(r_[:, 0:3], 0.0), [], [r_])
        dgr = Ring([k.sb("sdg%d" % i, [128, 4, 128], BF16) for i in range(2)])
        xs_tm = k.sb("xs_tm", [128, 16, 1024], BF16)
        B_tm = k.sb("B_tm", [128, 16, 256], BF16)
        dt_sp = k.sb("dt_sp", [128, 16, 16], F32)
        dta = k.sb("dta", [128, 16, 16], F32)
        gU = k.sb("gU", [128, 16, 128], F32)
        acs = k.sb("acs", [128, 32], F32)
        eac = k.sb("eac", [128, 16], F32)
        cdec = k.sb("cdec", [128, 16], F32)
        wst = k.sb("wst", [128, 16], F32)
        t1r = Ring([k.sb("st1%d" % i, [128, 512], F32) for i in range(2)])
        t2r = Ring([k.sb("st2%d" % i, [128, 512], F32) for i in range(2)])
        mtr = Ring([k.sb("smt%d" % i, [128, 512], BF16) for i in range(2)])
        cbm = [k.sb("cbm%d" % g, [128, 128], F32) for g in range(2)]
        xdtr = Ring([k.sb("xdt%d" % i, [128, 1024], BF16) for i in range(2)])
        xdtwr = Ring([k.sb("xdtw%d" % i, [128, 1024], BF16) for i in range(2)])
        yr = Ring([k.sb("sy%d" % i, [128, 1024], F32) for i in range(2)])
        tmp2 = k.sb("stmp2", [128, 1024], F32)
        zr = Ring([k.sb("sz%d" % i, [128, 1024], F32) for i in range(2)])
        junk = k.sb("sjunk", [128, 512], BF16)
        ss2 = k.sb("sss2", [128, 4], F32)
        obr = Ring([k.sb("sob%d" % i, [128, 1024], BF16) for i in range(2)])
        obTr = Ring([k.sb("sobT%d" % i, [128, 8, 128], BF16) for i in range(2)])
        prev32 = [k.sb("prev32_%d" % g, [128, 512], F32) for g in range(2)]
        prevb = [k.sb("prevb_%d" % g, [128, 512], BF16) for g in range(2)]
        for s in range(NSEQ):
            t0 = s * S
            for c in range(12):
                raw = rawr.next(); dg = dgr.next()
                k.dma("sp", [(raw[:, 3:3 + S], scr.xbcT[c * 128:(c + 1) * 128, t0:t0 + S])], writes=[raw], sembuf=raw)
                for tap in range(4):
                    G(lambda e, dg=dg, tap=tap, c=c: e.tensor_scalar(dg[:, tap, :], C.identb[:, :], cw[:, c, tap:tap + 1], None, op0=ALU.mult),
                      [C.identb, cw], [dg])
                for tb in range(4):
                    ps = psf(C)
                    for tap in range(4):
                        P(lambda e, ps=ps, dg=dg, tap=tap, raw=raw, tb=tb: e.matmul(
                            ps[:, :], dg[:, tap, :], raw[:, tb * 512 + tap: tb * 512 + tap + 512], start=(tap == 0), stop=(tap == 3)), [dg, raw], [ps])
                    A(lambda e, ps=ps, c=c, tb=tb: e.activation(xc[c][:, tb * 512:(tb + 1) * 512], ps[:, :], AF.Silu, bias=cbias[:, c:c + 1], scale=1.0),
                      [ps, cbias], [xc[c]])
            for kt in range(16):
                pp = psb(C)
                for c in range(8):
                    P(lambda e, pp=pp, c=c, kt=kt: e.transpose(pp[:, c * 128:(c + 1) * 128], xc[c][:, kt * 128:(kt + 1) * 128], C.identb[:, :]),
                      [xc[c], C.identb], [pp])
                copy_op(k, evac_eng(C), xs_tm[:, kt, :], pp[:, :], [pp], [xs_tm])
                pp = psb(C)
                for g in range(2):
                    P(lambda e, pp=pp, g=g, kt=kt: e.transpose(pp[:, g * 128:(g + 1) * 128], xc[8 + g][:, kt * 128:(kt + 1) * 128], C.identb[:, :]),
                      [xc[8 + g], C.identb], [pp])
                copy_op(k, evac_eng(C), B_tm[:, kt, :], pp[:, 0:256], [pp], [B_tm])
            k.dma("sp", [(dt_sp[:, :, :], scr.dt[t0:t0 + S, :].rearrange("(kt p) h -> p kt h", p=128))], writes=[dt_sp], sembuf=dt_sp)
            V(lambda e: e.tensor_tensor(dt_sp[:, :, :], dt_sp[:, :, :], dtb_bc[:, :].unsqueeze(1).broadcast_to([128, 16, 16]), op=ALU.add),
              [dt_sp, dtb_bc], [dt_sp])
            A(lambda e: e.activation(dt_sp[:, :, :], dt_sp[:, :, :], AF.Exp), [dt_sp], [dt_sp])
            A(lambda e: e.activation(dt_sp[:, :, :], dt_sp[:, :, :], AF.Ln, bias=1.0, scale=1.0), [dt_sp], [dt_sp])
            V(lambda e: e.tensor_tensor(dta[:, :, :], dt_sp[:, :, :], a_bc[:, :].unsqueeze(1).broadcast_to([128, 16, 16]), op=ALU.mult),
              [dt_sp, a_bc], [dta])
            for g in range(2):
                G(lambda e, g=g: e.memset(prev32[g][:, :], 0.0), [], [prev32[g]])
                G(lambda e, g=g: e.memset(prevb[g][:, :], 0.0), [], [prevb[g]])
            for c in range(16):
                xdt = xdtr.next(); xdtw = xdtwr.next(); y = yr.next(); zt = zr.next(); ob = obr.next(); obT = obTr.next()
                k.dma("sp", [(zt[:, :], scr.z[t0 + c * 128:t0 + (c + 1) * 128, :])], writes=[zt], sembuf=zt)
                ps = psf(C)
                P(lambda e, ps=ps, c=c: e.matmul(ps[:, 0:16], C.U32[:, :], dta[:, c, :], start=True, stop=True), [C.U32, dta], [ps])
                P(lambda e, ps=ps, c=c: e.matmul(ps[:, 16:32], C.ones32[:, :], dta[:, c, :], start=True, stop=True), [C.ones32, dta], [ps])
                V(lambda e, ps=ps: e.tensor_copy(acs[:, :], ps[:, 0:32]), [ps], [acs])
                A(lambda e: e.activation(eac[:, :], acs[:, 0:16], AF.Exp), [acs], [eac])
                A(lambda e: e.activation(cdec[:, :], acs[:, 16:32], AF.Exp), [acs], [cdec])
                V(lambda e: e.tensor_tensor(wst[:, :], acs[:, 16:32], acs[:, 0:16], op=ALU.subtract), [acs], [wst])
                A(lambda e: e.activation(wst[:, :], wst[:, :], AF.Exp), [wst], [wst])
                V(lambda e, c=c: e.tensor_tensor(gU[:, :, :], C.U32[:, :].unsqueeze(1).broadcast_to([128, 16, 128]),
                                                 dta[:, c, :].unsqueeze(2).broadcast_to([128, 16, 128]), op=ALU.mult), [C.U32, dta], [gU])
                V(lambda e, xdt=xdt, c=c: e.tensor_tensor(xdt[:, :].rearrange("p (h d) -> p h d", h=16), xs_tm[:, c, :].rearrange("p (h d) -> p h d", h=16),
                                                          dt_sp[:, c, :].unsqueeze(2).broadcast_to([128, 16, 64]), op=ALU.mult), [xs_tm, dt_sp], [xdt])
                G(lambda e, xdt=xdt, xdtw=xdtw: e.tensor_tensor(xdtw[:, :].rearrange("p (h d) -> p h d", h=16), xdt[:, :].rearrange("p (h d) -> p h d", h=16),
                                                                wst[:, :].unsqueeze(2).broadcast_to([128, 16, 64]), op=ALU.mult), [xdt, wst], [xdtw])
                for g in range(2):
                    ps = psf(C)
                    P(lambda e, ps=ps, g=g, c=c: e.matmul(ps[:, 0:128], xc[8 + g][:, c * 128:(c + 1) * 128], xc[10 + g][:, c * 128:(c + 1) * 128], start=True, stop=True),
                      [xc[8 + g], xc[10 + g]], [ps])
                    V(lambda e, ps=ps, g=g: e.tensor_tensor(cbm[g][:, :], ps[:, 0:128], C.U32[:, :], op=ALU.mult), [ps, C.U32], [cbm[g]])
                    P(lambda e, g=g, c=c: e.matmul(yo[g][:, :], xc[10 + g][:, c * 128:(c + 1) * 128], prevb[g][:, :], start=True, stop=True),
                      [xc[10 + g], prevb[g]], [yo[g]])
                    ysl0 = y[:, g * 512:(g + 1) * 512]
                    V(lambda e, ysl0=ysl0, g=g: e.tensor_tensor(ysl0.rearrange("p (h d) -> p h d", h=8), yo[g][:, :].rearrange("p (h d) -> p h d", h=8),
                                                              eac[:, g * 8:(g + 1) * 8].unsqueeze(2).broadcast_to([128, 8, 64]), op=ALU.mult), [yo[g], eac], [y])
                for hg in range(4):
                    g = hg // 2
                    R = psf(C); t1 = t1r.next(); t2 = t2r.next(); mt = mtr.next()
                    P(lambda e, R=R, hg=hg: e.matmul(R[:, :], C.ones32[:, :], gU[:, hg * 4:(hg + 1) * 4, :].rearrange("p a b -> p (a b)"), start=True, stop=True),
                      [C.ones32, gU], [R])
                    V(lambda e, R=R, t1=t1, hg=hg: e.tensor_tensor(t1[:, :].rearrange("p (a b) -> p a b", a=4), R[:, :].rearrange("p (a b) -> p a b", a=4),
                                                                  acs[:, hg * 4:(hg + 1) * 4].unsqueeze(2).broadcast_to([128, 4, 128]), op=ALU.subtract), [R, acs], [t1])
                    V(lambda e, t1=t1: e.tensor_scalar(t1[:, :], t1[:, :], 0.0, None, op0=ALU.min), [t1], [t1])
                    A(lambda e, t1=t1, t2=t2: e.activation(t2[:, :], t1[:, :], AF.Exp), [t1], [t2])
                    V(lambda e, t2=t2, mt=mt, g=g: e.tensor_tensor(mt[:, :].rearrange("p (a b) -> p a b", a=4), t2[:, :].rearrange("p (a b) -> p a b", a=4),
                                                                  cbm[g][:, :].unsqueeze(1).broadcast_to([128, 4, 128]), op=ALU.mult), [t2, cbm[g]], [mt])
                    for hh in range(4):
                        h = hg * 4 + hh
                        P(lambda e, mt=mt, hh=hh, h=h, g=g, xdt=xdt: e.matmul(yd[g][:, (h % 8) * 64:(h % 8 + 1) * 64], mt[:, hh * 128:(hh + 1) * 128],
                                                                             xdt[:, h * 64:(h + 1) * 64], start=True, stop=True), [mt, xdt], [yd[g]])
                for g in range(2):
                    ysl = y[:, g * 512:(g + 1) * 512]
                    V(lambda e, ysl=ysl, g=g: e.tensor_tensor(ysl, ysl, yd[g][:, :], op=ALU.add), [y, yd[g]], [y])
                G(lambda e, c=c: e.tensor_tensor(tmp2[:, :].rearrange("p (h d) -> p h d", h=16), xs_tm[:, c, :].rearrange("p (h d) -> p h d", h=16),
                                                 d_bc[:, :].unsqueeze(2).broadcast_to([128, 16, 64]), op=ALU.mult), [xs_tm, d_bc], [tmp2])
                G(lambda e, y=y: e.tensor_tensor(y[:, :], y[:, :], tmp2[:, :], op=ALU.add), [y, tmp2], [y])
                for g in range(2):
                    st = psf(C)
                    P(lambda e, st=st, g=g, c=c, xdtw=xdtw: e.matmul(st[:, :], B_tm[:, c, g * 128:(g + 1) * 128], xdtw[:, g * 512:(g + 1) * 512], start=True, stop=True),
                      [B_tm, xdtw], [st])
                    V(lambda e, g=g: e.tensor_tensor(prev32[g][:, :].rearrange("p (h d) -> p h d", h=8), prev32[g][:, :].rearrange("p (h d) -> p h d", h=8),
                                                     cdec[:, g * 8:(g + 1) * 8].unsqueeze(2).broadcast_to([128, 8, 64]), op=ALU.mult), [prev32[g], cdec], [prev32[g]])
                    V(lambda e, g=g, st=st: e.tensor_tensor(prev32[g][:, :], prev32[g][:, :], st[:, :], op=ALU.add), [prev32[g], st], [prev32[g]])
                    G(lambda e, g=g: e.tensor_copy(prevb[g][:, :], prev32[g][:, :]), [prev32[g]], [prevb[g]])
                A(lambda e, zt=zt: e.activation(zt[:, :], zt[:, :], AF.Silu), [zt], [zt])
                V(lambda e, y=y, zt=zt: e.tensor_tensor(y[:, :], y[:, :], zt[:, :], op=ALU.mult), [y, zt], [y])
                G(lambda e: e.memset(ss2[:, :], 0.0), [], [ss2])
                for g in range(2):
                    A(lambda e, y=y, g=g: e.activation(junk[:, :], y[:, g * 512:(g + 1) * 512], AF.Square, accum_out=ss2[:, g:g + 1]), [y, ss2], [junk, ss2])
                A(lambda e: e.activation(ss2[:, 2:4], ss2[:, 0:2], AF.Sqrt, bias=EPS, scale=1.0 / 512), [ss2], [ss2])
                V(lambda e: e.reciprocal(ss2[:, 2:4], ss2[:, 2:4]), [ss2], [ss2])
                for g in range(2):
                    V(lambda e, y=y, ob=ob, g=g: e.scalar_tensor_tensor(ob[:, g * 512:(g + 1) * 512], y[:, g * 512:(g + 1) * 512], ss2[:, 2 + g:3 + g],
                                                                       nw_bc[:, g * 512:(g + 1) * 512], op0=ALU.mult, op1=ALU.mult), [y, ss2, nw_bc], [ob])
                pp = psb(C)
                for j in range(8):
                    P(lambda e, pp=pp, ob=ob, j=j: e.transpose(pp[:, j * 128:(j + 1) * 128], ob[:, j * 128:(j + 1) * 128], C.identb[:, :]), [ob, C.identb], [pp])
                copy_op(k, evac_eng(C), obT[:, :, :], pp[:, :].rearrange("p (a b) -> p a b", a=8), [pp], [obT])
                k.dma("sp", [(scr.mixT[512:1536, t0 + c * 128:t0 + (c + 1) * 128].rearrange("(j p) t -> p j t", p=128), obT[:, :, :])], reads=[obT], sembuf=obT)
        C.pfn = 6


def host_consts():
    slopes = np.array([2.0 ** (-(h + 1)) for h in range(8)], dtype=np.float64)
    etab = np.zeros((68, 8, 16, 128), dtype=np.float32)
    for h in range(8):
        for kt in range(16):
            etab[((h % 2) * 4 + h // 2) * 8 + kt // 2, h, kt, :] = 1.0
            etab[64, h, kt, :] = -128.0 * slopes[h]
            etab[65, h, kt, :] = -slopes[h]
            etab[66, h, kt, :] = slopes[h] * np.arange(128)
            etab[67, h, kt, :] = 128.0 * slopes[h] * kt
    auxc = np.zeros((4, S), dtype=np.float32)
    pos = np.arange(S)
    auxc[0] = pos // 128
    auxc[1] = pos % 128
    auxc[2] = 1.0
    auxc[3] = 1.0
    return {"etab": etab.reshape(68, 8 * 16 * 128), "auxc": auxc}


def build(upto="full", dumps=()):
    nc = bass.Bass("TRN2", target_bir_lowering=False)
    root = ExitStack()
    k = K(nc, root)
    _k_init_extra(k)
    k.cur_dsems = []
    C = Ctx()
    x_d = k.dram("x", [NTOK, D], F32, kind="ExternalInput").t
    p_d = [k.dram("p%d" % i, [NTOK, 256], F32, kind="ExternalInput").t for i in range(2)]
    Wt = {n: k.dram(n, shp, F32, kind="ExternalInput").t for n, shp in WSHAPES.items()}
    etab_d = k.dram("etab", [68, 8 * 16 * 128], F32, kind="ExternalInput").t
    auxc_d = k.dram("auxc", [4, S], F32, kind="ExternalInput").t
    y_d = k.dram("y", [NTOK, D], F32, kind="ExternalOutput").t
    scr = Ctx()
    scr.qkT = k.dram("s_qkT", [1024, NTOK], BF16).t
    scr.v = k.dram("s_v", [NTOK, 512], BF16).t
    scr.z = k.dram("s_z", [NTOK, 1024], F32).t
    scr.xbcT = k.dram("s_xbcT", [1536, NTOK], BF16).t
    scr.dt = k.dram("s_dt", [NTOK, 16], F32).t
    scr.mixT = k.dram("s_mixT", [2048, NTOK], BF16).t
    scr.actT = k.dram("s_actT", [DFF, NTOK], BF16).t
    scr.qkvT = k.dram("s_qkvT", [4096, NTOK], BF16).t
    scr.zT = k.dram("s_zT", [2048, NTOK], F32).t
    scr.ba = k.dram("s_ba", [NTOK, 32], F32).t
    xa = k.dram("s_xa", [NTOK, D], F32).t
    xb = k.dram("s_xb", [NTOK, D], F32).t
    mk_consts(k, C)
    k.barrier()
    if "auxT" in dumps:
        C.dbg_aux = k.dram("dbg_auxT", [68, S], BF16, kind="ExternalOutput").t
        dumps = [d_ for d_ in dumps if d_ != "auxT"]
    order = ["in0", "attn", "ssd", "x1", "x2", "x3", "in1", "gdn", "x4", "x5", "full"]
    lim = order.index(upto)
    last = x_d

    def done(name):
        return order.index(name) > lim

    stage_inproj0(k, C, x_d, Wt["norm_mix"][0], Wt["w_in_even"][0], scr)
    if not done("attn"):
        stage_attn(k, C, scr, Wt["moba_q_norm"][0], Wt["moba_k_norm"][0], etab_d, auxc_d)
    if not done("ssd"):
        stage_ssd(k, C, scr, Wt)
    if not done("x1"):
        stage_resid_linear(k, C, scr.mixT[0:1536, :], 12, Wt["w_out_even"][0], x_d, xa)
        last = xa
    if not done("x2"):
        stage_ffn_a(k, C, xa, Wt["norm_ffn"][0], Wt["ffn_w_gate"][0], Wt["ffn_w_up"][0], Wt["ffn_conv_w"][0], Wt["ffn_conv_b"][0], scr.actT)
        stage_resid_linear(k, C, scr.actT, 22, Wt["ffn_w_down"][0], xa, xb)
        last = xb
    if not done("x3"):
        stage_ple(k, C, xb, Wt["norm_ple"][0], Wt["ple_w_gate"][0], Wt["ple_w_proj"][0], p_d[0], xa)
        last = xa
    if not done("in1"):
        stage_inproj1(k, C, xa, Wt["norm_mix"][1], Wt["w_in_odd"][0], scr)
    if not done("gdn"):
        stage_gdn(k, C, scr, Wt)
    if not done("x4"):
        stage_resid_linear(k, C, scr.mixT, 16, Wt["w_out_odd"][0], xa, xb)
        last = xb
    if not done("x5"):
        stage_ffn_a(k, C, xb, Wt["norm_ffn"][1], Wt["ffn_w_gate"][1], Wt["ffn_w_up"][1], Wt["ffn_conv_w"][1], Wt["ffn_conv_b"][1], scr.actT)
        stage_resid_linear(k, C, scr.actT, 22, Wt["ffn_w_down"][1], xb, xa)
        last = xa
    if not done("full"):
        stage_ple(k, C, xa, Wt["norm_ple"][1], Wt["ple_w_gate"][1], Wt["ple_w_proj"][1], p_d[1], xb)
        last = xb
    with Stage(k):
        cp = k.sb("cpbuf", [128, 4, D], F32)
        for i in range(NTOK // 512):
            k.dma("sp", [(cp[:, :, :], last[i * 512:(i + 1) * 512, :].rearrange("(a p) d -> p a d", p=128))], writes=[cp], sembuf=cp)
            k.dma("sp", [(y_d[i * 512:(i + 1) * 512, :].rearrange("(a p) d -> p a d", p=128), cp[:, :, :])], reads=[cp], sembuf=cp)
        for name in dumps:
            src = getattr(scr, name)
            shp = list(src.shape)
            dd = k.dram("dbg_" + name, shp, src.dtype, kind="ExternalOutput").t
            rows = shp[0]
            cb2 = k.sb("cpb_" + name, [128, shp[1]], src.dtype)
            for i in range(rows // 128):
                k.dma("sp", [(cb2[:, :], src[i * 128:(i + 1) * 128, :])], writes=[cb2], sembuf=cb2)
                k.dma("sp", [(dd[i * 128:(i + 1) * 128, :], cb2[:, :])], reads=[cb2], sembuf=cb2)
    k.finish([])
    root.close()
    return nc


_NC_CACHE = {}


def kernel(**inputs):
    n = 8
    hc = host_consts()
    x = np.ascontiguousarray(inputs["x"], dtype=np.float32).reshape(n, NTOK, D)
    p = np.ascontiguousarray(inputs["p"], dtype=np.float32)
    in_maps = []
    for c in range(n):
        m = {"x": x[c], "p0": np.ascontiguousarray(p[0, 2 * c:2 * c + 2].reshape(NTOK, 256)),
             "p1": np.ascontiguousarray(p[1, 2 * c:2 * c + 2].reshape(NTOK, 256)),
             "etab": hc["etab"], "auxc": hc["auxc"]}
        for nme in WSHAPES:
            m[nme] = np.ascontiguousarray(inputs[nme], dtype=np.float32)
        in_maps.append(m)
    if "full" not in _NC_CACHE:
        _NC_CACHE["full"] = build("full")
    res = run_bass_kernel_spmd(_NC_CACHE["full"], in_maps, core_ids=list(range(n)))
    out = np.stack([r["y"] for r in res.results], axis=0)
    return out.reshape(16, S, D).astype(np.float32)


def stage_inproj1(k, C, x_src, gain_ap, Wd2, scr):
    with Stage(k):
        hT = [k.sb("hT%d" % i, [128, 8, 512], BF16) for i in range(NTOK // 512)]
        norm_to_hT(k, C, x_src, 0, NTOK, gain_ap, hT)
        wring = Ring([k.sb("w%d" % i, [128, 8, 512], BF16) for i in range(3)])
        stb = Ring([k.sb("stb%d" % i, [128, 512], BF16) for i in range(4)])
        stf = Ring([k.sb("stf%d" % i, [128, 512], F32) for i in range(3)])
        rhs_fn = lambda kc, tb: (hT[tb], hT[tb][:, kc, :])
        lhs_fn = lambda kc, tt: (hT[tt // 4], hT[tt // 4][:, kc, (tt % 4) * 128:(tt % 4 + 1) * 128])

        def ev_fm(dst, ring):
            def ev(c, m, tb, ps):
                st = ring.next()
                copy_op(k, evac_eng(C), st[0:m, :], ps[0:m, :], [ps], [st])
                k.dma("sp", [(dst[c:c + m, tb * 512:(tb + 1) * 512], st[0:m, :])], reads=[st], sembuf=st)
            return ev

        def ev_tm(cb, n, tt, ps):
            st = stf.next()
            copy_op(k, evac_eng(C), st[:, 0:n], ps[:, 0:n], [ps], [st])
            k.dma("sp", [(scr.ba[tt * 128:(tt + 1) * 128, cb:cb + n], st[:, 0:n])], reads=[st], sembuf=st)

        linear_fm(k, C, rhs_fn, 8, Wd2, 0, 0, 4096, NTOK // 512, ev_fm(scr.qkvT, stb), wring)
        linear_fm(k, C, rhs_fn, 8, Wd2, 0, 4096, 2048, NTOK // 512, ev_fm(scr.zT, stf), wring)
        linear_tm(k, C, lhs_fn, 8, Wd2, 0, 6144, 32, NTOK // 128, ev_tm, wring)


def stage_gdn(k, C, scr, Wt, nseq=NSEQ, nkh=8, nck=None, phases=(1, 2)):
    V = lambda fn, r, w: k.op("dve", fn, reads=r, writes=w)
    A = lambda fn, r, w: k.op("act", fn, reads=r, writes=w)
    G = lambda fn, r, w: k.op("pool", fn, reads=r, writes=w)
    P = lambda fn, r, w: k.op("pe", fn, reads=r, writes=w)
    NCK = S // 128
    with Stage(k):
        cw = k.sb("gcw", [128, 32, 4], F32)
        convw2 = Wt["gdn_conv_w"][0]
        load_cols(k, C, lambda c: cw[:, c, :], convw2, 4, 32)
        k.barrier()
        dtb_bc = bc_row(k, "gdtb", Wt["gdn_dt_bias"][0], 16)
        a_bc = bc_row(k, "ga", Wt["gdn_a_log"][0], 16)
        A(lambda e: e.activation(a_bc[:, :], a_bc[:, :], AF.Exp), [a_bc], [a_bc])
        V(lambda e: e.tensor_scalar(a_bc[:, :], a_bc[:, :], -1.0, None, op0=ALU.mult), [a_bc], [a_bc])
        nwc = k.sb("gnw", [128, 1], F32)
        load_col(k, nwc, Wt["gdn_norm"][0], 128, 1)
        ba = k.sb("gba", [128, NCK, 32], F32)
        bet = k.sb("gbet", [128, NCK, 16], F32)
        nbet = k.sb("gnbet", [128, NCK, 16], F32)
        gg = k.sb("ggg", [128, NCK, 16], F32)
        rawr = Ring([k.sb("graw%d" % i, [128, 3 + S], BF16) for i in range(2)])
        for r_ in rawr.bufs:
            G(lambda e, r_=r_: e.memset(r_[:, 0:3], 0.0), [], [r_])
        dgr = Ring([k.sb("gdg%d" % i, [128, 4, 128], BF16) for i in range(2)])
        cvr = Ring([k.sb("gcv%d" % i, [128, S], BF16) for i in range(2)])
        sqr = Ring([k.sb("gsq%d" % i, [128, 512], BF16) for i in range(2)])
        rsr = Ring([k.sb("grs%d" % i, [128, 512], F32) for i in range(2)])
        QT = k.sb("gQT", [128, S], BF16)
        KT = k.sb("gKT", [128, S], BF16)
        K_tm = k.sb("gK_tm", [128, NCK, 128], BF16)
        V_tm = k.sb("gV_tm", [128, NCK, 256], BF16)
        u0b = k.sb("gu0b", [128, NCK, 2, 128], BF16)
        w0T = k.sb("gw0T", [128, NCK, 2, 128], BF16)
        qkT = k.sb("gqkT", [128, NCK, 2, 128], BF16)
        QdT = k.sb("gQdT", [128, NCK, 2, 128], BF16)
        kdec = k.sb("gkdec", [128, NCK, 2, 128], BF16)
        egl = k.sb("gegl", [128, NCK, 2], F32)
        acsA = k.sb("gacsA", [128, NCK, 4], F32)
        ecolA = k.sb("gecolA", [128, NCK, 4], F32)
        gUall = k.sb("ggUall", [128, 8, 2, 128], F32)
        t1r = Ring([k.sb("gt1%d" % i, [128, 512], F32) for i in range(2)])
        decr = Ring([k.sb("gdec%d" % i, [128, 512], F32) for i in range(2)])
        eRr = Ring([k.sb("geR%d" % i, [128, 512], F32) for i in range(2)])
        tmpr = Ring([k.sb("gtmp%d" % i, [128, 512], F32) for i in range(2)])
        Ybuf = [[k.sb("gY%d_%d" % (g_, i), [128, 4, 128], BF16) for i in range(2)] for g_ in range(4)]
        Wbuf = [[k.sb("gW%d_%d" % (g_, i), [128, 4, 128], BF16) for i in range(2)] for g_ in range(4)]
        Tbuf = [[k.sb("gT%d_%d" % (g_, i), [128, 4, 128], BF16) for i in range(2)] for g_ in range(4)]
        kegA = [k.sb("gkeg%d" % g_, [128, 2, 2, 128], BF16) for g_ in range(4)]
        S32 = [k.sb("gS32_%d" % i, [128, 128], F32) for i in range(2)]
        Sb = [k.sb("gSb_%d" % i, [128, 128], BF16) for i in range(2)]
        vnr = Ring([k.sb("gvn%d" % i, [128, 128], BF16) for i in range(3)])
        szT = [k.sb("gsz%d" % i, [128, S], F32) for i in range(2)]
        outT = [k.sb("gout%d" % i, [128, S], BF16) for i in range(2)]
        osq = Ring([k.sb("gosq%d" % i, [128, 128], BF16) for i in range(6)])
        ocpr = Ring([k.sb("gocp%d" % i, [128, 128], F32) for i in range(6)])
        ors = Ring([k.sb("gors%d" % i, [128, 128], F32) for i in range(4)])
        otm = Ring([k.sb("gotm%d" % i, [128, 128], F32) for i in range(4)])

        def conv_chunk(ch, t0, dst):
            raw = rawr.next(); dg = dgr.next()
            k.dma("sp", [(raw[:, 3:3 + S], scr.qkvT[ch * 128:(ch + 1) * 128, t0:t0 + S])], writes=[raw], sembuf=raw)
            for tap in range(4):
                G(lambda e, dg=dg, tap=tap: e.tensor_scalar(dg[:, tap, :], C.identb[:, :], cw[:, ch, tap:tap + 1], None, op0=ALU.mult), [C.identb, cw], [dg])
            for tb in range(4):
                ps = psf(C)
                for tap in range(4):
                    P(lambda e, ps=ps, dg=dg, tap=tap, raw=raw, tb=tb: e.matmul(
                        ps[:, :], dg[:, tap, :], raw[:, tb * 512 + tap: tb * 512 + tap + 512], start=(tap == 0), stop=(tap == 3)), [dg, raw], [ps])
                A(lambda e, ps=ps, tb=tb: e.activation(dst[:, tb * 512:(tb + 1) * 512], ps[:, :], AF.Silu), [ps], [dst])

        def l2norm(src, dst, scale):
            for tb in range(4):
                sq = sqr.next(); rs = rsr.next(); ps = psf(C)
                A(lambda e, sq=sq, tb=tb: e.activation(sq[:, :], src[:, tb * 512:(tb + 1) * 512], AF.Square), [src], [sq])
                P(lambda e, ps=ps, sq=sq: e.matmul(ps[:, :], C.onesb[:, :], sq[:, :], start=True, stop=True), [C.onesb, sq], [ps])
                A(lambda e, ps=ps, rs=rs: e.activation(rs[:, :], ps[:, :], AF.Sqrt, bias=EPS, scale=1.0), [ps], [rs])
                V(lambda e, rs=rs: e.reciprocal(rs[:, :], rs[:, :]), [rs], [rs])
                V(lambda e, rs=rs, tb=tb: e.scalar_tensor_tensor(dst[:, tb * 512:(tb + 1) * 512], src[:, tb * 512:(tb + 1) * 512], scale, rs[:, :],
                                                                 op0=ALU.mult, op1=ALU.mult), [src, rs], [dst])

        C.pfn = 5
        for s in range(nseq):
            t0 = s * S
            k.dma("sp", [(ba[:, :, :], scr.ba[t0:t0 + S, :].rearrange("(c p) h -> p c h", p=128))], writes=[ba], sembuf=ba)
            A(lambda e: e.activation(bet[:, :, :], ba[:, :, 0:16], AF.Sigmoid), [ba], [bet])
            V(lambda e: e.tensor_scalar(nbet[:, :, :], bet[:, :, :], -1.0, None, op0=ALU.mult), [bet], [nbet])
            V(lambda e: e.tensor_tensor(gg[:, :, :], ba[:, :, 16:32], dtb_bc[:, :].unsqueeze(1).broadcast_to([128, NCK, 16]), op=ALU.add), [ba, dtb_bc], [gg])
            A(lambda e: e.activation(gg[:, :, :], gg[:, :, :], AF.Exp), [gg], [gg])
            A(lambda e: e.activation(gg[:, :, :], gg[:, :, :], AF.Ln, bias=1.0, scale=1.0), [gg], [gg])
            V(lambda e: e.tensor_tensor(gg[:, :, :], gg[:, :, :], a_bc[:, :].unsqueeze(1).broadcast_to([128, NCK, 16]), op=ALU.mult), [gg, a_bc], [gg])
            for kh in range(nkh):
                cq = cvr.next(); conv_chunk(kh, t0, cq); l2norm(cq, QT, 128.0 ** -0.5)
                ck = cvr.next(); conv_chunk(8 + kh, t0, ck); l2norm(ck, KT, 1.0)
                for c in range(0, NCK, 8):
                    pp = psb(C)
                    for j in range(8):
                        P(lambda e, pp=pp, j=j, c=c: e.transpose(pp[:, j * 128:(j + 1) * 128], KT[:, (c + j) * 128:(c + j + 1) * 128], C.identb[:, :]), [KT, C.identb], [pp])
                    copy_op(k, evac_eng(C), K_tm[:, c:c + 8, :], pp[:, :].rearrange("p (a b) -> p a b", a=8), [pp], [K_tm])
                for hv in range(2):
                    cv = cvr.next(); conv_chunk(16 + 2 * kh + hv, t0, cv)
                    for c in range(0, NCK, 8):
                        pp = psb(C)
                        for j in range(8):
                            P(lambda e, pp=pp, j=j, c=c, cv=cv: e.transpose(pp[:, j * 128:(j + 1) * 128], cv[:, (c + j) * 128:(c + j + 1) * 128], C.identb[:, :]), [cv, C.identb], [pp])
                        copy_op(k, evac_eng(C), V_tm[:, c:c + 8, hv * 128:(hv + 1) * 128], pp[:, :].rearrange("p (a b) -> p a b", a=8), [pp], [V_tm])
                    h = 2 * kh + hv
                    k.dma("sp", [(szT[hv][:, :], scr.zT[h * 128:(h + 1) * 128, t0:t0 + S])], writes=[szT[hv]], sembuf=szT[hv])
                    A(lambda e, hv=hv: e.activation(szT[hv][:, :], szT[hv][:, :], AF.Silu), [szT[hv]], [szT[hv]])
                NC1 = (nck or NCK) if 1 in phases else 0
                if NC1:
                    pa = psf(C)
                    for c in range(NC1):
                        P(lambda e, pa=pa, c=c, kh=kh: e.matmul(pa[:, c * 4:c * 4 + 2], C.U32[:, :], gg[:, c, 2 * kh:2 * kh + 2], start=True, stop=True), [C.U32, gg], [pa])
                        P(lambda e, pa=pa, c=c, kh=kh: e.matmul(pa[:, c * 4 + 2:c * 4 + 4], C.ones32[:, :], gg[:, c, 2 * kh:2 * kh + 2], start=True, stop=True), [C.ones32, gg], [pa])
                    V(lambda e, pa=pa: e.tensor_copy(acsA[:, 0:NC1, :], pa[:, 0:NC1 * 4].rearrange("p (c f) -> p c f", f=4)), [pa], [acsA])
                    A(lambda e: e.activation(ecolA[:, 0:NC1, 0:2], acsA[:, 0:NC1, 0:2], AF.Exp), [acsA], [ecolA])
                    V(lambda e: e.tensor_tensor(ecolA[:, 0:NC1, 2:4], acsA[:, 0:NC1, 2:4], acsA[:, 0:NC1, 0:2], op=ALU.subtract), [acsA], [ecolA])
                    A(lambda e: e.activation(ecolA[:, 0:NC1, 2:4], ecolA[:, 0:NC1, 2:4], AF.Exp), [ecolA], [ecolA])
                    A(lambda e: e.activation(egl[:, 0:NC1, :], acsA[:, 0:NC1, 2:4], AF.Exp), [acsA], [egl])
                for half0 in range(0, NC1, 8):
                    ncs = min(8, NC1 - half0)
                    ngr = ncs // 2
                    G(lambda e, half0=half0, ncs=ncs, kh=kh: e.tensor_tensor(
                        gUall[:, 0:ncs, :, :], C.U32[:, :].unsqueeze(1).unsqueeze(1).broadcast_to([128, ncs, 2, 128]),
                        gg[:, half0:half0 + ncs, 2 * kh:2 * kh + 2].unsqueeze(3).broadcast_to([128, ncs, 2, 128]), op=ALU.mult), [C.U32, gg], [gUall])
                    Yc = [None] * ngr; Wc = [None] * ngr; Tc = [None] * ngr
                    for gi in range(ngr):
                        c0 = half0 + 2 * gi
                        bsl = bet[:, c0:c0 + 2, 2 * kh:2 * kh + 2].unsqueeze(3).broadcast_to([128, 2, 2, 128])
                        v4 = lambda t: t[:, :].rearrange("p (a b c) -> p a b c", a=2, b=2)
                        R = psf(C); t1 = t1r.next(); dec = decr.next(); eR = eRr.next(); tmp = tmpr.next()
                        P(lambda e, R=R, gi=gi: e.matmul(R[:, :], C.ones32[:, :], gUall[:, 2 * gi:2 * gi + 2, :, :].rearrange("p a b c -> p (a b c)"), start=True, stop=True),
                          [C.ones32, gUall], [R])
                        V(lambda e, R=R, t1=t1, c0=c0: e.tensor_tensor(v4(t1), v4(R), acsA[:, c0:c0 + 2, 0:2].unsqueeze(3).broadcast_to([128, 2, 2, 128]), op=ALU.subtract), [R, acsA], [t1])
                        V(lambda e, t1=t1: e.tensor_scalar(t1[:, :], t1[:, :], 0.0, None, op0=ALU.min), [t1], [t1])
                        A(lambda e, R=R, eR=eR: e.activation(eR[:, :], R[:, :], AF.Exp), [R], [eR])
                        A(lambda e, t1=t1, dec=dec: e.activation(dec[:, :], t1[:, :], AF.Exp), [t1], [dec])
                        G(lambda e, dec=dec: e.tensor_tensor(dec[:, :].rearrange("p (a c) -> p a c", a=4), dec[:, :].rearrange("p (a c) -> p a c", a=4),
                                                             C.U32[:, :].unsqueeze(1).broadcast_to([128, 4, 128]), op=ALU.mult), [dec, C.U32], [dec])
                        pk = C.pf[5]
                        for cl in range(2):
                            cs = slice((c0 + cl) * 128, (c0 + cl + 1) * 128)
                            P(lambda e, cl=cl, cs=cs: e.matmul(pk[:, cl * 256:cl * 256 + 128], KT[:, cs], KT[:, cs], start=True, stop=True), [KT], [pk])
                            P(lambda e, cl=cl, cs=cs: e.matmul(pk[:, cl * 256 + 128:cl * 256 + 256], KT[:, cs], QT[:, cs], start=True, stop=True), [KT, QT], [pk])
                        pk3 = pk[:, :].rearrange("p (a f) -> p a f", a=2)
                        V(lambda e, tmp=tmp, dec=dec, pk3=pk3: e.tensor_tensor(v4(tmp), pk3[:, :, 0:128].unsqueeze(2).broadcast_to([128, 2, 2, 128]), v4(dec), op=ALU.mult), [pk, dec], [tmp])
                        V(lambda e, tmp=tmp, bsl=bsl: e.tensor_tensor(v4(tmp), v4(tmp), bsl, op=ALU.mult), [tmp, bet], [tmp])
                        X = Ybuf[gi][0]
                        G(lambda e, X=X, tmp=tmp: e.tensor_tensor(X[:, :, :], tmp[:, :].rearrange("p (a c) -> p a c", a=4), C.SU32[:, :].unsqueeze(1).broadcast_to([128, 4, 128]), op=ALU.mult),
                          [tmp, C.SU32], [X])
                        V(lambda e, dec=dec, pk3=pk3, c0=c0: e.tensor_tensor(qkT[:, c0:c0 + 2, :, :], pk3[:, :, 128:256].unsqueeze(2).broadcast_to([128, 2, 2, 128]), v4(dec), op=ALU.mult), [pk, dec], [qkT])
                        G(lambda e, eR=eR, c0=c0: e.tensor_tensor(QdT[:, c0:c0 + 2, :, :], QT[:, c0 * 128:(c0 + 2) * 128].rearrange("p (a c) -> p a c", a=2).unsqueeze(2).broadcast_to([128, 2, 2, 128]),
                                                                 v4(eR), op=ALU.mult), [QT, eR], [QdT])
                        kb4 = K_tm[:, c0:c0 + 2, :].unsqueeze(2).broadcast_to([128, 2, 2, 128])
                        G(lambda e, gi=gi, c0=c0, kb4=kb4: e.tensor_tensor(kegA[gi][:, :, :, :], kb4, ecolA[:, c0:c0 + 2, 0:2].unsqueeze(3).broadcast_to([128, 2, 2, 128]), op=ALU.mult), [K_tm, ecolA], [kegA[gi]])
                        G(lambda e, c0=c0, kb4=kb4: e.tensor_tensor(kdec[:, c0:c0 + 2, :, :], kb4, ecolA[:, c0:c0 + 2, 2:4].unsqueeze(3).broadcast_to([128, 2, 2, 128]), op=ALU.mult), [K_tm, ecolA], [kdec])
                        pp = psb(C)
                        for p4 in range(4):
                            P(lambda e, pp=pp, X=X, p4=p4: e.transpose(pp[:, p4 * 128:(p4 + 1) * 128], X[:, p4, :], C.identb[:, :]), [X, C.identb], [pp])
                        W = Wbuf[gi][0]
                        copy_op(k, evac_eng(C), W[:, :, :], pp[:, 0:512].rearrange("p (a c) -> p a c", a=4), [pp], [W])
                        Tt = Tbuf[gi][0]
                        V(lambda e, Tt=Tt, X=X: e.tensor_tensor(Tt[:, :, :], C.identb[:, :].unsqueeze(1).broadcast_to([128, 4, 128]), X[:, :, :], op=ALU.subtract), [C.identb, X], [Tt])
                        Yc[gi], Wc[gi], Tc[gi] = X, W, Tt
                    for lvl in range(1, 7):
                        nb_ = lvl % 2
                        for gi in range(ngr):
                            Y, W = Yc[gi], Wc[gi]
                            pw = psf(C)
                            for p4 in range(4):
                                P(lambda e, pw=pw, Y=Y, W=W, p4=p4: e.matmul(pw[:, p4 * 128:(p4 + 1) * 128], Y[:, p4, :], W[:, p4, :], start=True, stop=True), [Y, W], [pw])
                            if lvl < 6:
                                py = psf(C)
                                for p4 in range(4):
                                    P(lambda e, py=py, Y=Y, W=W, p4=p4: e.matmul(py[:, p4 * 128:(p4 + 1) * 128], W[:, p4, :], Y[:, p4, :], start=True, stop=True), [Y, W], [py])
                            W2 = Wbuf[gi][nb_]
                            copy_op(k, "act", W2[:, :, :], pw[:, :].rearrange("p (a c) -> p a c", a=4), [pw], [W2])
                            if lvl < 6:
                                Y2 = Ybuf[gi][nb_]
                                copy_op(k, "dve", Y2[:, :, :], py[:, :].rearrange("p (a c) -> p a c", a=4), [py], [Y2])
                                Yc[gi] = Y2
                            Wc[gi] = W2
                        for gi in range(ngr):
                            W, Tt = Wc[gi], Tc[gi]
                            pt_ = psf(C)
                            for p4 in range(4):
                                P(lambda e, pt_=pt_, W=W, Tt=Tt, p4=p4: e.matmul(pt_[:, p4 * 128:(p4 + 1) * 128], W[:, p4, :], Tt[:, p4, :], start=True, stop=False), [W, Tt], [pt_])
                                P(lambda e, pt_=pt_, Tt=Tt, p4=p4: e.matmul(pt_[:, p4 * 128:(p4 + 1) * 128], C.identb[:, :], Tt[:, p4, :], start=False, stop=True), [C.identb, Tt], [pt_])
                            Tt2 = Tbuf[gi][nb_]
                            copy_op(k, evac_eng(C), Tt2[:, :, :], pt_[:, :].rearrange("p (a c) -> p a c", a=4), [pt_], [Tt2])
                            Tc[gi] = Tt2
                    for gi in range(ngr):
                        c0 = half0 + 2 * gi
                        Tt = Tc[gi]
                        pu = psf(C); pw_ = psf(C)
                        for cl in range(2):
                            for hv in range(2):
                                p4 = cl * 2 + hv
                                P(lambda e, pu=pu, Tt=Tt, p4=p4, cl=cl, hv=hv, c0=c0: e.matmul(pu[:, p4 * 128:(p4 + 1) * 128], Tt[:, p4, :], V_tm[:, c0 + cl, hv * 128:(hv + 1) * 128], start=True, stop=True), [Tt, V_tm], [pu])
                                P(lambda e, pw_=pw_, Tt=Tt, p4=p4, cl=cl, hv=hv, gi=gi: e.matmul(pw_[:, p4 * 128:(p4 + 1) * 128], kegA[gi][:, cl, hv, :], Tt[:, p4, :], start=True, stop=True), [Tt, kegA[gi]], [pw_])
                        V(lambda e, pu=pu, c0=c0, kh=kh: e.tensor_tensor(u0b[:, c0:c0 + 2, :, :], pu[:, :].rearrange("p (a b c) -> p a b c", a=2, b=2),
                                                                      bet[:, c0:c0 + 2, 2 * kh:2 * kh + 2].unsqueeze(3).broadcast_to([128, 2, 2, 128]), op=ALU.mult), [pu, bet], [u0b])
                        copy_op(k, "act", w0T[:, c0:c0 + 2, :, :], pw_[:, :].rearrange("p (a b c) -> p a b c", a=2, b=2), [pw_], [w0T])
                for hv in range(2):
                    G(lambda e, hv=hv: e.memset(S32[hv][:, :], 0.0), [], [S32[hv]])
                    G(lambda e, hv=hv: e.memset(Sb[hv][:, :], 0.0), [], [Sb[hv]])
                pend_out = []

                def emit_post(c, hv, sq, ocp):
                    cs = slice(c * 128, (c + 1) * 128)
                    rs = ors.next(); tm_ = otm.next(); pn = psf(C)
                    P(lambda e, pn=pn, sq=sq: e.matmul(pn[:, 0:128], C.onesb[:, :], sq[:, :], start=True, stop=True), [C.onesb, sq], [pn])
                    A(lambda e, pn=pn, rs=rs: e.activation(rs[:, :], pn[:, 0:128], AF.Sqrt, bias=EPS, scale=1.0 / 128), [pn], [rs])
                    V(lambda e, rs=rs: e.reciprocal(rs[:, :], rs[:, :]), [rs], [rs])
                    V(lambda e, tm_=tm_, ocp=ocp, rs=rs: e.scalar_tensor_tensor(tm_[:, :], ocp[:, :], nwc[:, 0:1], rs[:, :], op0=ALU.mult, op1=ALU.mult), [ocp, nwc, rs], [tm_])
                    G(lambda e, tm_=tm_, hv=hv, cs=cs: e.tensor_tensor(outT[hv][:, cs], tm_[:, :], szT[hv][:, cs], op=ALU.mult), [tm_, szT[hv]], [outT[hv]])

                for c in range((nck or NCK) if 2 in phases else 0):
                    cur_out = []
                    for hv in range(2):
                        h = 2 * kh + hv
                        p1 = psf(C); vn = vnr.next()
                        P(lambda e, p1=p1, c=c, hv=hv: e.matmul(p1[:, 0:128], w0T[:, c, hv, :], Sb[hv][:, :], start=True, stop=True), [w0T, Sb[hv]], [p1])
                        V(lambda e, p1=p1, vn=vn, c=c, hv=hv, h=h: e.scalar_tensor_tensor(vn[:, :], p1[:, 0:128], nbet[:, c, h:h + 1], u0b[:, c, hv, :], op0=ALU.mult, op1=ALU.add),
                          [p1, nbet, u0b], [vn])
                        po = psf(C)
                        P(lambda e, po=po, c=c, hv=hv: e.matmul(po[:, 0:128], Sb[hv][:, :], QdT[:, c, hv, :], start=True, stop=False), [Sb[hv], QdT], [po])
                        P(lambda e, po=po, c=c, hv=hv, vn=vn: e.matmul(po[:, 0:128], vn[:, :], qkT[:, c, hv, :], start=False, stop=True), [vn, qkT], [po])
                        p2 = psf(C)
                        P(lambda e, p2=p2, c=c, hv=hv, vn=vn: e.matmul(p2[:, 0:128], kdec[:, c, hv, :], vn[:, :], start=True, stop=True), [kdec, vn], [p2])
                        V(lambda e, p2=p2, c=c, hv=hv: e.scalar_tensor_tensor(S32[hv][:, :], S32[hv][:, :], egl[:, c, hv:hv + 1], p2[:, 0:128], op0=ALU.mult, op1=ALU.add),
                          [S32[hv], egl, p2], [S32[hv]])
                        G(lambda e, hv=hv: e.tensor_copy(Sb[hv][:, :], S32[hv][:, :]), [S32[hv]], [Sb[hv]])
                        sq = osq.next(); ocp = ocpr.next()
                        A(lambda e, sq=sq, po=po: e.activation(sq[:, :], po[:, 0:128], AF.Square), [po], [sq])
                        A(lambda e, ocp=ocp, po=po: e.copy(ocp[:, :], po[:, 0:128]), [po], [ocp])
                        cur_out.append((c, hv, sq, ocp))
                    for args in pend_out:
                        emit_post(*args)
                    pend_out = cur_out
                for args in pend_out:
                    emit_post(*args)
                for hv in range(2):
                    h = 2 * kh + hv
                    k.dma("sp", [(scr.mixT[h * 128:(h + 1) * 128, t0:t0 + S], outT[hv][:, :])], reads=[outT[hv]], sembuf=outT[hv])
        C.pfn = 6
```
